# Optimizing a Trainium2 kernel written in Bass

```python
import math
import jax
import jax.numpy as jnp
from jax import lax
import numpy as np

D_MODEL = 1024
BATCH = 16
SEQ = 2048
DEPTH = 2

HEAD_DIM = 64
DSA_HEADS = 4
IDX_HEADS = 4
IDX_DIM = 32
DSA_TOPK = 256
MOBA_HEADS = 4
MOBA_BLOCK = 256
MOBA_TOPK = 3
MOBA_Q_CHUNK = 32
MLA_HEADS = 4
MLA_Q_LORA = 384
MLA_KV_LORA = 256
MLA_NOPE = 64
MLA_ROPE = 32
MLA_V = 64
ROPE_THETA = 10000.0
DIL_PATTERNS = ((128, 1), (512, 4), (2048, 16))
DIL_GROUPS = 3
DIL_HEADS = 4
N_BRANCH = 4
BRANCH_WIDTH = 4 * HEAD_DIM
D_FF = 2816
REL_BUCKETS = 32
REL_MAX_DIST = 2048
N_BIAS_HEADS = DSA_HEADS + MOBA_HEADS + DIL_GROUPS * DIL_HEADS
Q_BLOCK = 128
RMS_EPS = 1e-6

IN_SPLITS = (
    DSA_HEADS * HEAD_DIM,
    HEAD_DIM,
    HEAD_DIM,
    IDX_HEADS * IDX_DIM,
    IDX_DIM,
    IDX_HEADS,
    3 * MOBA_HEADS * HEAD_DIM,
    MLA_Q_LORA,
    MLA_KV_LORA,
    MLA_ROPE,
    3 * DIL_GROUPS * DIL_HEADS * HEAD_DIM,
    N_BRANCH * D_MODEL,
)
IN_WIDTH = sum(IN_SPLITS)
IN_OFFSETS = tuple(int(o) for o in np.cumsum(IN_SPLITS)[:-1])

kernel_name = 'hybrid_gated_sparse_mixer_trunk'


def rms_norm(x, gain):
    xf = x.astype(jnp.float32)
    xf = xf * lax.rsqrt(jnp.mean(xf * xf, axis=-1, keepdims=True) + RMS_EPS)
    return (xf * gain.astype(jnp.float32)).astype(x.dtype)


def rel_bucket(dist):
    n = jnp.maximum(dist, 0)
    max_exact = REL_BUCKETS // 2
    nf = jnp.maximum(n, 1).astype(jnp.float32)
    log_b = max_exact + (jnp.log(nf / max_exact) / math.log(REL_MAX_DIST / max_exact)
                         * (REL_BUCKETS - max_exact)).astype(jnp.int32)
    return jnp.where(n < max_exact, n, jnp.minimum(log_b, REL_BUCKETS - 1))


def masked_softmax(logits, mask):
    return jax.nn.softmax(jnp.where(mask, logits, -jnp.inf), axis=-1)


def apply_rope(x, pos):
    half = x.shape[-1] // 2
    freqs = ROPE_THETA ** (-jnp.arange(half, dtype=jnp.float32) / half)
    ang = pos.astype(jnp.float32)[:, None] * freqs[None, :]
    cos = jnp.cos(ang)[None, :, None, :]
    sin = jnp.sin(ang)[None, :, None, :]
    xf = x.astype(jnp.float32)
    x1, x2 = xf[..., :half], xf[..., half:]
    return jnp.concatenate([x1 * cos - x2 * sin, x1 * sin + x2 * cos], axis=-1).astype(x.dtype)


def to_blocks(a, block):
    b, t = a.shape[:2]
    return jnp.swapaxes(a.reshape(b, t // block, block, *a.shape[2:]), 0, 1)


def from_blocks(a):
    a = jnp.swapaxes(a, 0, 1)
    return a.reshape(a.shape[0], a.shape[1] * a.shape[2], *a.shape[3:])


def dsa_attention(q, k, v, iq, ik, iw, bias_table):
    b, t_len, _, dh = q.shape
    n_sel = min(DSA_TOPK, t_len // 4)
    key_pos = jnp.arange(t_len)
    gather = jax.vmap(lambda a, i: a[i])
    scale = dh ** -0.5

    def block(args):
        qb, iqb, iwb, start = args
        t = start + jnp.arange(Q_BLOCK)
        rel = jax.nn.relu(jnp.einsum('bqhc,bsc->bqsh', iqb, ik).astype(jnp.float32) * IDX_DIM ** -0.5)
        score = jnp.einsum('bqsh,bqh->bqs', rel, iwb.astype(jnp.float32))
        score = jnp.where(key_pos[None, None, :] <= t[None, :, None], score, -jnp.inf)
        _, sel = lax.top_k(score, n_sel)
        kg = gather(k, sel)
        vg = gather(v, sel)
        dist = t[None, :, None] - sel
        bias = jnp.moveaxis(bias_table[:, rel_bucket(dist)], 0, 2)
        logits = jnp.einsum('bqhc,bqkc->bqhk', qb, kg).astype(jnp.float32) * scale + bias
        p = masked_softmax(logits, (dist >= 0)[:, :, None, :])
        return jnp.einsum('bqhk,bqkc->bqhc', p.astype(vg.dtype), vg)

    starts = jnp.arange(t_len // Q_BLOCK) * Q_BLOCK
    out = lax.map(block, (to_blocks(q, Q_BLOCK), to_blocks(iq, Q_BLOCK), to_blocks(iw, Q_BLOCK), starts))
    return from_blocks(out)


def moba_attention(q, k, v, bias_table):
    b, t_len, h, dh = q.shape
    nb = -(-t_len // MOBA_BLOCK)
    t_pad = nb * MOBA_BLOCK
    pad = ((0, 0), (0, t_pad - t_len), (0, 0), (0, 0))
    kp = jnp.pad(k, pad)
    vp = jnp.pad(v, pad)
    kb = kp.reshape(b, nb, MOBA_BLOCK, h, dh)
    k_mean = jnp.mean(kb.astype(jnp.float32), axis=2)
    kbh = kb.transpose(0, 3, 1, 2, 4)
    vbh = vp.reshape(b, nb, MOBA_BLOCK, h, dh).transpose(0, 3, 1, 2, 4)
    n_sel = min(MOBA_TOPK, nb - 1)
    blk_ids = jnp.arange(nb)
    offs = jnp.arange(MOBA_BLOCK)
    head_ids = jnp.arange(h)[None, :, None, None]
    gather = jax.vmap(jax.vmap(lambda a, i: a[i]))
    scale = dh ** -0.5
    qc_len = MOBA_Q_CHUNK

    def chunk(args):
        qc, start = args
        t = start + jnp.arange(qc_len)
        own = start // MOBA_BLOCK
        ko = lax.dynamic_slice_in_dim(kp, own * MOBA_BLOCK, MOBA_BLOCK, axis=1)
        vo = lax.dynamic_slice_in_dim(vp, own * MOBA_BLOCK, MOBA_BLOCK, axis=1)
        own_dist = t[:, None] - (own * MOBA_BLOCK + offs)[None, :]
        logits = [jnp.einsum('bqhc,bkhc->bhqk', qc, ko).astype(jnp.float32) * scale
                  + bias_table[:, rel_bucket(own_dist)][None]]
        masks = [jnp.broadcast_to((own_dist >= 0)[None, None], (b, h, qc_len, MOBA_BLOCK))]
        if n_sel > 0:
            gate = jnp.einsum('bqhc,bnhc->bhqn', qc.astype(jnp.float32), k_mean)
            gate = jnp.where(blk_ids < own, gate, -jnp.inf)
            _, sel = lax.top_k(gate, n_sel)
            kg = gather(kbh, sel)
            vg = gather(vbh, sel)
            kpos = sel[..., None] * MOBA_BLOCK + offs
            past_dist = (t[:, None, None] - kpos).reshape(b, h, qc_len, n_sel * MOBA_BLOCK)
            past_logits = jnp.einsum('bqhc,bhqnkc->bhqnk', qc, kg).astype(jnp.float32) * scale
            logits.append(past_logits.reshape(b, h, qc_len, n_sel * MOBA_BLOCK)
                          + bias_table[head_ids, rel_bucket(past_dist)])
            masks.append(jnp.broadcast_to((sel < own)[..., None], sel.shape + (MOBA_BLOCK,))
                         .reshape(b, h, qc_len, n_sel * MOBA_BLOCK))
        p = masked_softmax(jnp.concatenate(logits, axis=-1), jnp.concatenate(masks, axis=-1))
        out = jnp.einsum('bhqk,bkhc->bqhc', p[..., :MOBA_BLOCK].astype(vo.dtype), vo)
        if n_sel > 0:
            p_past = p[..., MOBA_BLOCK:].reshape(b, h, qc_len, n_sel, MOBA_BLOCK)
            out = out + jnp.einsum('bhqnk,bhqnkc->bqhc', p_past.astype(vg.dtype), vg)
        return out

    starts = jnp.arange(t_len // qc_len) * qc_len
    out = lax.map(chunk, (to_blocks(q, qc_len), starts))
    return from_blocks(out)


def causal_dense_attention(q, k, v):
    t_len, c = q.shape[1], q.shape[-1]
    key_pos = jnp.arange(t_len)
    scale = c ** -0.5

    def block(args):
        qb, start = args
        t = start + jnp.arange(Q_BLOCK)
        logits = jnp.einsum('bqhc,bkhc->bhqk', qb, k).astype(jnp.float32) * scale
        p = masked_softmax(logits, key_pos[None, :] <= t[:, None])
        return jnp.einsum('bhqk,bkhc->bqhc', p.astype(v.dtype), v)

    starts = jnp.arange(t_len // Q_BLOCK) * Q_BLOCK
    return from_blocks(lax.map(block, (to_blocks(q, Q_BLOCK), starts)))


def dilated_attention(q, k, v, bias_table):
    b, t_len, _, h, dh = q.shape
    scale = dh ** -0.5
    outs, lses = [], []
    for g, (window, dil) in enumerate(DIL_PATTERNS):
        span = window // dil
        m = t_len // dil
        nbk = -(-m // span)
        mp = nbk * span

        def to_band(a):
            a = a.reshape(b, m, dil, h, dh).transpose(0, 2, 1, 3, 4)
            a = jnp.pad(a, ((0, 0), (0, 0), (0, mp - m), (0, 0), (0, 0)))
            return a.reshape(b, dil, nbk, span, h, dh)

        def with_prev(a):
            prev = jnp.pad(a[:, :, :-1], ((0, 0), (0, 0), (1, 0), (0, 0), (0, 0), (0, 0)))
            return jnp.concatenate([prev, a], axis=3)

        qs = to_band(q[:, :, g])
        kc = with_prev(to_band(k[:, :, g]))
        vc = with_prev(to_band(v[:, :, g]))
        qi = jnp.arange(span)[:, None]
        kj = jnp.arange(2 * span)[None, :]
        dist = span + qi - kj
        key_sub = (jnp.arange(nbk)[:, None, None] - 1) * span + kj[None]
        mask = (dist >= 0) & (dist <= span) & (key_sub >= 0)
        bias = bias_table[g * h:(g + 1) * h][:, rel_bucket(dist * dil)]
        logits = jnp.einsum('brnqhc,brnkhc->brnhqk', qs, kc).astype(jnp.float32) * scale + bias
        logits = jnp.where(mask[:, None], logits, -jnp.inf)
        lse = jax.nn.logsumexp(logits, axis=-1, keepdims=True)
        p = jnp.exp(logits - lse)
        o = jnp.einsum('brnhqk,brnkhc->brnqhc', p.astype(vc.dtype), vc)
        o = o.reshape(b, dil, mp, h, dh)[:, :, :m].transpose(0, 2, 1, 3, 4).reshape(b, t_len, h, dh)
        l = lse[..., 0].transpose(0, 1, 2, 4, 3).reshape(b, dil, mp, h)[:, :, :m]
        l = l.transpose(0, 2, 1, 3).reshape(b, t_len, h)
        outs.append(o)
        lses.append(l)
    alpha = jax.nn.softmax(jnp.stack(lses, axis=0), axis=0)
    return jnp.einsum('gbth,gbthc->bthc', alpha.astype(q.dtype), jnp.stack(outs, axis=0))


def swiglu(h, w_in, w_out):
    gate, up = jnp.split(h @ w_in, 2, axis=-1)
    return (jax.nn.silu(gate) * up) @ w_out


def hybrid_mixer(h, w_in, b_gate, qk_a, qk_b, qk_c, qk_d, mla_nq, w_uq, mla_nkv, w_ukv,
                 w_branch, w_out, rel_bias):
    b, t_len, _ = h.shape
    pos = jnp.arange(t_len)
    (a_q, a_k, a_v, i_q, i_k, i_w, b_qkv, c_q, c_kv, c_kr, d_qkv, gate_logits) = \
        jnp.split(h @ w_in, IN_OFFSETS, axis=-1)

    qa = rms_norm(a_q.reshape(b, t_len, DSA_HEADS, HEAD_DIM), qk_a[0])
    ka = rms_norm(a_k, qk_a[1])
    out_a = dsa_attention(qa, ka, a_v, i_q.reshape(b, t_len, IDX_HEADS, IDX_DIM), i_k,
                          i_w * IDX_HEADS ** -0.5, rel_bias[:DSA_HEADS])

    bqkv = b_qkv.reshape(b, t_len, 3, MOBA_HEADS, HEAD_DIM)
    out_b = moba_attention(rms_norm(bqkv[:, :, 0], qk_b[0]), rms_norm(bqkv[:, :, 1], qk_b[1]),
                           bqkv[:, :, 2], rel_bias[DSA_HEADS:DSA_HEADS + MOBA_HEADS])

    cq = (rms_norm(c_q, mla_nq) @ w_uq).reshape(b, t_len, MLA_HEADS, MLA_NOPE + MLA_ROPE)
    ckv = (rms_norm(c_kv, mla_nkv) @ w_ukv).reshape(b, t_len, MLA_HEADS, MLA_NOPE + MLA_V)
    kfull = jnp.concatenate(
        [ckv[..., :MLA_NOPE], jnp.broadcast_to(c_kr[:, :, None, :], (b, t_len, MLA_HEADS, MLA_ROPE))], axis=-1)
    qn = rms_norm(cq, qk_c[0])
    kn = rms_norm(kfull, qk_c[1])
    qn = jnp.concatenate([qn[..., :MLA_NOPE], apply_rope(qn[..., MLA_NOPE:], pos)], axis=-1)
    kn = jnp.concatenate([kn[..., :MLA_NOPE], apply_rope(kn[..., MLA_NOPE:], pos)], axis=-1)
    out_c = causal_dense_attention(qn, kn, ckv[..., MLA_NOPE:])

    dqkv = d_qkv.reshape(b, t_len, 3, DIL_GROUPS, DIL_HEADS, HEAD_DIM)
    out_d = dilated_attention(rms_norm(dqkv[:, :, 0], qk_d[0]), rms_norm(dqkv[:, :, 1], qk_d[1]),
                              dqkv[:, :, 2], rel_bias[DSA_HEADS + MOBA_HEADS:])

    branches = jnp.stack([o.reshape(b, t_len, BRANCH_WIDTH) for o in (out_a, out_b, out_c, out_d)], axis=2)
    gates = jax.nn.sigmoid(gate_logits.reshape(b, t_len, N_BRANCH, D_MODEL) + b_gate)
    merged = jnp.einsum('btnc,ncd->btnd', branches, w_branch)
    return jnp.sum(gates * merged, axis=2) @ w_out


def setup_inputs(seed: int = 0) -> dict:
    key = jax.random.key(seed)
    ks = jax.random.split(key, 17)

    def nrm(k, shape, scale):
        return scale * jax.random.normal(k, shape, jnp.float32)

    def gain(k, shape):
        return 1.0 + 0.05 * jax.random.normal(k, shape, jnp.float32)

    return {
        'x': nrm(ks[0], (BATCH, SEQ, D_MODEL), 1.0),
        'norm_gain': gain(ks[1], (DEPTH, 3, D_MODEL)),
        'w_in': nrm(ks[2], (DEPTH, D_MODEL, IN_WIDTH), D_MODEL ** -0.5),
        'b_gate': nrm(ks[3], (DEPTH, N_BRANCH, D_MODEL), 0.02),
        'qk_gain_a': gain(ks[4], (DEPTH, 2, HEAD_DIM)),
        'qk_gain_b': gain(ks[5], (DEPTH, 2, HEAD_DIM)),
        'qk_gain_c': gain(ks[6], (DEPTH, 2, MLA_NOPE + MLA_ROPE)),
        'qk_gain_d': gain(ks[7], (DEPTH, 2, HEAD_DIM)),
        'mla_norm_q': gain(ks[8], (DEPTH, MLA_Q_LORA)),
        'w_mla_uq': nrm(ks[9], (DEPTH, MLA_Q_LORA, MLA_HEADS * (MLA_NOPE + MLA_ROPE)), MLA_Q_LORA ** -0.5),
        'mla_norm_kv': gain(ks[10], (DEPTH, MLA_KV_LORA)),
        'w_mla_ukv': nrm(ks[11], (DEPTH, MLA_KV_LORA, MLA_HEADS * (MLA_NOPE + MLA_V)), MLA_KV_LORA ** -0.5),
        'w_branch': nrm(ks[12], (DEPTH, N_BRANCH, BRANCH_WIDTH, D_MODEL), BRANCH_WIDTH ** -0.5),
        'w_out': nrm(ks[13], (DEPTH, D_MODEL, D_MODEL), D_MODEL ** -0.5),
        'rel_bias': nrm(ks[14], (N_BIAS_HEADS, REL_BUCKETS), 0.5),
        'w_ffn_in': nrm(ks[15], (DEPTH, 2, D_MODEL, 2 * D_FF), D_MODEL ** -0.5),
        'w_ffn_out': nrm(ks[16], (DEPTH, 2, D_FF, D_MODEL), D_FF ** -0.5),
    }


def reference(x, norm_gain, w_in, b_gate, qk_gain_a, qk_gain_b, qk_gain_c, qk_gain_d, mla_norm_q,
              w_mla_uq, mla_norm_kv, w_mla_ukv, w_branch, w_out, rel_bias, w_ffn_in, w_ffn_out):
    for l in range(DEPTH):
        x = x + 0.5 * swiglu(rms_norm(x, norm_gain[l, 0]), w_ffn_in[l, 0], w_ffn_out[l, 0])
        x = x + hybrid_mixer(rms_norm(x, norm_gain[l, 1]), w_in[l], b_gate[l], qk_gain_a[l], qk_gain_b[l],
                             qk_gain_c[l], qk_gain_d[l], mla_norm_q[l], w_mla_uq[l], mla_norm_kv[l],
                             w_mla_ukv[l], w_branch[l], w_out[l], rel_bias)
        x = x + 0.5 * swiglu(rms_norm(x, norm_gain[l, 2]), w_ffn_in[l, 1], w_ffn_out[l, 1])
    return x
```

```python
import numpy as np
import math
from contextlib import ExitStack
import concourse.bass as bass
import concourse.mybir as mybir
from concourse.bass_utils import run_bass_kernel_spmd

F32 = mybir.dt.float32
BF16 = mybir.dt.bfloat16
AF = mybir.ActivationFunctionType
ALU = mybir.AluOpType

D = 1024
T = 2048
DFF = 2816
INW = 8388
NCH = 4
NEG = -30000.0
EPS = 1e-6

O_AQ, O_AK, O_AV, O_IQ, O_IK, O_IW = 0, 256, 320, 384, 512, 544
O_BQ, O_BK, O_BV = 548, 804, 1060
O_CQ, O_CKV, O_CKR = 1316, 1700, 1956
O_DQ, O_DK, O_DV = 1988, 2756, 3524
O_G = 4292

PAGE = 1024
ARENA_BYTES = 224000


class Reg:
    __slots__ = ("w", "r", "excl")

    def __init__(self, excl=False):
        self.w = None
        self.r = {}
        self.excl = excl


class TT:
    __slots__ = ("ap", "regs")

    def __init__(self, ap, regs):
        self.ap = ap
        self.regs = regs


ENGS = ["pe", "act", "dve", "pool", "sp"]


class Sched:
    def __init__(self):
        self.ops = {e: [] for e in ENGS}
        self.cnt = {e: 0 for e in ENGS}
        self.waited = {e: {} for e in ENGS}
        self.dma_tot = {}

    def _waits(self, eng, reads, writes, k):
        need = {}
        for r in reads:
            t = r.w
            if t is None:
                continue
            key, val = t
            if key == eng and eng == "pe":
                continue
            if need.get(key, 0) < val:
                need[key] = val
        for w in writes:
            toks = list(w.r.values())
            if w.w is not None:
                toks.append(w.w)
            for key, val in toks:
                if key == eng and eng == "pe":
                    continue
                if need.get(key, 0) < val:
                    need[key] = val
        out = []
        wd = self.waited[eng]
        for key, val in need.items():
            if wd.get(key, 0) >= val:
                continue
            wd[key] = val
            out.append((key, val))
        return out

    def op(self, eng, fn, reads=(), writes=(), mode=None):
        if any(r.excl for r in reads):
            writes = list(writes) + [r for r in reads if r.excl]
            reads = [r for r in reads if not r.excl]
        k = self.cnt[eng] + 1
        waits = self._waits(eng, reads, writes, k)
        self.cnt[eng] = k
        self.ops[eng].append((0, fn, waits, mode))
        tok = (eng, k)
        for r in reads:
            r.r[eng] = tok
        for w in writes:
            w.w = tok
            w.r = {}

    def dma(self, queue, fn, stream, reads=(), writes=()):
        waits = self._waits(queue, reads, writes, 1 << 60)
        tot = self.dma_tot.get(stream, 0) + 16
        self.dma_tot[stream] = tot
        self.ops[queue].append((1, fn, waits, stream))
        key = ("D", stream)
        tok = (key, tot)
        for r in reads:
            r.r[key] = tok
        for w in writes:
            w.w = tok
            w.r = {}

    def final_waits(self, eng, streams):
        waits = []
        for s in streams:
            if s in self.dma_tot:
                waits.append((("D", s), self.dma_tot[s]))
        self.ops[eng].append((2, None, waits, None))

    def emit(self, nc):
        with ExitStack() as es:
            sems = {}
            for e in ENGS:
                sems[e] = es.enter_context(nc.semaphore("s_" + e))
            for i, s in enumerate(self.dma_tot):
                sems[("D", s)] = es.enter_context(nc.semaphore("d%d" % i))
            block = es.enter_context(nc.Block())
            names = {"pe": "tensor", "act": "scalar", "dve": "vector", "pool": "gpsimd", "sp": "sync"}

            def mk(e):
                def body(h):
                    se = sems[e]
                    last_mode = (128, 128, 0)
                    for kind, fn, waits, stream in self.ops[e]:
                        for key, val in waits:
                            h.wait_ge(sems[key], val)
                        if kind == 0:
                            if e == "pe":
                                md = stream if stream is not None else (128, 128, 0)
                                if md != last_mode:
                                    h.drain()
                                    self.ndrain = getattr(self, "ndrain", 0) + 1
                                    last_mode = md
                            fn(h).then_inc(se, 1)
                        elif kind == 1:
                            fn(h).then_inc(sems[("D", stream)], 16)
                return body

            for e in ENGS:
                getattr(block, names[e])(mk(e))


def rel_bucket_np(dist):
    n = np.maximum(dist, 0)
    max_exact = 16
    nf = np.maximum(n, 1).astype(np.float32)
    log_b = max_exact + (np.log(nf / np.float32(max_exact)) / np.float32(math.log(2048 / max_exact))
                         * np.float32(32 - max_exact)).astype(np.int32)
    return np.where(n < max_exact, n, np.minimum(log_b, 31))


def host_consts():
    c = {}
    eye = np.eye(128, dtype=np.float32)
    c["ident"] = eye
    c["anti"] = eye[::-1].copy()
    c["ones"] = np.ones((128, 128), np.float32)
    bd = np.zeros((128, 128), np.float32)
    bd[:64, :64] = 1
    bd[64:, 64:] = 1
    c["bd64"] = bd
    return c


def host_consts2():
    c = {}
    c["sel"] = np.kron(np.eye(32, dtype=np.float32), np.ones((1, 128), np.float32))
    bm = np.zeros((8, 4, 8), np.float32)
    for own in range(8):
        bm[own, :, own:] = -1e30
    c["bmk"] = np.broadcast_to(bm.reshape(1, 256), (128, 256)).copy()
    E = np.zeros((64, 128), np.float32)
    for k in range(64):
        E[k, (k // 32) * 64:(k // 32) * 64 + 64] = 1
    c["E"] = E
    cmm = np.where(np.arange(128)[None, :] <= np.arange(128)[:, None], 0.0, -1e30).astype(np.float32)
    c["cm"] = cmm
    c["F"] = E.T.copy()
    G = np.zeros((64, 64), np.float32)
    G[:32, :32] = 1
    G[32:, 32:] = 1
    c["G"] = G
    rot = np.zeros((64, 64), np.float32)
    for b in range(2):
        for m in range(16):
            rot[b * 32 + m + 16, b * 32 + m] = -1.0
            rot[b * 32 + m, b * 32 + m + 16] = 1.0
    c["rot"] = rot
    freqs = 10000.0 ** (-np.arange(16, dtype=np.float32) / 16)
    ang = np.arange(T, dtype=np.float32)[None, :] * np.tile(freqs, 4)[:, None].astype(np.float32)
    c["cos"] = np.cos(ang).astype(np.float32)
    c["sin"] = np.sin(ang).astype(np.float32)
    return c


class Builder:
    def __init__(self, nseq=2, stages=99, dbg=None):
        self.nseq = nseq
        self.stages = stages
        self.dbg = dbg
        self.S = Sched()
        self.nc = bass.Bass("TRN2", target_bir_lowering=False, dynamic_dma_scratch_size=4096)
        self.din = {}
        self.wslot_i = 0
        self.bank_i = 0
        self.bankA_i = 0

    def dram_in(self, name, shape, dt=F32):
        t = self.nc.dram_tensor(name, list(shape), dt, kind="ExternalInput").ap()
        self.din[name] = t
        return t

    def carve(self, off, free_shape, dt):
        n = 1
        for s in free_shape:
            n *= s
        nb = n * (4 if dt == F32 else 2)
        assert off % 4 == 0 and off + nb <= ARENA_BYTES, (off, nb)
        ap = self.arena[:, off // 2:(off + nb) // 2]
        if dt == F32:
            ap = ap.bitcast(F32)
        if len(free_shape) == 2:
            ap = ap.rearrange("p (a b) -> p a b", a=free_shape[0])
        elif len(free_shape) == 3:
            ap = ap.rearrange("p (a b c) -> p a b c", a=free_shape[0], b=free_shape[1])
        return ap, nb

    def lalloc(self, free_shape, dt):
        ap, nb = self.carve(self.loc_base + self.loc_cur, free_shape, dt)
        p0 = self.loc_cur // PAGE
        self.loc_cur += (nb + PAGE - 1) // PAGE * PAGE
        p1 = self.loc_cur // PAGE
        assert self.loc_base + self.loc_cur <= ARENA_BYTES, ("local overflow", self.loc_cur)
        return TT(ap, self.loc_pages[p0:p1])

    def lreset(self):
        self.loc_cur = 0

    def bank(self):
        b = self.bank_i
        self.bank_i = (b + 1) % 4
        return b

    def bankA(self):
        b = self.bankA_i
        self.bankA_i = (b + 1) % 4
        return 4 + b

    def mm(self, out, lhsT, rhs, start, stop, reads, writes):
        ru = lambda v: 32 if v <= 32 else (64 if v <= 64 else 128)
        kk = lhsT.shape[0]
        mmm = 1
        for d in lhsT.shape[1:]:
            mmm *= d
        self.S.op("pe", lambda h: h.matmul(out, lhsT, rhs, start=start, stop=stop), reads, writes,
                  mode=(ru(kk), ru(mmm), lhsT.offset // (lhsT.tensor.shape[1] * 32) if kk < 128 else 0))

    def act(self, out, in_, func, reads, writes, scale=1.0, bias=0.0, accum=None):
        if accum is None:
            self.S.op("act", lambda h: h.activation(out=out, in_=in_, func=func, scale=scale, bias=bias),
                      reads, writes)
        else:
            self.S.op("act", lambda h: h.activation(out=out, in_=in_, func=func, scale=scale, bias=bias,
                                                      accum_out=accum), reads, writes)

    def ts(self, eng, out, in0, s1, op0, reads, writes, s2=None, op1=None, accum=None):
        def fn(h):
            kw = {}
            if op1 is not None:
                kw["op1"] = op1
            if accum is not None:
                kw["accum_out"] = accum
            return h.tensor_scalar(out=out, in0=in0, scalar1=s1, scalar2=s2, op0=op0, **kw)
        self.S.op(eng, fn, reads, writes)

    def stt(self, out, in0, scalar, in1, op0, op1, reads, writes):
        self.S.op("dve", lambda h: h.scalar_tensor_tensor(out=out, in0=in0, scalar=scalar, in1=in1,
                                                            op0=op0, op1=op1), reads, writes)

    def tt(self, eng, out, in0, in1, op, reads, writes):
        self.S.op(eng, lambda h: h.tensor_tensor(out=out, in0=in0, in1=in1, op=op), reads, writes)

    def copy(self, eng, out, in_, reads, writes):
        if eng == "act":
            self.S.op("act", lambda h: h.copy(out=out, in_=in_), reads, writes)
        else:
            self.S.op(eng, lambda h: h.tensor_copy(out=out, in_=in_), reads, writes)

    def recip(self, out, in_, reads, writes):
        self.S.op("dve", lambda h: h.reciprocal(out=out, in_=in_), reads, writes)

    def dma(self, queue, out, in_, stream, reads, writes):
        if queue == "pool":
            self.S.dma(queue, lambda h: h.dma_start(out=out, in_=in_, max_dma_last_dim=2048), stream, reads, writes)
        else:
            self.S.dma(queue, lambda h: h.dma_start(out=out, in_=in_), stream, reads, writes)

    def wload(self, pieces, nk):
        s = self.wslot_i
        self.wslot_i = (s + 1) % len(self.wslots)
        ap_full, reg = self.wslots[s]
        tot = max(o + n for _, o, n in pieces)
        assert nk * tot * 2 <= 8192, (nk, tot)
        view = ap_full[:, 0:nk * tot].rearrange("p (k c) -> p k c", k=nk)
        for src, o, n in pieces:
            srcv = src.rearrange("(k p) c -> p k c", p=128)
            self.dma("pool", view[:, :, o:o + n], srcv, ("w", s), [], [reg])
        return view, [reg]

    def build(self):
        nc = self.nc
        ns = self.nseq
        x_in = self.dram_in("x", (ns, T, D))
        wfi = self.dram_in("w_ffn_in", (2, 2, D, 2 * DFF))
        wfo = self.dram_in("w_ffn_out", (2, 2, DFF, D))
        vecs = self.dram_in("vecs", (128, NVEC))
        self.w_in = self.dram_in("w_in", (2, D, INW))
        self.w_branch = self.dram_in("w_branch", (2, 4, 256, D))
        self.w_out = self.dram_in("w_out", (2, D, D))
        self.w_uq = self.dram_in("w_mla_uq", (2, 384, 384))
        self.w_ukv = self.dram_in("w_mla_ukv", (2, 256, 512))
        self.dram_in("gext", (9, NG))
        self.dram_in("gd", (12, NGD))
        for k, v in host_consts2().items():
            self.dram_in("c_" + k, v.shape)
        if self.dbg:
            self.dbg_out = self.nc.dram_tensor("dbg", [4, 128, 2, T], BF16, kind="ExternalOutput").ap()
        cst = {k: self.dram_in("c_" + k, v.shape) for k, v in host_consts().items()}
        self.y_out = nc.dram_tensor("y", [ns, T, D], F32, kind="ExternalOutput").ap()
        with ExitStack() as es:
            arena_t = es.enter_context(nc.sbuf_tensor("arena", [128, ARENA_BYTES // 2], BF16))
            self.arena = arena_t[:, :]
            ps_t = es.enter_context(nc.psum_tensor("ps", [128, 8, 512], F32))
            self.PS = ps_t
            self.PSR = [Reg(excl=True) for _ in range(8)]
            off = 0
            self.xT, nb = self.carve(off, (8, T), F32); off += nb
            self.xTr = [[Reg() for _ in range(NCH)] for _ in range(8)]
            self.hT, nb = self.carve(off, (8, T), BF16); off += nb
            self.hTr = [[Reg() for _ in range(NCH)] for _ in range(8)]
            self.QO = []
            self.QOr = []
            for n in range(4):
                q, nb = self.carve(off, (2, T), BF16); off += nb
                self.QO.append(q)
                self.QOr.append([[Reg() for _ in range(NCH)] for _ in range(2)])
            self.wslots = []
            for s in range(3):
                w, nb = self.carve(off, (4096,), BF16); off += nb
                self.wslots.append((w, Reg()))
            self.cT = {}
            creg = Reg()
            self.creg = creg
            for k, v in host_consts().items():
                ap, nb = self.carve(off, (v.shape[1],), BF16); off += nb
                self.cT[k] = ap
                self.dma("pool", ap, cst[k], "const", [], [creg])
            ap, nb = self.carve(off, (128,), F32); off += nb
            self.identf = ap
            self.dma("sp", ap, cst["ident"], "constf", [], [creg])
            ap, nb = self.carve(off, (NVEC,), F32); off += nb
            self.vecs = ap
            self.eps_ap = ap[:, VEC_EPS:VEC_EPS + 1]
            self.dma("sp", ap, vecs, "constf", [], [creg])
            off = (off + PAGE - 1) // PAGE * PAGE
            self.loc_base = off
            npages = (ARENA_BYTES - off) // PAGE
            self.loc_pages = [Reg() for _ in range(npages)]
            self.loc_cur = 0
            print("persistent bytes", off, "local pages", npages)

            for b in range(ns):
                self.load_x(x_in, b)
                st = 0
                for l in range(2):
                    for f in range(2):
                        if f == 1:
                            if st < self.stages:
                                self.mixer(l)
                                if self.dbg:
                                    st = 1000
                            st += 1
                        if st < self.stages:
                            self.ffn(wfi[l, f], wfo[l, f], VEC_NG + (l * 3 + (0 if f == 0 else 2)) * 8)
                        st += 1
                self.store_x(b)
            self.S.final_waits("sp", ["out0", "out1"])
            self.S.emit(nc)
        return nc

    def load_x(self, x_in, b):
        self.lreset()
        stg = [self.lalloc((4, D), F32) for _ in range(2)]
        for c in range(NCH):
            s = stg[c % 2]
            src = x_in[b, c * 512:(c + 1) * 512, :].rearrange("(a p) d -> p a d", p=128)
            self.dma("sp", s.ap, src, "xin%d" % (c % 2), [], s.regs)
            for j in range(8):
                bk = self.bank()
                for a in range(4):
                    o = self.PS[:, bk, a * 128:(a + 1) * 128]
                    i = s.ap[:, a, j * 128:(j + 1) * 128]
                    self.S.op("pe", lambda h, o=o, i=i: h.transpose(o, i, self.identf),
                              s.regs + [self.creg], [self.PSR[bk]])
                dst = self.xT[:, j, c * 512:(c + 1) * 512]
                self.copy("act" if j % 2 else "dve", dst, self.PS[:, bk, :], [self.PSR[bk]], [self.xTr[j][c]])

    def store_x(self, b):
        self.lreset()
        stg = [self.lalloc((4, D), F32) for _ in range(2)]
        for c in range(NCH):
            s = stg[c % 2]
            for a in range(4):
                for jj in range(2):
                    bk = self.bank()
                    for j4 in range(4):
                        j = jj * 4 + j4
                        o = self.PS[:, bk, j4 * 128:(j4 + 1) * 128]
                        i = self.xT[:, j, c * 512 + a * 128: c * 512 + (a + 1) * 128]
                        self.S.op("pe", lambda h, o=o, i=i: h.transpose(o, i, self.identf),
                                  [self.xTr[j][c], self.creg], [self.PSR[bk]])
                    dst = s.ap[:, a, jj * 512:(jj + 1) * 512]
                    self.copy("act" if jj % 2 else "dve", dst, self.PS[:, bk, :], [self.PSR[bk]], s.regs)
            dstd = self.y_out[b, c * 512:(c + 1) * 512, :].rearrange("(a p) d -> p a d", p=128)
            self.dma("sp", dstd, s.ap, "out%d" % (c % 2), s.regs, [])

    def rmsnorm_x(self, gain_col):
        sq = [self.lalloc((512,), BF16) for _ in range(2)]
        rs = self.lalloc((512,), F32)
        ones = self.cT["ones"]
        for c in range(NCH):
            cs = slice(c * 512, (c + 1) * 512)
            bk = self.bank()
            for j in range(8):
                q = sq[j % 2]
                self.act(q.ap, self.xT[:, j, cs], AF.Square, [self.xTr[j][c]], q.regs)
                self.mm(self.PS[:, bk, :], ones, q.ap, j == 0, j == 7, q.regs + [self.creg], [self.PSR[bk]])
            self.act(rs.ap, self.PS[:, bk, :], AF.Sqrt, [self.PSR[bk]], rs.regs, scale=1.0 / D, bias=self.eps_ap)
            self.recip(rs.ap, rs.ap, rs.regs, rs.regs)
            for j in range(8):
                g = self.vecs[:, gain_col + j:gain_col + j + 1]
                self.stt(self.hT[:, j, cs], self.xT[:, j, cs], g, rs.ap, ALU.mult, ALU.mult,
                         [self.xTr[j][c], self.creg] + rs.regs, [self.hTr[j][c]])

    def ffn(self, w_in, w_out, gain_col):
        self.lreset()
        self.rmsnorm_x(gain_col)
        gT = self.lalloc((8, T), BF16)
        gp = lambda i, c: gT.regs[i * 4 + c: i * 4 + c + 1]
        sl = [self.lalloc((512,), F32) for _ in range(2)]
        sli = 0
        for (g0, nf) in ((0, 8), (8, 8), (16, 6)):
            for fb in range(0, nf, 2):
                f0 = (g0 + fb) * 128
                wv, wr = self.wload([(w_in[:, f0:f0 + 256], 0, 256),
                                     (w_in[:, DFF + f0:DFF + f0 + 256], 256, 256)], 8)
                for c in range(NCH):
                    cs = slice(c * 512, (c + 1) * 512)
                    for ft in range(2):
                        bg = self.bank()
                        bu = self.bankA()
                        for k in range(8):
                            self.mm(self.PS[:, bg, :], wv[:, k, ft * 128:(ft + 1) * 128], self.hT[:, k, cs],
                                    k == 0, k == 7, wr + [self.hTr[k][c]], [self.PSR[bg]])
                        for k in range(8):
                            self.mm(self.PS[:, bu, :], wv[:, k, 256 + ft * 128:256 + (ft + 1) * 128],
                                    self.hT[:, k, cs], k == 0, k == 7, wr + [self.hTr[k][c]], [self.PSR[bu]])
                        s = sl[sli % 2]
                        sli += 1
                        self.act(s.ap, self.PS[:, bg, :], AF.Silu, [self.PSR[bg]], s.regs)
                        self.tt("dve", gT.ap[:, fb + ft, cs], s.ap, self.PS[:, bu, :], ALU.mult,
                                s.regs + [self.PSR[bu]], gp(fb + ft, c))
            for dq in range(2):
                wv, wr = self.wload([(w_out[g0 * 128:(g0 + nf) * 128, dq * 512:(dq + 1) * 512], 0, 512)], nf)
                for c in range(NCH):
                    cs = slice(c * 512, (c + 1) * 512)
                    for dt in range(4):
                        d = dq * 4 + dt
                        bk = self.bank()
                        for i in range(nf):
                            self.mm(self.PS[:, bk, :], wv[:, i, dt * 128:(dt + 1) * 128], gT.ap[:, i, cs],
                                    i == 0, i == nf - 1, wr + gp(i, c), [self.PSR[bk]])
                        self.stt(self.xT[:, d, cs], self.PS[:, bk, :], 0.5, self.xT[:, d, cs], ALU.mult, ALU.add,
                                 [self.PSR[bk], self.xTr[d][c]], [self.xTr[d][c]])


VEC_NG = 0
VEC_EPS = 48
VEC_BG = 56
VEC_QK = 120
NVEC = 160
NG = 2175
NGD = 383


def make_vecs(inp):
    v = np.zeros((128, NVEC), np.float32)
    ng = np.asarray(inp["norm_gain"], np.float32)
    v[:, VEC_NG:VEC_NG + 48] = ng.reshape(6, 8, 128).transpose(2, 0, 1).reshape(128, 48)
    v[:, VEC_EPS] = EPS
    v[:, VEC_EPS + 1] = EPS * 64
    v[:, VEC_EPS + 2] = EPS * 96
    bg = np.asarray(inp["b_gate"], np.float32)
    v[:, VEC_BG:VEC_BG + 64] = bg.reshape(8, 8, 128).transpose(2, 0, 1).reshape(128, 64)
    t2 = lambda a: np.concatenate([a, a])
    for l in range(2):
        o = VEC_QK + l * 16
        v[:, o + 0] = t2(np.asarray(inp["qk_gain_a"])[l, 0])
        v[:, o + 1] = t2(np.asarray(inp["qk_gain_a"])[l, 1])
        v[:, o + 2] = t2(np.asarray(inp["qk_gain_b"])[l, 0])
        v[:, o + 3] = t2(np.asarray(inp["qk_gain_b"])[l, 1])
        v[:, o + 4] = t2(np.asarray(inp["qk_gain_d"])[l, 0])
        v[:, o + 5] = t2(np.asarray(inp["qk_gain_d"])[l, 1])
        qc = np.asarray(inp["qk_gain_c"], np.float32)
        v[:, o + 6] = t2(qc[l, 0, :64])
        v[:, o + 7] = t2(qc[l, 1, :64])
        v[:64, o + 8] = t2(qc[l, 0, 64:])
        v[:64, o + 9] = t2(qc[l, 1, 64:])
        v[:, o + 10:o + 13] = np.asarray(inp["mla_norm_q"], np.float32)[l].reshape(3, 128).T
        v[:, o + 13:o + 15] = np.asarray(inp["mla_norm_kv"], np.float32)[l].reshape(2, 128).T
    return v


def make_gext(inp):
    rb = np.asarray(inp["rel_bias"], np.float32)
    dist = np.arange(NG) - 127
    bk = rel_bucket_np(dist)
    g = np.full((9, NG), NEG, np.float32)
    for h in range(8):
        g[h, 127:] = rb[h, bk[127:]]
    g[8, 127:] = 0.0
    gd = np.full((12, NGD), NEG, np.float32)
    dd = np.arange(NGD) - 127
    for gi, dil in enumerate((1, 4, 16)):
        ok = (dd >= 0) & (dd <= 128)
        b2 = rel_bucket_np(dd * dil)
        for h in range(4):
            gd[gi * 4 + h, ok] = rb[8 + gi * 4 + h, b2[ok]]
    return g, gd


def _hn_alloc(self):
    self.hn_sq = [self.lalloc((512,), BF16) for _ in range(2)]
    self.hn_tmp = [self.lalloc((512,), F32) for _ in range(2)]
    self.hn_rs = self.lalloc((512,), F32)
    self.hn_i = 0


def _vcol(self, c):
    return self.vecs[:, c:c + 1]


def _proj_fm(self, w, pieces, cb, tiles=None):
    sp = []
    o = 0
    for c0, n in pieces:
        sp.append((w[:, c0:c0 + n], o, n))
        o += n
    wv, wr = self.wload(sp, 8)
    if tiles is None:
        tiles = [(i * 128, min(128, o - i * 128)) for i in range((o + 127) // 128)]
    for c in range(NCH):
        cs = slice(c * 512, (c + 1) * 512)
        for ti, (tc0, m) in enumerate(tiles):
            bk = self.bank()
            for k in range(8):
                self.mm(self.PS[0:m, bk, :], wv[:, k, tc0:tc0 + m], self.hT[:, k, cs], k == 0, k == 7,
                        wr + [self.hTr[k][c]], [self.PSR[bk]])
            cb(ti, c, bk, m)


def _proj_tm(self, w, pieces, cb, tok_ap=None):
    sp = []
    o = 0
    for c0, n in pieces:
        sp.append((w[:, c0:c0 + n], o, n))
        o += n
    wv, wr = self.wload(sp, 8)
    for tt in range(16):
        bk = self.bank()
        for k in range(8):
            if tok_ap is None:
                lt = self.hT[:, k, tt * 128:(tt + 1) * 128]
                rd = [self.hTr[k][tt // 4]]
            else:
                lt, rd = tok_ap(k, tt)
            self.mm(self.PS[:, bk, 0:o], lt, wv[:, k, 0:o], k == 0, k == 7, wr + rd, [self.PSR[bk]])
        cb(tt, bk, o)


def _head_norm(self, bk, m, gain_col, eps_col, ss_scale, dst_ap, dst_regs, blk="bd64"):
    i = self.hn_i
    self.hn_i += 1
    sq = self.hn_sq[i % 2]
    tmp = self.hn_tmp[i % 2]
    rs = self.hn_rs
    pr = [self.PSR[bk]]
    self.act(sq.ap[0:m, :], self.PS[0:m, bk, :], AF.Square, pr, sq.regs)
    self.copy("act", tmp.ap[0:m, :], self.PS[0:m, bk, :], pr, tmp.regs)
    b2 = self.bankA()
    self.mm(self.PS[0:m, b2, :], self.cT[blk][0:m, 0:m], sq.ap[0:m, :], True, True, sq.regs + [self.creg], [self.PSR[b2]])
    self.act(rs.ap[0:m, :], self.PS[0:m, b2, :], AF.Sqrt, [self.PSR[b2]], rs.regs, scale=ss_scale,
             bias=self.vecs[0:m, eps_col:eps_col + 1])
    self.recip(rs.ap[0:m, :], rs.ap[0:m, :], rs.regs, rs.regs)
    self.stt(dst_ap, tmp.ap[0:m, :], self.vecs[0:m, gain_col:gain_col + 1], rs.ap[0:m, :], ALU.mult, ALU.mult,
             tmp.regs + rs.regs + [self.creg], dst_regs)


def _load_strip(self, dst, src_dram, row, ncols, stream):
    base = src_dram[row:row + 1, 0:ncols]
    ap = bass.AP(tensor=base.tensor, offset=base.offset, ap=[[1, 128], [1, ncols]])
    self.dma("pool", dst.ap[:, 0:ncols], ap, stream, [], dst.regs)


def _attn_seg(self, q0, nq, nsb, qk_fn, bias_fn, v_fn, e, dst_ap, dst_regs):
    bo = self.bankA()
    bd = self.bankA()
    ones = self.cT["ones"]
    for sb in range(nsb):
        qs = max(q0, sb * 128)
        n = q0 + nq - qs
        c0 = qs - q0
        bs = self.bank()
        mms = qk_fn(sb, qs, n) + bias_fn(sb, qs, n)
        for i, (o0, on, lt, rh, rd) in enumerate(mms):
            self.mm(self.PS[:, bs, o0:o0 + on], lt, rh, i == 0, i == len(mms) - 1, rd, [self.PSR[bs]])
        p = self.Pb[self.pi % 2]
        self.pi += 1
        self.act(p.ap[:, 0:n], self.PS[:, bs, 0:n], AF.Exp, [self.PSR[bs]], p.regs)
        vl, vr = v_fn(sb)
        self.mm(self.PS[:, bo, c0:c0 + n], vl, p.ap[:, 0:n], sb == 0, sb == nsb - 1, vr + p.regs, [self.PSR[bo]])
        self.mm(self.PS[:, bd, c0:c0 + n], ones, p.ap[:, 0:n], sb == 0, sb == nsb - 1, p.regs + [self.creg],
                [self.PSR[bd]])
    r = slice(64 * e, 64 * e + 64)
    rc = self.rcb
    self.recip(rc.ap[r, 0:nq], self.PS[r, bd, 0:nq], [self.PSR[bd]], rc.regs)
    self.tt("dve", dst_ap, self.PS[r, bo, 0:nq], rc.ap[r, 0:nq], ALU.mult, [self.PSR[bo]] + rc.regs, dst_regs)


def _attn_bufs(self):
    self.Pb = [self.lalloc((512,), BF16) for _ in range(2)]
    self.pi = 0
    self.rcb = self.lalloc((512,), F32)


def _mix_B(self, l):
    w = self.w_in[l]
    QO, QOr = self.QO[1], self.QOr[1]
    self.lreset()
    KB = self.lalloc((2, T), BF16)
    VB = self.lalloc((16, 256), BF16)
    maskT = self.lalloc((T,), BF16)
    SEL = self.lalloc((4096,), BF16)
    self.dma("pool", SEL.ap[0:32, :], self.din["c_sel"], "misc0", [], SEL.regs)
    bmk = self.lalloc((256,), F32)
    self.dma("sp", bmk.ap, self.din["c_bmk"], "misc1", [], bmk.regs)
    small = self.lalloc((256,), F32)
    kms = self.lalloc((2, 8), BF16)
    mb = self.lalloc((32,), BF16)
    mark = self.loc_cur
    self.hn_alloc()
    gq = VEC_QK + l * 16
    self.proj_fm(w, [(O_BQ, 256)], lambda ti, c, bk, m: self.head_norm(
        bk, 128, gq + 2, VEC_EPS + 1, 1.0, QO[:, ti, c * 512:(c + 1) * 512], [QOr[ti][c]]))
    self.proj_fm(w, [(O_BK, 256)], lambda ti, c, bk, m: self.head_norm(
        bk, 128, gq + 3, VEC_EPS, 1.0 / 64, KB.ap[:, ti, c * 512:(c + 1) * 512], KB.regs))
    self.proj_tm(w, [(O_BV, 256)], lambda tt, bk, o: self.copy(
        "act", VB.ap[:, tt, :], self.PS[:, bk, 0:256], [self.PSR[bk]], VB.regs))
    import os
    CUT = int(os.environ.get("BCUT", "9"))
    if CUT <= 1:
        return
    for j in range(2):
        for n in range(8):
            o = small.ap[:, j * 8 + n:j * 8 + n + 1]
            i = KB.ap[:, j, n * 256:(n + 1) * 256]
            self.S.op("dve", lambda h, o=o, i=i: h.reduce_sum(out=o, in_=i, axis=mybir.AxisListType.X),
                      KB.regs, small.regs)
    self.copy("dve", kms.ap, small.ap[:, 0:16].rearrange("p (a b) -> p a b", a=2), small.regs, kms.regs)
    gm = small.ap[:, 16:48]
    mx = small.ap[:, 48:80]
    C2 = int(os.environ.get("BCUT2", "9"))
    if C2 <= 0:
        return
    for qt in range(16):
        own = qt // 2
        bk = self.bank()
        for h in range(4):
            j, e = h // 2, h % 2
            self.mm(self.PS[:, bk, h * 8:(h + 1) * 8], QO[64 * e:64 * e + 64, j, qt * 128:(qt + 1) * 128],
                    kms.ap[64 * e:64 * e + 64, j, :], True, True, [QOr[j][qt // 4]] + kms.regs, [self.PSR[bk]])
        self.tt("dve", gm, self.PS[:, bk, 0:32], bmk.ap[:, own * 32:(own + 1) * 32], ALU.add,
                [self.PSR[bk]] + bmk.regs, small.regs)
        if C2 <= 1:
            continue
        for h in range(4):
            o = mx[:, h * 8:(h + 1) * 8]
            i = gm[:, h * 8:(h + 1) * 8]
            self.S.op("dve", lambda hh, o=o, i=i: hh.max(out=o, in_=i), small.regs, small.regs)
        if C2 <= 2:
            continue
        for h in range(4):
            self.ts("dve", mb.ap[:, h * 8:(h + 1) * 8], gm[:, h * 8:(h + 1) * 8], mx[:, h * 8 + 2:h * 8 + 3],
                    ALU.is_lt, small.regs, mb.regs, s2=NEG, op1=ALU.mult)
        if C2 <= 3:
            continue
        mv = mb.ap.rearrange("p (a b) -> p a b", a=4)[:, :, own:8]
        self.S.op("dve", lambda hh, mv=mv: hh.memset(mv, 0.0), [], mb.regs)
        if C2 <= 4:
            continue
        bt = self.bank()
        po = self.PS[0:32, bt, 0:128]
        self.mm(po, mb.ap, self.cT["ident"], True, True, mb.regs + [self.creg], [self.PSR[bt]])
        if C2 <= 5:
            continue
        self.copy("act", maskT.ap[0:32, qt * 128:(qt + 1) * 128], po, [self.PSR[bt]], maskT.regs)
    if CUT <= 2:
        return
    self.loc_cur = mark
    self.attn_bufs()
    strips = [self.lalloc((T,), BF16) for _ in range(2)]
    anti = self.cT["anti"]
    for h in range(4):
        if CUT <= 3 and h >= 1:
            break
        j, e = h // 2, h % 2
        st = strips[h % 2]
        self.load_strip(st, self.din["gext"], 4 + h, T, "strip%d" % (h % 2))
        rs_ = slice(64 * e, 64 * e + 64)
        for c in range(NCH):
            def qk_fn(sb, qs, n):
                return [(0, n, KB.ap[rs_, j, sb * 128:(sb + 1) * 128], QO[rs_, j, qs:qs + n],
                         KB.regs + [QOr[j][c]])]

            def bias_fn(sb, qs, n):
                off = qs - sb * 128
                nb = sb // 2
                return [(0, n, anti, st.ap[:, off:off + n], st.regs + [self.creg]),
                        (0, n, SEL.ap[0:32, (h * 8 + nb) * 128:(h * 8 + nb + 1) * 128], maskT.ap[0:32, qs:qs + n],
                         SEL.regs + maskT.regs)]

            def v_fn(sb):
                return VB.ap[:, sb, j * 128:(j + 1) * 128], VB.regs
            self.attn_seg(c * 512, 512, 4 * (c + 1), qk_fn, bias_fn, v_fn, e,
                          QO[rs_, j, c * 512:(c + 1) * 512], [QOr[j][c]])


for _n, _f in list(globals().items()):
    if _n.startswith("_") and callable(_f) and _n[1:] in (
            "hn_alloc", "vcol", "proj_fm", "proj_tm", "head_norm", "load_strip", "attn_seg", "attn_bufs", "mix_B"):
        setattr(Builder, _n[1:], _f)

def _merge(self, l):
    w = self.w_in[l]
    wbr = self.w_branch[l]
    wo = self.w_out[l]
    self.lreset()
    mT = self.lalloc((8, T), BF16)
    mp = lambda d, c: mT.regs[d * 4 + c:d * 4 + c + 1]
    gs = [self.lalloc((512,), F32) for _ in range(2)]
    acc = [self.lalloc((512,), F32) for _ in range(2)]
    tmp = [self.lalloc((512,), F32) for _ in range(2)]
    gi = 0
    for dp in range(4):
        d0 = dp * 256
        wg = []
        for half in range(2):
            wg.append(self.wload([(w[:, O_G + (2 * half) * 1024 + d0:O_G + (2 * half) * 1024 + d0 + 256], 0, 256),
                                  (w[:, O_G + (2 * half + 1) * 1024 + d0:O_G + (2 * half + 1) * 1024 + d0 + 256], 256, 256)], 8))
        wb, wbr_r = self.wload([(wbr[n, :, d0:d0 + 256], n * 256, 256) for n in range(4)], 2)
        for c in range(NCH):
            cs = slice(c * 512, (c + 1) * 512)
            for dt in range(2):
                d = dp * 2 + dt
                a = acc[(c * 2 + dt) % 2]
                for n in range(4):
                    wv, wr = wg[n // 2]
                    bg = self.bank()
                    for k in range(8):
                        self.mm(self.PS[:, bg, :], wv[:, k, (n % 2) * 256 + dt * 128:(n % 2) * 256 + (dt + 1) * 128],
                                self.hT[:, k, cs], k == 0, k == 7, wr + [self.hTr[k][c]], [self.PSR[bg]])
                    g = gs[gi % 2]
                    t = tmp[gi % 2]
                    gi += 1
                    self.act(g.ap, self.PS[:, bg, :], AF.Sigmoid, [self.PSR[bg]], g.regs,
                             bias=self.vcol(VEC_BG + (l * 4 + n) * 8 + d))
                    bm = self.bankA()
                    for jj in range(2):
                        self.mm(self.PS[:, bm, :], wb[:, jj, n * 256 + dt * 128:n * 256 + (dt + 1) * 128],
                                self.QO[n][:, jj, cs], jj == 0, jj == 1, wbr_r + [self.QOr[n][jj][c]], [self.PSR[bm]])
                    if n == 0:
                        self.tt("dve", a.ap, g.ap, self.PS[:, bm, :], ALU.mult, g.regs + [self.PSR[bm]], a.regs)
                    else:
                        self.tt("dve", t.ap, g.ap, self.PS[:, bm, :], ALU.mult, g.regs + [self.PSR[bm]], t.regs)
                        if n < 3:
                            self.tt("dve", a.ap, a.ap, t.ap, ALU.add, a.regs + t.regs, a.regs)
                        else:
                            self.tt("dve", mT.ap[:, d, cs], a.ap, t.ap, ALU.add, a.regs + t.regs, mp(d, c))
    for dq in range(2):
        wv, wr = self.wload([(wo[:, dq * 512:(dq + 1) * 512], 0, 512)], 8)
        for c in range(NCH):
            cs = slice(c * 512, (c + 1) * 512)
            for dt in range(4):
                d = dq * 4 + dt
                bk = self.bank()
                for k in range(8):
                    self.mm(self.PS[:, bk, :], wv[:, k, dt * 128:(dt + 1) * 128], mT.ap[:, k, cs], k == 0, k == 7,
                            wr + mp(k, c), [self.PSR[bk]])
                self.tt("dve", self.xT[:, d, cs], self.PS[:, bk, :], self.xT[:, d, cs], ALU.add,
                        [self.PSR[bk], self.xTr[d][c]], [self.xTr[d][c]])


def _mixer(self, l):
    self.lreset()
    self.rmsnorm_x(VEC_NG + (l * 3 + 1) * 8)
    en = self.dbg if self.dbg else "ABCD"
    if "A" in en:
        self.mix_A(l)
    if "B" in en:
        self.mix_B(l)
    if "C" in en:
        self.mix_C(l)
    if "D" in en:
        self.mix_D(l)
    if self.dbg:
        for n in range(4):
            if "ABCD"[n] in en:
                rr = [r for pr in self.QOr[n] for r in pr]
                self.dma("sp", self.dbg_out[n], self.QO[n], "out0", rr, [])
        return
    self.merge(l)


Builder.merge = _merge
Builder.mixer = _mixer


def _mix_C(self, l):
    w = self.w_in[l]
    QO, QOr = self.QO[2], self.QOr[2]
    self.lreset()
    QR = self.lalloc((2, T), BF16)
    KC = self.lalloc((2, T), BF16)
    KR = self.lalloc((2, T), BF16)
    VC = self.lalloc((16, 256), BF16)
    Em = self.lalloc((128,), BF16)
    Fm = self.lalloc((64,), BF16)
    Gm = self.lalloc((64,), BF16)
    ROT = self.lalloc((64,), F32)
    stc = self.lalloc((512,), BF16)
    self.dma("pool", Em.ap[0:64, :], self.din["c_E"], "cE", [], Em.regs)
    self.dma("pool", Fm.ap, self.din["c_F"], "cF", [], Fm.regs)
    self.dma("pool", Gm.ap[0:64, :], self.din["c_G"], "cG", [], Gm.regs)
    self.dma("sp", ROT.ap[0:64, :], self.din["c_rot"], "cROT", [], ROT.regs)
    self.load_strip(stc, self.din["gext"], 8, 512, "strip0")
    mark = self.loc_cur
    cqg = self.lalloc((3, 512), BF16)
    rl = self.lalloc((512,), F32)
    u_n = self.lalloc((512,), F32)
    u_r = self.lalloc((512,), F32)
    u_k = self.lalloc((512,), F32)
    sq_n = self.lalloc((512,), BF16)
    sq_r = self.lalloc((512,), BF16)
    rs_n = self.lalloc((512,), F32)
    rs_r = self.lalloc((512,), F32)
    cos = self.lalloc((512,), F32)
    sin = self.lalloc((512,), F32)
    t1 = self.lalloc((512,), F32)
    t2 = self.lalloc((512,), F32)
    rtm = self.lalloc((8,), F32)
    ones = self.cT["ones"]
    bd64 = self.cT["bd64"]
    gv = VEC_QK + l * 16
    H = slice(0, 64)
    st = {}

    def headnorm_rope(src_r, gain_n, gain_r, eps_col, ss_scale, dst_n, dst_n_regs, dst_r, dst_r_regs, c):
        cs = slice(c * 512, (c + 1) * 512)
        self.act(sq_n.ap, u_n.ap, AF.Square, u_n.regs, sq_n.regs)
        self.act(sq_r.ap[H, :], src_r.ap[H, :], AF.Square, src_r.regs, sq_r.regs)
        bsn = self.bankA()
        self.mm(self.PS[:, bsn, :], bd64, sq_n.ap, True, False, sq_n.regs + [self.creg], [self.PSR[bsn]])
        self.mm(self.PS[:, bsn, :], Em.ap[H, :], sq_r.ap[H, :], False, True, sq_r.regs + Em.regs, [self.PSR[bsn]])
        bsr = self.bankA()
        self.mm(self.PS[H, bsr, :], Fm.ap[:, 0:64], sq_n.ap, True, False, sq_n.regs + Fm.regs, [self.PSR[bsr]])
        self.mm(self.PS[H, bsr, :], Gm.ap[H, 0:64], sq_r.ap[H, :], False, True, sq_r.regs + Gm.regs, [self.PSR[bsr]])
        self.act(rs_n.ap, self.PS[:, bsn, :], AF.Sqrt, [self.PSR[bsn]], rs_n.regs, scale=ss_scale, bias=self.vcol(eps_col))
        self.recip(rs_n.ap, rs_n.ap, rs_n.regs, rs_n.regs)
        self.act(rs_r.ap[H, :], self.PS[H, bsr, :], AF.Sqrt, [self.PSR[bsr]], rs_r.regs, scale=ss_scale,
                 bias=self.vecs[H, eps_col:eps_col + 1])
        self.recip(rs_r.ap[H, :], rs_r.ap[H, :], rs_r.regs, rs_r.regs)
        self.stt(dst_n, u_n.ap, self.vcol(gain_n), rs_n.ap, ALU.mult, ALU.mult, u_n.regs + rs_n.regs + [self.creg], dst_n_regs)
        self.stt(t1.ap[H, :], src_r.ap[H, :], self.vecs[H, gain_r:gain_r + 1], rs_r.ap[H, :], ALU.mult, ALU.mult,
                 src_r.regs + rs_r.regs + [self.creg], t1.regs)
        import os
        if int(os.environ.get("CQ", "9")) <= 3:
            return
        bp = self.bank()
        self.mm(self.PS[H, bp, :], ROT.ap[H, 0:64], t1.ap[H, :], True, True, ROT.regs + t1.regs, [self.PSR[bp]])
        self.tt("dve", t2.ap[H, :], self.PS[H, bp, :], sin.ap[H, :], ALU.mult, [self.PSR[bp]] + sin.regs, t2.regs)
        self.tt("dve", t1.ap[H, :], t1.ap[H, :], cos.ap[H, :], ALU.mult, t1.regs + cos.regs, t1.regs)
        self.tt("dve", dst_r, t1.ap[H, :], t2.ap[H, :], ALU.add, t1.regs + t2.regs, dst_r_regs)

    def load_cs(c):
        self.dma("sp", cos.ap[H, :], self.din["c_cos"][:, c * 512:(c + 1) * 512], "ccos", [], cos.regs)
        self.dma("sp", sin.ap[H, :], self.din["c_sin"][:, c * 512:(c + 1) * 512], "csin", [], sin.regs)

    wuq = self.w_uq[l]
    pcs = []
    for h in range(4):
        pcs.append((wuq[:, h * 96:h * 96 + 64], h * 64, 64))
        pcs.append((wuq[:, h * 96 + 64:h * 96 + 96], 256 + h * 32, 32))
    import os
    CD = int(os.environ.get("CD", "9"))
    if CD <= 0:
        return
    wq, wq_r = self.wload(pcs, 3)
    if CD <= 1:
        load_cs(0)
        return

    def cb_q(ti, c, bk, m):
        cs = slice(c * 512, (c + 1) * 512)
        if ti == 0:
            st["ss"] = self.bankA()
            load_cs(c)
        bss = st["ss"]
        self.act(sq_n.ap, self.PS[:, bk, :], AF.Square, [self.PSR[bk]], sq_n.regs)
        self.mm(self.PS[:, bss, :], ones, sq_n.ap, ti == 0, ti == 2, sq_n.regs + [self.creg], [self.PSR[bss]])
        self.ts("dve", cqg.ap[:, ti, :], self.PS[:, bk, :], self.vcol(gv + 10 + ti), ALU.mult,
                [self.PSR[bk], self.creg], cqg.regs)
        if ti < 2:
            return
        import os
        CQ = int(os.environ.get("CQ", "9"))
        if CQ <= 1:
            return
        self.act(rl.ap, self.PS[:, bss, :], AF.Sqrt, [self.PSR[bss]], rl.regs, scale=1.0 / 384, bias=self.vcol(VEC_EPS))
        self.recip(rl.ap, rl.ap, rl.regs, rl.regs)
        for j in range(2):
            bn = self.bank()
            for k in range(3):
                self.mm(self.PS[:, bn, :], wq[:, k, j * 128:(j + 1) * 128], cqg.ap[:, k, :], k == 0, k == 2,
                        wq_r + cqg.regs, [self.PSR[bn]])
            self.tt("dve", u_n.ap, self.PS[:, bn, :], rl.ap, ALU.mult, [self.PSR[bn]] + rl.regs, u_n.regs)
            br = self.bank()
            for k in range(3):
                self.mm(self.PS[H, br, :], wq[:, k, 256 + j * 64:256 + (j + 1) * 64], cqg.ap[:, k, :], k == 0, k == 2,
                        wq_r + cqg.regs, [self.PSR[br]])
            self.tt("dve", u_r.ap[H, :], self.PS[H, br, :], rl.ap[H, :], ALU.mult, [self.PSR[br]] + rl.regs, u_r.regs)
            if CQ <= 2:
                continue
            headnorm_rope(u_r, gv + 6, gv + 8, VEC_EPS + 2, 1.0, QO[:, j, cs], [QOr[j][c]],
                          QR.ap[H, j, cs], QR.regs, c)

    self.proj_fm(w, [(O_CQ, 384)], cb_q)
    import os
    CC = int(os.environ.get("CCUT", "9"))
    if CC <= 1:
        return

    wukv = self.w_ukv[l]
    pcs = []
    for h in range(4):
        pcs.append((wukv[:, h * 128:h * 128 + 64], h * 64, 64))
        pcs.append((wukv[:, h * 128 + 64:h * 128 + 128], 256 + h * 64, 64))
    wk, wk_r = self.wload(pcs, 2)
    ckvg = cqg

    def cb_k(ti, c, bk, m):
        cs = slice(c * 512, (c + 1) * 512)
        if ti == 0:
            st["ss"] = self.bankA()
            st["tm"] = self.bankA()
            load_cs(c)
        bss, btm = st["ss"], st["tm"]
        if ti < 2:
            self.act(sq_n.ap, self.PS[:, bk, :], AF.Square, [self.PSR[bk]], sq_n.regs)
            self.mm(self.PS[:, bss, :], ones, sq_n.ap, ti == 0, ti == 1, sq_n.regs + [self.creg], [self.PSR[bss]])
            for a in range(4):
                self.mm(self.PS[:, btm, a:a + 1], sq_n.ap[:, a * 128:(a + 1) * 128], ones[:, 0:1],
                        ti == 0 and a == 0, ti == 1 and a == 3, sq_n.regs + [self.creg], [self.PSR[btm]])
            self.ts("dve", ckvg.ap[:, ti, :], self.PS[:, bk, :], self.vcol(gv + 13 + ti), ALU.mult,
                    [self.PSR[bk], self.creg], ckvg.regs)
            return
        self.copy("act", u_k.ap[H, :], self.PS[H, bk, :], [self.PSR[bk]], u_k.regs)
        self.act(rl.ap, self.PS[:, bss, :], AF.Sqrt, [self.PSR[bss]], rl.regs, scale=1.0 / 256, bias=self.vcol(VEC_EPS))
        self.recip(rl.ap, rl.ap, rl.regs, rl.regs)
        self.act(rtm.ap[:, 0:4], self.PS[:, btm, 0:4], AF.Sqrt, [self.PSR[btm]], rtm.regs, scale=1.0 / 256,
                 bias=self.vcol(VEC_EPS))
        self.recip(rtm.ap[:, 0:4], rtm.ap[:, 0:4], rtm.regs, rtm.regs)
        for j in range(2):
            bn = self.bank()
            for k in range(2):
                self.mm(self.PS[:, bn, :], wk[:, k, j * 128:(j + 1) * 128], ckvg.ap[:, k, :], k == 0, k == 1,
                        wk_r + ckvg.regs, [self.PSR[bn]])
            self.tt("dve", u_n.ap, self.PS[:, bn, :], rl.ap, ALU.mult, [self.PSR[bn]] + rl.regs, u_n.regs)
            headnorm_rope(u_k, gv + 7, gv + 9, VEC_EPS, 1.0 / 96, KC.ap[:, j, cs], KC.regs,
                          KR.ap[H, j, cs], KR.regs, c)
        for a in range(4):
            bv = self.bank()
            for k in range(2):
                self.mm(self.PS[:, bv, 0:256], ckvg.ap[:, k, a * 128:(a + 1) * 128], wk[:, k, 256:512], k == 0, k == 1,
                        wk_r + ckvg.regs, [self.PSR[bv]])
            self.ts("dve", VC.ap[:, c * 4 + a, :], self.PS[:, bv, 0:256], rtm.ap[:, a:a + 1], ALU.mult,
                    [self.PSR[bv]] + rtm.regs, VC.regs)

    self.proj_fm(w, [(O_CKV, 256), (O_CKR, 32), (O_CKR, 32)], cb_k, tiles=[(0, 128), (128, 128), (256, 64)])

    if CC <= 2:
        return
    self.loc_cur = mark
    self.attn_bufs()
    anti = self.cT["anti"]
    for h in range(4):
        j, e = h // 2, h % 2
        rs_ = slice(64 * e, 64 * e + 64)
        rr = slice(32 * e, 32 * e + 32)
        for c in range(NCH):
            def qk_fn(sb, qs, n):
                return [(0, n, KC.ap[rs_, j, sb * 128:(sb + 1) * 128], QO[rs_, j, qs:qs + n], KC.regs + [QOr[j][c]]),
                        (0, n, KR.ap[rr, j, sb * 128:(sb + 1) * 128], QR.ap[rr, j, qs:qs + n], KR.regs + QR.regs)]

            def bias_fn(sb, qs, n):
                if qs != sb * 128:
                    return []
                return [(0, n, anti, stc.ap[:, 0:n], stc.regs + [self.creg])]

            def v_fn(sb):
                return VC.ap[:, sb, j * 128:(j + 1) * 128], VC.regs
            self.attn_seg(c * 512, 512, 4 * (c + 1), qk_fn, bias_fn, v_fn, e,
                          QO[rs_, j, c * 512:(c + 1) * 512], [QOr[j][c]])


Builder.mix_C = _mix_C


def _mix_D(self, l):
    w = self.w_in[l]
    QO, QOr = self.QO[3], self.QOr[3]
    self.lreset()
    nacc = self.lalloc((T,), F32)
    dacc = self.lalloc((T,), F32)
    sd = [self.lalloc((256,), BF16) for _ in range(6)]
    QD = self.lalloc((T,), BF16)
    KD = self.lalloc((T,), BF16)
    VD = self.lalloc((16, 128), BF16)
    self.hn_alloc()
    self.attn_bufs()
    anti = self.cT["anti"]
    ones = self.cT["ones"]
    gv = VEC_QK + l * 16
    allh = lambda k: [self.hTr[k][c] for c in range(NCH)]
    for j in range(2):
        for g, dil in enumerate((1, 4, 16)):
            nbk = 16 // dil
            for e in range(2):
                self.load_strip(sd[g * 2 + e], self.din["gd"], g * 4 + 2 * j + e, 256, "sd%d" % (g * 2 + e))

            def cb(ti, c, bk, m):
                cs = slice(c * 512, (c + 1) * 512)
                if ti == 0:
                    self.head_norm(bk, 128, gv + 4, VEC_EPS + 1, 1.0, QD.ap[:, cs], QD.regs)
                else:
                    self.head_norm(bk, 128, gv + 5, VEC_EPS, 1.0 / 64, KD.ap[:, cs], KD.regs)
            self.proj_fm(w, [(O_DQ + g * 256 + j * 128, 128), (O_DK + g * 256 + j * 128, 128)], cb)

            def tok_ap(k, blk):
                r, n = blk // nbk, blk % nbk
                base = r + dil * n * 128
                return self.hT[:, k, base:base + 127 * dil + 1:dil], allh(k)
            self.proj_tm(w, [(O_DV + g * 256 + j * 128, 128)],
                         lambda blk, bk, o: self.copy("act", VD.ap[:, blk, :], self.PS[:, bk, 0:128], [self.PSR[bk]], VD.regs),
                         tok_ap=tok_ap)
            for e in range(2):
                rows = slice(64 * e, 64 * e + 64)
                st = sd[g * 2 + e]
                for r in range(dil):
                    for n in range(nbk):
                        bo = self.bankA()
                        bd = self.bankA()
                        qb = r + dil * n * 128
                        qsl = slice(qb, qb + 127 * dil + 1, dil)
                        kbs = ([n - 1] if n > 0 else []) + [n]
                        for i, kn in enumerate(kbs):
                            kb = r + dil * kn * 128
                            off = 128 if kn != n else 0
                            bs = self.bank()
                            self.mm(self.PS[:, bs, 0:128], KD.ap[rows, kb:kb + 127 * dil + 1:dil], QD.ap[rows, qsl], True, False,
                                    KD.regs + QD.regs, [self.PSR[bs]])
                            self.mm(self.PS[:, bs, 0:128], anti, st.ap[:, off:off + 128], False, True,
                                    st.regs + [self.creg], [self.PSR[bs]])
                            p = self.Pb[self.pi % 2]
                            self.pi += 1
                            self.act(p.ap[:, 0:128], self.PS[:, bs, 0:128], AF.Exp, [self.PSR[bs]], p.regs)
                            last = i == len(kbs) - 1
                            self.mm(self.PS[:, bo, 0:128], VD.ap[:, r * nbk + kn, :], p.ap[:, 0:128], i == 0, last,
                                    VD.regs + p.regs, [self.PSR[bo]])
                            self.mm(self.PS[:, bd, 0:128], ones, p.ap[:, 0:128], i == 0, last, p.regs + [self.creg],
                                    [self.PSR[bd]])
                        if g == 0:
                            self.copy("act", nacc.ap[rows, qsl], self.PS[rows, bo, 0:128], [self.PSR[bo]], nacc.regs)
                            self.copy("dve", dacc.ap[rows, qsl], self.PS[rows, bd, 0:128], [self.PSR[bd]], dacc.regs)
                        else:
                            self.tt("dve", nacc.ap[rows, qsl], self.PS[rows, bo, 0:128], nacc.ap[rows, qsl], ALU.add,
                                    [self.PSR[bo]] + nacc.regs, nacc.regs)
                            self.tt("dve", dacc.ap[rows, qsl], self.PS[rows, bd, 0:128], dacc.ap[rows, qsl], ALU.add,
                                    [self.PSR[bd]] + dacc.regs, dacc.regs)
        rc = self.rcb
        for c in range(NCH):
            cs = slice(c * 512, (c + 1) * 512)
            self.recip(rc.ap, dacc.ap[:, cs], dacc.regs, rc.regs)
            self.tt("dve", QO[:, j, cs], nacc.ap[:, cs], rc.ap, ALU.mult, nacc.regs + rc.regs, [QOr[j][c]])


NIT = 24
S0 = 64.0


def _mix_A(self, l):
    w = self.w_in[l]
    QO, QOr = self.QO[0], self.QOr[0]
    self.lreset()
    KA = self.lalloc((T,), BF16)
    VA = self.lalloc((16, 128), BF16)
    IQ = self.lalloc((T,), BF16)
    IQ3 = self.lalloc((T,), BF16)
    IK3 = self.lalloc((T,), BF16)
    wab = self.lalloc((16, 4), F32)
    wsg = self.lalloc((16, 4), F32)
    cm = self.lalloc((128,), F32)
    self.dma("sp", cm.ap, self.din["c_cm"], "ccm", [], cm.regs)
    mark = self.loc_cur
    self.hn_alloc()
    gv = VEC_QK + l * 16
    self.proj_fm(w, [(O_AQ, 256)], lambda ti, c, bk, m: self.head_norm(
        bk, 128, gv + 0, VEC_EPS + 1, 1.0, QO[:, ti, c * 512:(c + 1) * 512], [QOr[ti][c]]))
    self.proj_fm(w, [(O_AK, 64), (O_AK, 64)], lambda ti, c, bk, m: self.head_norm(
        bk, 128, gv + 1, VEC_EPS, 1.0 / 64, KA.ap[:, c * 512:(c + 1) * 512], KA.regs))

    def cb_i(ti, c, bk, m):
        cs = slice(c * 512, (c + 1) * 512)
        dst = (IQ, IQ3, IK3)[ti]
        self.copy("act" if ti % 2 else "dve", dst.ap[0:m, cs], self.PS[0:m, bk, :], [self.PSR[bk]], dst.regs)
    self.proj_fm(w, [(O_IQ, 128), (O_IK, 32), (O_IK, 32), (O_IK, 32)], cb_i, tiles=[(0, 96), (96, 32), (128, 96)])

    def cb_v(tt, bk, o):
        pr = [self.PSR[bk]]
        self.copy("act", VA.ap[:, tt, 0:64], self.PS[:, bk, 0:64], pr, VA.regs)
        self.copy("dve", VA.ap[:, tt, 64:128], self.PS[:, bk, 0:64], pr, VA.regs)
        self.act(wab.ap[:, tt, :], self.PS[:, bk, 64:68], AF.Abs, pr, wab.regs)
        self.act(wsg.ap[:, tt, :], self.PS[:, bk, 64:68], AF.Sign, pr, wsg.regs)
    self.proj_tm(w, [(O_AV, 64), (O_IW, 4)], cb_v)
    self.loc_cur = mark
    self.attn_bufs()
    score = self.lalloc((T,), F32)
    rt = [self.lalloc((512,), F32) for _ in range(2)]
    mbt = [self.lalloc((T,), BF16) for _ in range(2)]
    strips = [self.lalloc((T,), BF16) for _ in range(2)]
    small = self.lalloc((16,), F32)
    thr = small.ap[:, 0:1]
    cnt = small.ap[:, 1:2]
    g2 = small.ap[:, 2:3]
    anti = self.cT["anti"]
    ident = self.cT["ident"]
    ri = 0
    si = 0
    for qp in range(8):
        for i in range(2):
            qt = 2 * qp + i
            if qt < 2:
                continue
            nk = (qt + 1) * 128
            qsl = slice(qt * 128, (qt + 1) * 128)
            for s0 in range(0, nk, 512):
                sn = min(512, nk - s0)
                for h in range(4):
                    bz = self.bank()
                    if h < 3:
                        lt, rh = IQ.ap[32 * h:32 * h + 32, qsl], IK3.ap[32 * h:32 * h + 32, s0:s0 + sn]
                        rd = IQ.regs + IK3.regs
                    else:
                        lt, rh = IQ3.ap[0:32, qsl], IK3.ap[0:32, s0:s0 + sn]
                        rd = IQ3.regs + IK3.regs
                    self.mm(self.PS[:, bz, 0:sn], lt, rh, True, True, rd, [self.PSR[bz]])
                    r = rt[ri % 2]
                    ri += 1
                    self.act(r.ap[:, 0:sn], self.PS[:, bz, 0:sn], AF.Relu, [self.PSR[bz]] + wab.regs, r.regs,
                             scale=wab.ap[:, qt, h:h + 1])
                    if h == 0:
                        self.ts("dve", score.ap[:, s0:s0 + sn], r.ap[:, 0:sn], wsg.ap[:, qt, 0:1], ALU.mult,
                                r.regs + wsg.regs, score.regs)
                    else:
                        self.stt(score.ap[:, s0:s0 + sn], r.ap[:, 0:sn], wsg.ap[:, qt, h:h + 1], score.ap[:, s0:s0 + sn],
                                 ALU.mult, ALU.add, r.regs + wsg.regs + score.regs, score.regs)
            self.tt("dve", score.ap[:, qsl], score.ap[:, qsl], cm.ap, ALU.add, score.regs + cm.regs, score.regs)
            self.S.op("dve", lambda hh: hh.memset(thr, 0.0), [], small.regs)
            mb = mbt[i]
            for it in range(NIT):
                step = S0 / (2 ** it)
                self.ts("dve", mb.ap[:, 0:nk], score.ap[:, 0:nk], thr, ALU.is_gt, score.regs + small.regs,
                        mb.regs + small.regs, op1=ALU.add, accum=cnt)
                self.ts("dve", g2, cnt, 255.5, ALU.is_ge, small.regs, small.regs, s2=2.0 * step, op1=ALU.mult)
                self.stt(thr, g2, -step, thr, ALU.add, ALU.add, small.regs, small.regs)
            self.ts("dve", thr, thr, -S0 / (2 ** (NIT - 1)), ALU.add, small.regs, small.regs)
            self.ts("dve", mb.ap[:, 0:nk], score.ap[:, 0:nk], thr, ALU.is_le, score.regs + small.regs, mb.regs,
                    s2=NEG, op1=ALU.mult)
        q0 = qp * 256
        for h in range(4):
            j, e = h // 2, h % 2
            rs_ = slice(64 * e, 64 * e + 64)
            st = strips[si % 2]
            self.load_strip(st, self.din["gext"], h, q0 + 256, "strip%d" % (si % 2))
            si += 1

            def qk_fn(sb, qs, n):
                return [(0, n, KA.ap[rs_, sb * 128:(sb + 1) * 128], QO[rs_, j, qs:qs + n], KA.regs + [QOr[j][qp // 2]])]

            def bias_fn(sb, qs, n):
                off = qs - sb * 128
                out = [(0, n, anti, st.ap[:, off:off + n], st.regs + [self.creg])]
                if qp > 0:
                    for i in range(2):
                        t0 = q0 + i * 128
                        if t0 >= qs:
                            out.append((t0 - qs, 128, mbt[i].ap[:, sb * 128:(sb + 1) * 128], ident,
                                        mbt[i].regs + [self.creg]))
                return out

            def v_fn(sb):
                return VA.ap[:, sb, :], VA.regs
            self.attn_seg(q0, 256, 2 * (qp + 1), qk_fn, bias_fn, v_fn, e, QO[rs_, j, q0:q0 + 256], [QOr[j][qp // 2]])


Builder.mix_D = _mix_D
Builder.mix_A = _mix_A


_CACHE = {}


def get_nc(nseq, stages, dbg=None):
    key = (nseq, stages, dbg)
    if key not in _CACHE:
        b = Builder(nseq, stages, dbg)
        _CACHE[key] = b
        b.nc_built = b.build_wrapped()
    return _CACHE[key]


def _build_wrapped(self):
    return self.build()


Builder.build_wrapped = _build_wrapped


def make_inmap(inp):
    shared = {
        "w_ffn_in": np.ascontiguousarray(np.asarray(inp["w_ffn_in"], np.float32)),
        "w_ffn_out": np.ascontiguousarray(np.asarray(inp["w_ffn_out"], np.float32)),
        "vecs": make_vecs(inp),
    }
    for k, v in host_consts().items():
        shared["c_" + k] = v
    for k, v in host_consts2().items():
        shared["c_" + k] = v
    for k in ("w_in", "w_branch", "w_out", "w_mla_uq", "w_mla_ukv"):
        shared[k] = np.ascontiguousarray(np.asarray(inp[k], np.float32))
    shared["gext"], shared["gd"] = make_gext(inp)
    return shared


def kernel(**inp):
    ncores = 8
    nseq = 2
    b = get_nc(nseq, 99)
    x = np.ascontiguousarray(np.asarray(inp["x"], np.float32))
    shared = make_inmap(inp)
    in_maps = []
    for i in range(ncores):
        m = dict(shared)
        m["x"] = x[i * nseq:(i + 1) * nseq]
        in_maps.append(m)
    res = run_bass_kernel_spmd(b.nc_built, in_maps, core_ids=list(range(ncores)))
    return np.concatenate([r["y"] for r in res.results], axis=0)
```

```python
import numpy as np
import math
from contextlib import ExitStack
import concourse.bass as bass
import concourse.mybir as mybir
from concourse.bass_utils import run_bass_kernel_spmd

F32 = mybir.dt.float32
BF16 = mybir.dt.bfloat16
AF = mybir.ActivationFunctionType
ALU = mybir.AluOpType

D = 1024
T = 2048
DFF = 2816
INW = 8388
NCH = 4
NEG = -30000.0
EPS = 1e-6

O_AQ, O_AK, O_AV, O_IQ, O_IK, O_IW = 0, 256, 320, 384, 512, 544
O_BQ, O_BK, O_BV = 548, 804, 1060
O_CQ, O_CKV, O_CKR = 1316, 1700, 1956
O_DQ, O_DK, O_DV = 1988, 2756, 3524
O_G = 4292

PAGE = 1024
ARENA_BYTES = 224000


class Reg:
    __slots__ = ("w", "r", "excl")

    def __init__(self, excl=False):
        self.w = None
        self.r = {}
        self.excl = excl


class TT:
    __slots__ = ("ap", "regs")

    def __init__(self, ap, regs):
        self.ap = ap
        self.regs = regs


ENGS = ["pe", "act", "dve", "pool", "sp"]


class Sched:
    def __init__(self):
        self.ops = {e: [] for e in ENGS}
        self.cnt = {e: 0 for e in ENGS}
        self.waited = {e: {} for e in ENGS}
        self.dma_tot = {}

    def _waits(self, eng, reads, writes, k):
        need = {}
        for r in reads:
            t = r.w
            if t is None:
                continue
            key, val = t
            if key == eng and eng == "pe":
                continue
            if need.get(key, 0) < val:
                need[key] = val
        for w in writes:
            toks = list(w.r.values())
            if w.w is not None:
                toks.append(w.w)
            for key, val in toks:
                if key == eng and eng == "pe":
                    continue
                if need.get(key, 0) < val:
                    need[key] = val
        out = []
        wd = self.waited[eng]
        for key, val in need.items():
            if wd.get(key, 0) >= val:
                continue
            wd[key] = val
            out.append((key, val))
        return out

    def op(self, eng, fn, reads=(), writes=(), mode=None):
        if any(r.excl for r in reads):
            writes = list(writes) + [r for r in reads if r.excl]
            reads = [r for r in reads if not r.excl]
        k = self.cnt[eng] + 1
        waits = self._waits(eng, reads, writes, k)
        self.cnt[eng] = k
        self.ops[eng].append((0, fn, waits, mode))
        tok = (eng, k)
        for r in reads:
            r.r[eng] = tok
        for w in writes:
            w.w = tok
            w.r = {}

    def dma(self, queue, fn, stream, reads=(), writes=()):
        waits = self._waits(queue, reads, writes, 1 << 60)
        tot = self.dma_tot.get(stream, 0) + 16
        self.dma_tot[stream] = tot
        self.ops[queue].append((1, fn, waits, stream))
        key = ("D", stream)
        tok = (key, tot)
        for r in reads:
            r.r[key] = tok
        for w in writes:
            w.w = tok
            w.r = {}

    def final_waits(self, eng, streams):
        waits = []
        for s in streams:
            if s in self.dma_tot:
                waits.append((("D", s), self.dma_tot[s]))
        self.ops[eng].append((2, None, waits, None))

    def emit(self, nc):
        with ExitStack() as es:
            sems = {}
            for e in ENGS:
                sems[e] = es.enter_context(nc.semaphore("s_" + e))
            for i, s in enumerate(self.dma_tot):
                sems[("D", s)] = es.enter_context(nc.semaphore("d%d" % i))
            block = es.enter_context(nc.Block())
            names = {"pe": "tensor", "act": "scalar", "dve": "vector", "pool": "gpsimd", "sp": "sync"}

            def mk(e):
                def body(h):
                    se = sems[e]
                    last_mode = (128, 128, 0)
                    for kind, fn, waits, stream in self.ops[e]:
                        for key, val in waits:
                            h.wait_ge(sems[key], val)
                        if kind == 0:
                            if e == "pe":
                                md = stream if stream is not None else (128, 128, 0)
                                if md != last_mode:
                                    h.drain()
                                    self.ndrain = getattr(self, "ndrain", 0) + 1
                                    last_mode = md
                            fn(h).then_inc(se, 1)
                        elif kind == 1:
                            fn(h).then_inc(sems[("D", stream)], 16)
                return body

            for e in ENGS:
                getattr(block, names[e])(mk(e))


def rel_bucket_np(dist):
    n = np.maximum(dist, 0)
    max_exact = 16
    nf = np.maximum(n, 1).astype(np.float32)
    log_b = max_exact + (np.log(nf / np.float32(max_exact)) / np.float32(math.log(2048 / max_exact))
                         * np.float32(32 - max_exact)).astype(np.int32)
    return np.where(n < max_exact, n, np.minimum(log_b, 31))


def host_consts():
    c = {}
    eye = np.eye(128, dtype=np.float32)
    c["ident"] = eye
    c["anti"] = eye[::-1].copy()
    c["ones"] = np.ones((128, 128), np.float32)
    bd = np.zeros((128, 128), np.float32)
    bd[:64, :64] = 1
    bd[64:, 64:] = 1
    c["bd64"] = bd
    return c


def host_consts2():
    c = {}
    c["sel"] = np.kron(np.eye(32, dtype=np.float32), np.ones((1, 128), np.float32))
    bm = np.zeros((8, 4, 8), np.float32)
    for own in range(8):
        bm[own, :, own:] = -1e30
    c["bmk"] = np.broadcast_to(bm.reshape(1, 256), (128, 256)).copy()
    E = np.zeros((64, 128), np.float32)
    for k in range(64):
        E[k, (k // 32) * 64:(k // 32) * 64 + 64] = 1
    c["E"] = E
    cmm = np.where(np.arange(128)[None, :] <= np.arange(128)[:, None], 0.0, -1e30).astype(np.float32)
    c["cm"] = cmm
    c["F"] = E.T.copy()
    G = np.zeros((64, 64), np.float32)
    G[:32, :32] = 1
    G[32:, 32:] = 1
    c["G"] = G
    rot = np.zeros((64, 64), np.float32)
    for b in range(2):
        for m in range(16):
            rot[b * 32 + m + 16, b * 32 + m] = -1.0
            rot[b * 32 + m, b * 32 + m + 16] = 1.0
    c["rot"] = rot
    freqs = 10000.0 ** (-np.arange(16, dtype=np.float32) / 16)
    ang = np.arange(T, dtype=np.float32)[None, :] * np.tile(freqs, 4)[:, None].astype(np.float32)
    c["cos"] = np.cos(ang).astype(np.float32)
    c["sin"] = np.sin(ang).astype(np.float32)
    return c


class Builder:
    def __init__(self, nseq=2, stages=99, dbg=None):
        self.nseq = nseq
        self.stages = stages
        self.dbg = dbg
        self.S = Sched()
        self.nc = bass.Bass("TRN2", target_bir_lowering=False, dynamic_dma_scratch_size=4096)
        self.din = {}
        self.wslot_i = 0
        self.bank_i = 0
        self.bankA_i = 0

    def dram_in(self, name, shape, dt=F32):
        t = self.nc.dram_tensor(name, list(shape), dt, kind="ExternalInput").ap()
        self.din[name] = t
        return t

    def carve(self, off, free_shape, dt):
        n = 1
        for s in free_shape:
            n *= s
        nb = n * (4 if dt == F32 else 2)
        assert off % 4 == 0 and off + nb <= ARENA_BYTES, (off, nb)
        ap = self.arena[:, off // 2:(off + nb) // 2]
        if dt == F32:
            ap = ap.bitcast(F32)
        if len(free_shape) == 2:
            ap = ap.rearrange("p (a b) -> p a b", a=free_shape[0])
        elif len(free_shape) == 3:
            ap = ap.rearrange("p (a b c) -> p a b c", a=free_shape[0], b=free_shape[1])
        return ap, nb

    def lalloc(self, free_shape, dt):
        ap, nb = self.carve(self.loc_base + self.loc_cur, free_shape, dt)
        p0 = self.loc_cur // PAGE
        self.loc_cur += (nb + PAGE - 1) // PAGE * PAGE
        p1 = self.loc_cur // PAGE
        assert self.loc_base + self.loc_cur <= ARENA_BYTES, ("local overflow", self.loc_cur)
        return TT(ap, self.loc_pages[p0:p1])

    def lreset(self):
        self.loc_cur = 0

    def bank(self):
        b = self.bank_i
        self.bank_i = (b + 1) % 4
        return b

    def bankA(self):
        b = self.bankA_i
        self.bankA_i = (b + 1) % 4
        return 4 + b

    def mm(self, out, lhsT, rhs, start, stop, reads, writes):
        ru = lambda v: 32 if v <= 32 else (64 if v <= 64 else 128)
        kk = lhsT.shape[0]
        mmm = 1
        for d in lhsT.shape[1:]:
            mmm *= d
        self.S.op("pe", lambda h: h.matmul(out, lhsT, rhs, start=start, stop=stop), reads, writes,
                  mode=(ru(kk), ru(mmm), lhsT.offset // (lhsT.tensor.shape[1] * 32) if kk < 128 else 0))

    def act(self, out, in_, func, reads, writes, scale=1.0, bias=0.0, accum=None):
        if accum is None:
            self.S.op("act", lambda h: h.activation(out=out, in_=in_, func=func, scale=scale, bias=bias),
                      reads, writes)
        else:
            self.S.op("act", lambda h: h.activation(out=out, in_=in_, func=func, scale=scale, bias=bias,
                                                      accum_out=accum), reads, writes)

    def ts(self, eng, out, in0, s1, op0, reads, writes, s2=None, op1=None, accum=None):
        def fn(h):
            kw = {}
            if op1 is not None:
                kw["op1"] = op1
            if accum is not None:
                kw["accum_out"] = accum
            return h.tensor_scalar(out=out, in0=in0, scalar1=s1, scalar2=s2, op0=op0, **kw)
        self.S.op(eng, fn, reads, writes)

    def stt(self, out, in0, scalar, in1, op0, op1, reads, writes):
        self.S.op("dve", lambda h: h.scalar_tensor_tensor(out=out, in0=in0, scalar=scalar, in1=in1,
                                                            op0=op0, op1=op1), reads, writes)

    def tt(self, eng, out, in0, in1, op, reads, writes):
        self.S.op(eng, lambda h: h.tensor_tensor(out=out, in0=in0, in1=in1, op=op), reads, writes)

    def copy(self, eng, out, in_, reads, writes):
        if eng == "act":
            self.S.op("act", lambda h: h.copy(out=out, in_=in_), reads, writes)
        else:
            self.S.op(eng, lambda h: h.tensor_copy(out=out, in_=in_), reads, writes)

    def recip(self, out, in_, reads, writes):
        self.S.op("dve", lambda h: h.reciprocal(out=out, in_=in_), reads, writes)

    def dma(self, queue, out, in_, stream, reads, writes):
        if queue == "pool":
            self.S.dma(queue, lambda h: h.dma_start(out=out, in_=in_, max_dma_last_dim=2048), stream, reads, writes)
        else:
            self.S.dma(queue, lambda h: h.dma_start(out=out, in_=in_), stream, reads, writes)

    def wload(self, pieces, nk):
        s = self.wslot_i
        self.wslot_i = (s + 1) % len(self.wslots)
        ap_full, reg = self.wslots[s]
        tot = max(o + n for _, o, n in pieces)
        assert nk * tot * 2 <= 8192, (nk, tot)
        view = ap_full[:, 0:nk * tot].rearrange("p (k c) -> p k c", k=nk)
        for src, o, n in pieces:
            srcv = src.rearrange("(k p) c -> p k c", p=128)
            self.dma("pool", view[:, :, o:o + n], srcv, ("w", s), [], [reg])
        return view, [reg]

    def build(self):
        nc = self.nc
        ns = self.nseq
        x_in = self.dram_in("x", (ns, T, D))
        wfi = self.dram_in("w_ffn_in", (2, 2, D, 2 * DFF))
        wfo = self.dram_in("w_ffn_out", (2, 2, DFF, D))
        vecs = self.dram_in("vecs", (128, NVEC))
        self.w_in = self.dram_in("w_in", (2, D, INW))
        self.w_branch = self.dram_in("w_branch", (2, 4, 256, D))
        self.w_out = self.dram_in("w_out", (2, D, D))
        self.w_uq = self.dram_in("w_mla_uq", (2, 384, 384))
        self.w_ukv = self.dram_in("w_mla_ukv", (2, 256, 512))
        self.dram_in("gext", (9, NG))
        self.dram_in("gd", (12, NGD))
        for k, v in host_consts2().items():
            self.dram_in("c_" + k, v.shape)
        if self.dbg:
            self.dbg_out = self.nc.dram_tensor("dbg", [4, 128, 2, T], BF16, kind="ExternalOutput").ap()
        cst = {k: self.dram_in("c_" + k, v.shape) for k, v in host_consts().items()}
        self.y_out = nc.dram_tensor("y", [ns, T, D], F32, kind="ExternalOutput").ap()
        with ExitStack() as es:
            arena_t = es.enter_context(nc.sbuf_tensor("arena", [128, ARENA_BYTES // 2], BF16))
            self.arena = arena_t[:, :]
            ps_t = es.enter_context(nc.psum_tensor("ps", [128, 8, 512], F32))
            self.PS = ps_t
            self.PSR = [Reg(excl=True) for _ in range(8)]
            off = 0
            self.xT, nb = self.carve(off, (8, T), F32); off += nb
            self.xTr = [[Reg() for _ in range(NCH)] for _ in range(8)]
            self.hT, nb = self.carve(off, (8, T), BF16); off += nb
            self.hTr = [[Reg() for _ in range(NCH)] for _ in range(8)]
            self.QO = []
            self.QOr = []
            for n in range(4):
                q, nb = self.carve(off, (2, T), BF16); off += nb
                self.QO.append(q)
                self.QOr.append([[Reg() for _ in range(NCH)] for _ in range(2)])
            self.wslots = []
            for s in range(3):
                w, nb = self.carve(off, (4096,), BF16); off += nb
                self.wslots.append((w, Reg()))
            self.cT = {}
            creg = Reg()
            self.creg = creg
            for k, v in host_consts().items():
                ap, nb = self.carve(off, (v.shape[1],), BF16); off += nb
                self.cT[k] = ap
                self.dma("pool", ap, cst[k], "const", [], [creg])
            ap, nb = self.carve(off, (128,), F32); off += nb
            self.identf = ap
            self.dma("sp", ap, cst["ident"], "constf", [], [creg])
            ap, nb = self.carve(off, (NVEC,), F32); off += nb
            self.vecs = ap
            self.eps_ap = ap[:, VEC_EPS:VEC_EPS + 1]
            self.dma("sp", ap, vecs, "constf", [], [creg])
            off = (off + PAGE - 1) // PAGE * PAGE
            self.loc_base = off
            npages = (ARENA_BYTES - off) // PAGE
            self.loc_pages = [Reg() for _ in range(npages)]
            self.loc_cur = 0
            print("persistent bytes", off, "local pages", npages)

            for b in range(ns):
                self.load_x(x_in, b)
                st = 0
                for l in range(2):
                    for f in range(2):
                        if f == 1:
                            if st < self.stages:
                                self.mixer(l)
                                if self.dbg:
                                    st = 1000
                            st += 1
                        if st < self.stages:
                            self.ffn(wfi[l, f], wfo[l, f], VEC_NG + (l * 3 + (0 if f == 0 else 2)) * 8)
                        st += 1
                self.store_x(b)
            self.S.final_waits("sp", ["out0", "out1"])
            self.S.emit(nc)
        return nc

    def load_x(self, x_in, b):
        self.lreset()
        stg = [self.lalloc((4, D), F32) for _ in range(2)]
        for c in range(NCH):
            s = stg[c % 2]
            src = x_in[b, c * 512:(c + 1) * 512, :].rearrange("(a p) d -> p a d", p=128)
            self.dma("sp", s.ap, src, "xin%d" % (c % 2), [], s.regs)
            for j in range(8):
                bk = self.bank()
                for a in range(4):
                    o = self.PS[:, bk, a * 128:(a + 1) * 128]
                    i = s.ap[:, a, j * 128:(j + 1) * 128]
                    self.S.op("pe", lambda h, o=o, i=i: h.transpose(o, i, self.identf),
                              s.regs + [self.creg], [self.PSR[bk]])
                dst = self.xT[:, j, c * 512:(c + 1) * 512]
                self.copy("act" if j % 2 else "dve", dst, self.PS[:, bk, :], [self.PSR[bk]], [self.xTr[j][c]])

    def store_x(self, b):
        self.lreset()
        stg = [self.lalloc((4, D), F32) for _ in range(2)]
        for c in range(NCH):
            s = stg[c % 2]
            for a in range(4):
                for jj in range(2):
                    bk = self.bank()
                    for j4 in range(4):
                        j = jj * 4 + j4
                        o = self.PS[:, bk, j4 * 128:(j4 + 1) * 128]
                        i = self.xT[:, j, c * 512 + a * 128: c * 512 + (a + 1) * 128]
                        self.S.op("pe", lambda h, o=o, i=i: h.transpose(o, i, self.identf),
                                  [self.xTr[j][c], self.creg], [self.PSR[bk]])
                    dst = s.ap[:, a, jj * 512:(jj + 1) * 512]
                    self.copy("act" if jj % 2 else "dve", dst, self.PS[:, bk, :], [self.PSR[bk]], s.regs)
            dstd = self.y_out[b, c * 512:(c + 1) * 512, :].rearrange("(a p) d -> p a d", p=128)
            self.dma("sp", dstd, s.ap, "out%d" % (c % 2), s.regs, [])

    def rmsnorm_x(self, gain_col):
        sq = [self.lalloc((512,), BF16) for _ in range(2)]
        rs = self.lalloc((512,), F32)
        ones = self.cT["ones"]
        for c in range(NCH):
            cs = slice(c * 512, (c + 1) * 512)
            bk = self.bank()
            for j in range(8):
                q = sq[j % 2]
                self.act(q.ap, self.xT[:, j, cs], AF.Square, [self.xTr[j][c]], q.regs)
                self.mm(self.PS[:, bk, :], ones, q.ap, j == 0, j == 7, q.regs + [self.creg], [self.PSR[bk]])
            self.act(rs.ap, self.PS[:, bk, :], AF.Sqrt, [self.PSR[bk]], rs.regs, scale=1.0 / D, bias=self.eps_ap)
            self.recip(rs.ap, rs.ap, rs.regs, rs.regs)
            for j in range(8):
                g = self.vecs[:, gain_col + j:gain_col + j + 1]
                self.stt(self.hT[:, j, cs], self.xT[:, j, cs], g, rs.ap, ALU.mult, ALU.mult,
                         [self.xTr[j][c], self.creg] + rs.regs, [self.hTr[j][c]])

    def ffn(self, w_in, w_out, gain_col):
        self.lreset()
        self.rmsnorm_x(gain_col)
        gT = self.lalloc((8, T), BF16)
        gp = lambda i, c: gT.regs[i * 4 + c: i * 4 + c + 1]
        sl = [self.lalloc((512,), F32) for _ in range(2)]
        sli = 0
        for (g0, nf) in ((0, 8), (8, 8), (16, 6)):
            for fb in range(0, nf, 2):
                f0 = (g0 + fb) * 128
                wv, wr = self.wload([(w_in[:, f0:f0 + 256], 0, 256),
                                     (w_in[:, DFF + f0:DFF + f0 + 256], 256, 256)], 8)
                for c in range(NCH):
                    cs = slice(c * 512, (c + 1) * 512)
                    for ft in range(2):
                        bg = self.bank()
                        bu = self.bankA()
                        for k in range(8):
                            self.mm(self.PS[:, bg, :], wv[:, k, ft * 128:(ft + 1) * 128], self.hT[:, k, cs],
                                    k == 0, k == 7, wr + [self.hTr[k][c]], [self.PSR[bg]])
                        for k in range(8):
                            self.mm(self.PS[:, bu, :], wv[:, k, 256 + ft * 128:256 + (ft + 1) * 128],
                                    self.hT[:, k, cs], k == 0, k == 7, wr + [self.hTr[k][c]], [self.PSR[bu]])
                        s = sl[sli % 2]
                        sli += 1
                        self.act(s.ap, self.PS[:, bg, :], AF.Silu, [self.PSR[bg]], s.regs)
                        self.tt("dve", gT.ap[:, fb + ft, cs], s.ap, self.PS[:, bu, :], ALU.mult,
                                s.regs + [self.PSR[bu]], gp(fb + ft, c))
            for dq in range(2):
                wv, wr = self.wload([(w_out[g0 * 128:(g0 + nf) * 128, dq * 512:(dq + 1) * 512], 0, 512)], nf)
                for c in range(NCH):
                    cs = slice(c * 512, (c + 1) * 512)
                    for dt in range(4):
                        d = dq * 4 + dt
                        bk = self.bank()
                        for i in range(nf):
                            self.mm(self.PS[:, bk, :], wv[:, i, dt * 128:(dt + 1) * 128], gT.ap[:, i, cs],
                                    i == 0, i == nf - 1, wr + gp(i, c), [self.PSR[bk]])
                        self.stt(self.xT[:, d, cs], self.PS[:, bk, :], 0.5, self.xT[:, d, cs], ALU.mult, ALU.add,
                                 [self.PSR[bk], self.xTr[d][c]], [self.xTr[d][c]])


VEC_NG = 0
VEC_EPS = 48
VEC_BG = 56
VEC_QK = 120
NVEC = 160
NG = 2175
NGD = 383


def make_vecs(inp):
    v = np.zeros((128, NVEC), np.float32)
    ng = np.asarray(inp["norm_gain"], np.float32)
    v[:, VEC_NG:VEC_NG + 48] = ng.reshape(6, 8, 128).transpose(2, 0, 1).reshape(128, 48)
    v[:, VEC_EPS] = EPS
    v[:, VEC_EPS + 1] = EPS * 64
    v[:, VEC_EPS + 2] = EPS * 96
    bg = np.asarray(inp["b_gate"], np.float32)
    v[:, VEC_BG:VEC_BG + 64] = bg.reshape(8, 8, 128).transpose(2, 0, 1).reshape(128, 64)
    t2 = lambda a: np.concatenate([a, a])
    for l in range(2):
        o = VEC_QK + l * 16
        v[:, o + 0] = t2(np.asarray(inp["qk_gain_a"])[l, 0])
        v[:, o + 1] = t2(np.asarray(inp["qk_gain_a"])[l, 1])
        v[:, o + 2] = t2(np.asarray(inp["qk_gain_b"])[l, 0])
        v[:, o + 3] = t2(np.asarray(inp["qk_gain_b"])[l, 1])
        v[:, o + 4] = t2(np.asarray(inp["qk_gain_d"])[l, 0])
        v[:, o + 5] = t2(np.asarray(inp["qk_gain_d"])[l, 1])
        qc = np.asarray(inp["qk_gain_c"], np.float32)
        v[:, o + 6] = t2(qc[l, 0, :64])
        v[:, o + 7] = t2(qc[l, 1, :64])
        v[:64, o + 8] = t2(qc[l, 0, 64:])
        v[:64, o + 9] = t2(qc[l, 1, 64:])
        v[:, o + 10:o + 13] = np.asarray(inp["mla_norm_q"], np.float32)[l].reshape(3, 128).T
        v[:, o + 13:o + 15] = np.asarray(inp["mla_norm_kv"], np.float32)[l].reshape(2, 128).T
    return v


def make_gext(inp):
    rb = np.asarray(inp["rel_bias"], np.float32)
    dist = np.arange(NG) - 127
    bk = rel_bucket_np(dist)
    g = np.full((9, NG), NEG, np.float32)
    for h in range(8):
        g[h, 127:] = rb[h, bk[127:]]
    g[8, 127:] = 0.0
    gd = np.full((12, NGD), NEG, np.float32)
    dd = np.arange(NGD) - 127
    for gi, dil in enumerate((1, 4, 16)):
        ok = (dd >= 0) & (dd <= 128)
        b2 = rel_bucket_np(dd * dil)
        for h in range(4):
            gd[gi * 4 + h, ok] = rb[8 + gi * 4 + h, b2[ok]]
    return g, gd


def _hn_alloc(self):
    self.hn_sq = [self.lalloc((512,), BF16) for _ in range(2)]
    self.hn_tmp = [self.lalloc((512,), F32) for _ in range(2)]
    self.hn_rs = self.lalloc((512,), F32)
    self.hn_i = 0


def _vcol(self, c):
    return self.vecs[:, c:c + 1]


def _proj_fm(self, w, pieces, cb, tiles=None):
    sp = []
    o = 0
    for c0, n in pieces:
        sp.append((w[:, c0:c0 + n], o, n))
        o += n
    wv, wr = self.wload(sp, 8)
    if tiles is None:
        tiles = [(i * 128, min(128, o - i * 128)) for i in range((o + 127) // 128)]
    for c in range(NCH):
        cs = slice(c * 512, (c + 1) * 512)
        for ti, (tc0, m) in enumerate(tiles):
            bk = self.bank()
            for k in range(8):
                self.mm(self.PS[0:m, bk, :], wv[:, k, tc0:tc0 + m], self.hT[:, k, cs], k == 0, k == 7,
                        wr + [self.hTr[k][c]], [self.PSR[bk]])
            cb(ti, c, bk, m)


def _proj_tm(self, w, pieces, cb, tok_ap=None):
    sp = []
    o = 0
    for c0, n in pieces:
        sp.append((w[:, c0:c0 + n], o, n))
        o += n
    wv, wr = self.wload(sp, 8)
    for tt in range(16):
        bk = self.bank()
        for k in range(8):
            if tok_ap is None:
                lt = self.hT[:, k, tt * 128:(tt + 1) * 128]
                rd = [self.hTr[k][tt // 4]]
            else:
                lt, rd = tok_ap(k, tt)
            self.mm(self.PS[:, bk, 0:o], lt, wv[:, k, 0:o], k == 0, k == 7, wr + rd, [self.PSR[bk]])
        cb(tt, bk, o)


def _head_norm(self, bk, m, gain_col, eps_col, ss_scale, dst_ap, dst_regs, blk="bd64", split=None):
    i = self.hn_i
    self.hn_i += 1
    sq = self.hn_sq[i % 2]
    tmp = self.hn_tmp[i % 2]
    rs = self.hn_rs
    pr = [self.PSR[bk]]
    self.act(sq.ap[0:m, :], self.PS[0:m, bk, :], AF.Square, pr, sq.regs)
    self.copy("act", tmp.ap[0:m, :], self.PS[0:m, bk, :], pr, tmp.regs)
    b2 = self.bankA()
    self.mm(self.PS[0:m, b2, :], self.cT[blk][0:m, 0:m], sq.ap[0:m, :], True, True, sq.regs + [self.creg], [self.PSR[b2]])
    self.act(rs.ap[0:m, :], self.PS[0:m, b2, :], AF.Sqrt, [self.PSR[b2]], rs.regs, scale=ss_scale,
             bias=self.vecs[0:m, eps_col:eps_col + 1])
    self.recip(rs.ap[0:m, :], rs.ap[0:m, :], rs.regs, rs.regs)
    if split is not None:
        for e, d in enumerate(split):
            r = slice(64 * e, 64 * e + 64)
            self.stt(d, tmp.ap[r, :], self.vecs[r, gain_col:gain_col + 1], rs.ap[r, :], ALU.mult, ALU.mult,
                     tmp.regs + rs.regs + [self.creg], dst_regs)
        return
    self.stt(dst_ap, tmp.ap[0:m, :], self.vecs[0:m, gain_col:gain_col + 1], rs.ap[0:m, :], ALU.mult, ALU.mult,
             tmp.regs + rs.regs + [self.creg], dst_regs)


def _load_strip(self, dst, src_dram, row, ncols, stream):
    base = src_dram[row:row + 1, 0:ncols]
    ap = bass.AP(tensor=base.tensor, offset=base.offset, ap=[[1, 128], [1, ncols]])
    self.dma("pool", dst.ap[:, 0:ncols], ap, stream, [], dst.regs)


def _attn_seg(self, q0, nq, nsb, qk_fn, bias_fn, v_fn, e, dst_ap, dst_regs):
    bo = self.bankA()
    bd = self.bankA()
    ones = self.cT["ones"]
    for sb in range(nsb):
        qs = max(q0, sb * 128)
        n = q0 + nq - qs
        c0 = qs - q0
        bs = self.bank()
        mms = qk_fn(sb, qs, n) + bias_fn(sb, qs, n)
        for i, (o0, on, lt, rh, rd) in enumerate(mms):
            self.mm(self.PS[:, bs, o0:o0 + on], lt, rh, i == 0, i == len(mms) - 1, rd, [self.PSR[bs]])
        p = self.Pb[self.pi % 2]
        self.pi += 1
        self.act(p.ap[:, 0:n], self.PS[:, bs, 0:n], AF.Exp, [self.PSR[bs]], p.regs)
        vl, vr = v_fn(sb)
        self.mm(self.PS[:, bo, c0:c0 + n], vl, p.ap[:, 0:n], sb == 0, sb == nsb - 1, vr + p.regs, [self.PSR[bo]])
        self.mm(self.PS[:, bd, c0:c0 + n], ones, p.ap[:, 0:n], sb == 0, sb == nsb - 1, p.regs + [self.creg],
                [self.PSR[bd]])
    r = slice(64 * e, 64 * e + 64)
    rc = self.rcb
    self.recip(rc.ap[r, 0:nq], self.PS[r, bd, 0:nq], [self.PSR[bd]], rc.regs)
    self.tt("dve", dst_ap, self.PS[r, bo, 0:nq], rc.ap[r, 0:nq], ALU.mult, [self.PSR[bo]] + rc.regs, dst_regs)


def _attn_bufs(self):
    self.Pb = [self.lalloc((512,), BF16) for _ in range(2)]
    self.pi = 0
    self.rcb = self.lalloc((512,), F32)


def _mix_B(self, l):
    w = self.w_in[l]
    QO, QOr = self.QO[1], self.QOr[1]
    self.lreset()
    KB = self.lalloc((2, T), BF16)
    KZ = [self.lalloc((T,), BF16) for _ in range(4)]
    VB = self.lalloc((16, 256), BF16)
    maskT = self.lalloc((T,), BF16)
    SEL = self.lalloc((4096,), BF16)
    for kz in KZ:
        self.S.op("dve", lambda hh, kz=kz: hh.memset(kz.ap, 0.0), [], kz.regs)
    self.S.op("dve", lambda hh: hh.memset(maskT.ap, 0.0), [], maskT.regs)
    self.S.op("dve", lambda hh: hh.memset(SEL.ap, 0.0), [], SEL.regs)
    self.dma("pool", SEL.ap[0:32, :], self.din["c_sel"], "misc0", [], SEL.regs)
    bmk = self.lalloc((256,), F32)
    self.dma("sp", bmk.ap, self.din["c_bmk"], "misc1", [], bmk.regs)
    small = self.lalloc((256,), F32)
    kms = self.lalloc((2, 8), BF16)
    mb = self.lalloc((32,), BF16)
    mark = self.loc_cur
    self.hn_alloc()
    gq = VEC_QK + l * 16
    self.proj_fm(w, [(O_BQ, 256)], lambda ti, c, bk, m: self.head_norm(
        bk, 128, gq + 2, VEC_EPS + 1, 1.0, QO[:, ti, c * 512:(c + 1) * 512], [QOr[ti][c]]))
    def cb_bk(ti, c, bk, m):
        cs = slice(c * 512, (c + 1) * 512)
        self.head_norm(bk, 128, gq + 3, VEC_EPS, 1.0 / 64, None, KB.regs + KZ[2 * ti].regs + KZ[2 * ti + 1].regs,
                       split=(KZ[2 * ti].ap[0:64, cs], KZ[2 * ti + 1].ap[64:128, cs]))
        self.copy("act", KB.ap[0:64, ti, cs], KZ[2 * ti].ap[0:64, cs], KZ[2 * ti].regs, KB.regs)
        self.copy("act", KB.ap[64:128, ti, cs], KZ[2 * ti + 1].ap[64:128, cs], KZ[2 * ti + 1].regs, KB.regs)
    self.proj_fm(w, [(O_BK, 256)], cb_bk)
    self.proj_tm(w, [(O_BV, 256)], lambda tt, bk, o: self.copy(
        "act", VB.ap[:, tt, :], self.PS[:, bk, 0:256], [self.PSR[bk]], VB.regs))
    import os
    CUT = int(os.environ.get("BCUT", "9"))
    if CUT <= 1:
        return
    for j in range(2):
        for n in range(8):
            o = small.ap[:, j * 8 + n:j * 8 + n + 1]
            i = KB.ap[:, j, n * 256:(n + 1) * 256]
            self.S.op("dve", lambda h, o=o, i=i: h.reduce_sum(out=o, in_=i, axis=mybir.AxisListType.X),
                      KB.regs, small.regs)
    self.copy("dve", kms.ap, small.ap[:, 0:16].rearrange("p (a b) -> p a b", a=2), small.regs, kms.regs)
    gm = small.ap[:, 16:48]
    mx = small.ap[:, 48:80]
    C2 = int(os.environ.get("BCUT2", "9"))
    if C2 <= 0:
        return
    for qt in range(16):
        own = qt // 2
        bk = self.bank()
        for h in range(4):
            j, e = h // 2, h % 2
            self.mm(self.PS[:, bk, h * 8:(h + 1) * 8], QO[64 * e:64 * e + 64, j, qt * 128:(qt + 1) * 128],
                    kms.ap[64 * e:64 * e + 64, j, :], True, True, [QOr[j][qt // 4]] + kms.regs, [self.PSR[bk]])
        self.tt("dve", gm, self.PS[:, bk, 0:32], bmk.ap[:, own * 32:(own + 1) * 32], ALU.add,
                [self.PSR[bk]] + bmk.regs, small.regs)
        if C2 <= 1:
            continue
        for h in range(4):
            o = mx[:, h * 8:(h + 1) * 8]
            i = gm[:, h * 8:(h + 1) * 8]
            self.S.op("dve", lambda hh, o=o, i=i: hh.max(out=o, in_=i), small.regs, small.regs)
        if C2 <= 2:
            continue
        for h in range(4):
            self.ts("dve", mb.ap[:, h * 8:(h + 1) * 8], gm[:, h * 8:(h + 1) * 8], mx[:, h * 8 + 2:h * 8 + 3],
                    ALU.is_lt, small.regs, mb.regs, s2=NEG, op1=ALU.mult)
        if C2 <= 3:
            continue
        mv = mb.ap.rearrange("p (a b) -> p a b", a=4)[:, :, own:8]
        self.S.op("dve", lambda hh, mv=mv: hh.memset(mv, 0.0), [], mb.regs)
        if C2 <= 4:
            continue
        bt = self.bank()
        po = self.PS[0:32, bt, 0:128]
        self.mm(po, mb.ap, self.cT["ident"], True, True, mb.regs + [self.creg], [self.PSR[bt]])
        if C2 <= 5:
            continue
        self.copy("act", maskT.ap[0:32, qt * 128:(qt + 1) * 128], po, [self.PSR[bt]], maskT.regs)
    if CUT <= 2:
        return
    self.loc_cur = mark
    self.attn_bufs()
    strips = [self.lalloc((T,), BF16) for _ in range(2)]
    anti = self.cT["anti"]
    for h in range(4):
        if CUT <= 3 and h >= 1:
            break
        j, e = h // 2, h % 2
        st = strips[h % 2]
        self.load_strip(st, self.din["gext"], 4 + h, T, "strip%d" % (h % 2))
        rs_ = slice(64 * e, 64 * e + 64)
        for c in range(NCH):
            def qk_fn(sb, qs, n):
                return [(0, n, KZ[h].ap[:, sb * 128:(sb + 1) * 128], QO[:, j, qs:qs + n],
                         KZ[h].regs + [QOr[j][c]])]

            def bias_fn(sb, qs, n):
                off = qs - sb * 128
                nb = sb // 2
                return [(0, n, anti, st.ap[:, off:off + n], st.regs + [self.creg]),
                        (0, n, SEL.ap[:, (h * 8 + nb) * 128:(h * 8 + nb + 1) * 128], maskT.ap[:, qs:qs + n],
                         SEL.regs + maskT.regs)]

            def v_fn(sb):
                return VB.ap[:, sb, j * 128:(j + 1) * 128], VB.regs
            self.attn_seg(c * 512, 512, 4 * (c + 1), qk_fn, bias_fn, v_fn, e,
                          QO[rs_, j, c * 512:(c + 1) * 512], [QOr[j][c]])


for _n, _f in list(globals().items()):
    if _n.startswith("_") and callable(_f) and _n[1:] in (
            "hn_alloc", "vcol", "proj_fm", "proj_tm", "head_norm", "load_strip", "attn_seg", "attn_bufs", "mix_B"):
        setattr(Builder, _n[1:], _f)

def _merge(self, l):
    w = self.w_in[l]
    wbr = self.w_branch[l]
    wo = self.w_out[l]
    self.lreset()
    mT = self.lalloc((8, T), BF16)
    mp = lambda d, c: mT.regs[d * 4 + c:d * 4 + c + 1]
    gs = [self.lalloc((512,), F32) for _ in range(2)]
    acc = [self.lalloc((512,), F32) for _ in range(2)]
    tmp = [self.lalloc((512,), F32) for _ in range(2)]
    gi = 0
    for dp in range(4):
        d0 = dp * 256
        wg = []
        for half in range(2):
            wg.append(self.wload([(w[:, O_G + (2 * half) * 1024 + d0:O_G + (2 * half) * 1024 + d0 + 256], 0, 256),
                                  (w[:, O_G + (2 * half + 1) * 1024 + d0:O_G + (2 * half + 1) * 1024 + d0 + 256], 256, 256)], 8))
        wb, wbr_r = self.wload([(wbr[n, :, d0:d0 + 256], n * 256, 256) for n in range(4)], 2)
        for c in range(NCH):
            cs = slice(c * 512, (c + 1) * 512)
            for dt in range(2):
                d = dp * 2 + dt
                a = acc[(c * 2 + dt) % 2]
                for n in range(4):
                    wv, wr = wg[n // 2]
                    bg = self.bank()
                    for k in range(8):
                        self.mm(self.PS[:, bg, :], wv[:, k, (n % 2) * 256 + dt * 128:(n % 2) * 256 + (dt + 1) * 128],
                                self.hT[:, k, cs], k == 0, k == 7, wr + [self.hTr[k][c]], [self.PSR[bg]])
                    g = gs[gi % 2]
                    t = tmp[gi % 2]
                    gi += 1
                    self.act(g.ap, self.PS[:, bg, :], AF.Sigmoid, [self.PSR[bg]], g.regs,
                             bias=self.vcol(VEC_BG + (l * 4 + n) * 8 + d))
                    bm = self.bankA()
                    for jj in range(2):
                        self.mm(self.PS[:, bm, :], wb[:, jj, n * 256 + dt * 128:n * 256 + (dt + 1) * 128],
                                self.QO[n][:, jj, cs], jj == 0, jj == 1, wbr_r + [self.QOr[n][jj][c]], [self.PSR[bm]])
                    if n == 0:
                        self.tt("dve", a.ap, g.ap, self.PS[:, bm, :], ALU.mult, g.regs + [self.PSR[bm]], a.regs)
                    else:
                        self.tt("dve", t.ap, g.ap, self.PS[:, bm, :], ALU.mult, g.regs + [self.PSR[bm]], t.regs)
                        if n < 3:
                            self.tt("dve", a.ap, a.ap, t.ap, ALU.add, a.regs + t.regs, a.regs)
                        else:
                            self.tt("dve", mT.ap[:, d, cs], a.ap, t.ap, ALU.add, a.regs + t.regs, mp(d, c))
    for dq in range(2):
        wv, wr = self.wload([(wo[:, dq * 512:(dq + 1) * 512], 0, 512)], 8)
        for c in range(NCH):
            cs = slice(c * 512, (c + 1) * 512)
            for dt in range(4):
                d = dq * 4 + dt
                bk = self.bank()
                for k in range(8):
                    self.mm(self.PS[:, bk, :], wv[:, k, dt * 128:(dt + 1) * 128], mT.ap[:, k, cs], k == 0, k == 7,
                            wr + mp(k, c), [self.PSR[bk]])
                self.tt("dve", self.xT[:, d, cs], self.PS[:, bk, :], self.xT[:, d, cs], ALU.add,
                        [self.PSR[bk], self.xTr[d][c]], [self.xTr[d][c]])


def _mixer(self, l):
    self.lreset()
    self.rmsnorm_x(VEC_NG + (l * 3 + 1) * 8)
    en = self.dbg if self.dbg else "ABCD"
    if "A" in en:
        self.mix_A(l)
    if "B" in en:
        self.mix_B(l)
    if "C" in en:
        self.mix_C(l)
    if "D" in en:
        self.mix_D(l)
    if self.dbg:
        for n in range(4):
            if "ABCD"[n] in en:
                rr = [r for pr in self.QOr[n] for r in pr]
                self.dma("sp", self.dbg_out[n], self.QO[n], "out0", rr, [])
        return
    self.merge(l)


Builder.merge = _merge
Builder.mixer = _mixer


def _mix_C(self, l):
    w = self.w_in[l]
    QO, QOr = self.QO[2], self.QOr[2]
    self.lreset()
    QR = self.lalloc((2, T), BF16)
    KC = self.lalloc((2, T), BF16)
    KR = self.lalloc((2, T), BF16)
    VC = self.lalloc((16, 256), BF16)
    Em = self.lalloc((128,), BF16)
    Fm = self.lalloc((64,), BF16)
    Gm = self.lalloc((64,), BF16)
    ROT = self.lalloc((64,), F32)
    stc = self.lalloc((512,), BF16)
    self.dma("pool", Em.ap[0:64, :], self.din["c_E"], "cE", [], Em.regs)
    self.dma("pool", Fm.ap, self.din["c_F"], "cF", [], Fm.regs)
    self.dma("pool", Gm.ap[0:64, :], self.din["c_G"], "cG", [], Gm.regs)
    self.dma("sp", ROT.ap[0:64, :], self.din["c_rot"], "cROT", [], ROT.regs)
    self.load_strip(stc, self.din["gext"], 8, 512, "strip0")
    mark = self.loc_cur
    cqg = self.lalloc((3, 512), BF16)
    rl = self.lalloc((512,), F32)
    u_n = self.lalloc((512,), F32)
    u_r = self.lalloc((512,), F32)
    u_k = self.lalloc((512,), F32)
    sq_n = self.lalloc((512,), BF16)
    sq_r = self.lalloc((512,), BF16)
    rs_n = self.lalloc((512,), F32)
    rs_r = self.lalloc((512,), F32)
    cos = self.lalloc((512,), F32)
    sin = self.lalloc((512,), F32)
    t1 = self.lalloc((512,), F32)
    t2 = self.lalloc((512,), F32)
    rtm = self.lalloc((8,), F32)
    ones = self.cT["ones"]
    bd64 = self.cT["bd64"]
    gv = VEC_QK + l * 16
    H = slice(0, 64)
    st = {}

    def headnorm_rope(src_r, gain_n, gain_r, eps_col, ss_scale, dst_n, dst_n_regs, dst_r, dst_r_regs, c):
        cs = slice(c * 512, (c + 1) * 512)
        self.act(sq_n.ap, u_n.ap, AF.Square, u_n.regs, sq_n.regs)
        self.act(sq_r.ap[H, :], src_r.ap[H, :], AF.Square, src_r.regs, sq_r.regs)
        bsn = self.bankA()
        self.mm(self.PS[:, bsn, :], bd64, sq_n.ap, True, False, sq_n.regs + [self.creg], [self.PSR[bsn]])
        self.mm(self.PS[:, bsn, :], Em.ap[H, :], sq_r.ap[H, :], False, True, sq_r.regs + Em.regs, [self.PSR[bsn]])
        bsr = self.bankA()
        self.mm(self.PS[H, bsr, :], Fm.ap[:, 0:64], sq_n.ap, True, False, sq_n.regs + Fm.regs, [self.PSR[bsr]])
        self.mm(self.PS[H, bsr, :], Gm.ap[H, 0:64], sq_r.ap[H, :], False, True, sq_r.regs + Gm.regs, [self.PSR[bsr]])
        self.act(rs_n.ap, self.PS[:, bsn, :], AF.Sqrt, [self.PSR[bsn]], rs_n.regs, scale=ss_scale, bias=self.vcol(eps_col))
        self.recip(rs_n.ap, rs_n.ap, rs_n.regs, rs_n.regs)
        self.act(rs_r.ap[H, :], self.PS[H, bsr, :], AF.Sqrt, [self.PSR[bsr]], rs_r.regs, scale=ss_scale,
                 bias=self.vecs[H, eps_col:eps_col + 1])
        self.recip(rs_r.ap[H, :], rs_r.ap[H, :], rs_r.regs, rs_r.regs)
        self.stt(dst_n, u_n.ap, self.vcol(gain_n), rs_n.ap, ALU.mult, ALU.mult, u_n.regs + rs_n.regs + [self.creg], dst_n_regs)
        self.stt(t1.ap[H, :], src_r.ap[H, :], self.vecs[H, gain_r:gain_r + 1], rs_r.ap[H, :], ALU.mult, ALU.mult,
                 src_r.regs + rs_r.regs + [self.creg], t1.regs)
        import os
        if int(os.environ.get("CQ", "9")) <= 3:
            return
        bp = self.bank()
        self.mm(self.PS[H, bp, :], ROT.ap[H, 0:64], t1.ap[H, :], True, True, ROT.regs + t1.regs, [self.PSR[bp]])
        self.tt("dve", t2.ap[H, :], self.PS[H, bp, :], sin.ap[H, :], ALU.mult, [self.PSR[bp]] + sin.regs, t2.regs)
        self.tt("dve", t1.ap[H, :], t1.ap[H, :], cos.ap[H, :], ALU.mult, t1.regs + cos.regs, t1.regs)
        self.tt("dve", dst_r, t1.ap[H, :], t2.ap[H, :], ALU.add, t1.regs + t2.regs, dst_r_regs)

    def load_cs(c):
        self.dma("sp", cos.ap[H, :], self.din["c_cos"][:, c * 512:(c + 1) * 512], "ccos", [], cos.regs)
        self.dma("sp", sin.ap[H, :], self.din["c_sin"][:, c * 512:(c + 1) * 512], "csin", [], sin.regs)

    wuq = self.w_uq[l]
    pcs = []
    for h in range(4):
        pcs.append((wuq[:, h * 96:h * 96 + 64], h * 64, 64))
        pcs.append((wuq[:, h * 96 + 64:h * 96 + 96], 256 + h * 32, 32))
    import os
    CD = int(os.environ.get("CD", "9"))
    if CD <= 0:
        return
    wq, wq_r = self.wload(pcs, 3)
    if CD <= 1:
        load_cs(0)
        return

    def cb_q(ti, c, bk, m):
        cs = slice(c * 512, (c + 1) * 512)
        if ti == 0:
            st["ss"] = self.bankA()
            load_cs(c)
        bss = st["ss"]
        self.act(sq_n.ap, self.PS[:, bk, :], AF.Square, [self.PSR[bk]], sq_n.regs)
        self.mm(self.PS[:, bss, :], ones, sq_n.ap, ti == 0, ti == 2, sq_n.regs + [self.creg], [self.PSR[bss]])
        self.ts("dve", cqg.ap[:, ti, :], self.PS[:, bk, :], self.vcol(gv + 10 + ti), ALU.mult,
                [self.PSR[bk], self.creg], cqg.regs)
        if ti < 2:
            return
        import os
        CQ = int(os.environ.get("CQ", "9"))
        if CQ <= 1:
            return
        self.act(rl.ap, self.PS[:, bss, :], AF.Sqrt, [self.PSR[bss]], rl.regs, scale=1.0 / 384, bias=self.vcol(VEC_EPS))
        self.recip(rl.ap, rl.ap, rl.regs, rl.regs)
        for j in range(2):
            bn = self.bank()
            for k in range(3):
                self.mm(self.PS[:, bn, :], wq[:, k, j * 128:(j + 1) * 128], cqg.ap[:, k, :], k == 0, k == 2,
                        wq_r + cqg.regs, [self.PSR[bn]])
            self.tt("dve", u_n.ap, self.PS[:, bn, :], rl.ap, ALU.mult, [self.PSR[bn]] + rl.regs, u_n.regs)
            br = self.bank()
            for k in range(3):
                self.mm(self.PS[H, br, :], wq[:, k, 256 + j * 64:256 + (j + 1) * 64], cqg.ap[:, k, :], k == 0, k == 2,
                        wq_r + cqg.regs, [self.PSR[br]])
            self.tt("dve", u_r.ap[H, :], self.PS[H, br, :], rl.ap[H, :], ALU.mult, [self.PSR[br]] + rl.regs, u_r.regs)
            if CQ <= 2:
                continue
            headnorm_rope(u_r, gv + 6, gv + 8, VEC_EPS + 2, 1.0, QO[:, j, cs], [QOr[j][c]],
                          QR.ap[H, j, cs], QR.regs, c)

    self.proj_fm(w, [(O_CQ, 384)], cb_q)
    import os
    CC = int(os.environ.get("CCUT", "9"))
    if CC <= 1:
        return

    wukv = self.w_ukv[l]
    pcs = []
    for h in range(4):
        pcs.append((wukv[:, h * 128:h * 128 + 64], h * 64, 64))
        pcs.append((wukv[:, h * 128 + 64:h * 128 + 128], 256 + h * 64, 64))
    wk, wk_r = self.wload(pcs, 2)
    ckvg = cqg

    def cb_k(ti, c, bk, m):
        cs = slice(c * 512, (c + 1) * 512)
        if ti == 0:
            st["ss"] = self.bankA()
            st["tm"] = self.bankA()
            load_cs(c)
        bss, btm = st["ss"], st["tm"]
        if ti < 2:
            self.act(sq_n.ap, self.PS[:, bk, :], AF.Square, [self.PSR[bk]], sq_n.regs)
            self.mm(self.PS[:, bss, :], ones, sq_n.ap, ti == 0, ti == 1, sq_n.regs + [self.creg], [self.PSR[bss]])
            for a in range(4):
                self.mm(self.PS[:, btm, a:a + 1], sq_n.ap[:, a * 128:(a + 1) * 128], ones[:, 0:1],
                        ti == 0 and a == 0, ti == 1 and a == 3, sq_n.regs + [self.creg], [self.PSR[btm]])
            self.ts("dve", ckvg.ap[:, ti, :], self.PS[:, bk, :], self.vcol(gv + 13 + ti), ALU.mult,
                    [self.PSR[bk], self.creg], ckvg.regs)
            return
        self.copy("act", u_k.ap[H, :], self.PS[H, bk, :], [self.PSR[bk]], u_k.regs)
        self.act(rl.ap, self.PS[:, bss, :], AF.Sqrt, [self.PSR[bss]], rl.regs, scale=1.0 / 256, bias=self.vcol(VEC_EPS))
        self.recip(rl.ap, rl.ap, rl.regs, rl.regs)
        self.act(rtm.ap[:, 0:4], self.PS[:, btm, 0:4], AF.Sqrt, [self.PSR[btm]], rtm.regs, scale=1.0 / 256,
                 bias=self.vcol(VEC_EPS))
        self.recip(rtm.ap[:, 0:4], rtm.ap[:, 0:4], rtm.regs, rtm.regs)
        for j in range(2):
            bn = self.bank()
            for k in range(2):
                self.mm(self.PS[:, bn, :], wk[:, k, j * 128:(j + 1) * 128], ckvg.ap[:, k, :], k == 0, k == 1,
                        wk_r + ckvg.regs, [self.PSR[bn]])
            self.tt("dve", u_n.ap, self.PS[:, bn, :], rl.ap, ALU.mult, [self.PSR[bn]] + rl.regs, u_n.regs)
            headnorm_rope(u_k, gv + 7, gv + 9, VEC_EPS, 1.0 / 96, KC.ap[:, j, cs], KC.regs,
                          KR.ap[H, j, cs], KR.regs, c)
        for a in range(4):
            bv = self.bank()
            for k in range(2):
                self.mm(self.PS[:, bv, 0:256], ckvg.ap[:, k, a * 128:(a + 1) * 128], wk[:, k, 256:512], k == 0, k == 1,
                        wk_r + ckvg.regs, [self.PSR[bv]])
            self.ts("dve", VC.ap[:, c * 4 + a, :], self.PS[:, bv, 0:256], rtm.ap[:, a:a + 1], ALU.mult,
                    [self.PSR[bv]] + rtm.regs, VC.regs)

    self.proj_fm(w, [(O_CKV, 256), (O_CKR, 32), (O_CKR, 32)], cb_k, tiles=[(0, 128), (128, 128), (256, 64)])

    if CC <= 2:
        return
    self.loc_cur = mark
    self.attn_bufs()
    anti = self.cT["anti"]
    for h in range(4):
        j, e = h // 2, h % 2
        rs_ = slice(64 * e, 64 * e + 64)
        rr = slice(32 * e, 32 * e + 32)
        for c in range(NCH):
            def qk_fn(sb, qs, n):
                return [(0, n, KC.ap[rs_, j, sb * 128:(sb + 1) * 128], QO[rs_, j, qs:qs + n], KC.regs + [QOr[j][c]]),
                        (0, n, KR.ap[rr, j, sb * 128:(sb + 1) * 128], QR.ap[rr, j, qs:qs + n], KR.regs + QR.regs)]

            def bias_fn(sb, qs, n):
                if qs != sb * 128:
                    return []
                return [(0, n, anti, stc.ap[:, 0:n], stc.regs + [self.creg])]

            def v_fn(sb):
                return VC.ap[:, sb, j * 128:(j + 1) * 128], VC.regs
            self.attn_seg(c * 512, 512, 4 * (c + 1), qk_fn, bias_fn, v_fn, e,
                          QO[rs_, j, c * 512:(c + 1) * 512], [QOr[j][c]])


Builder.mix_C = _mix_C


def _mix_D(self, l):
    w = self.w_in[l]
    QO, QOr = self.QO[3], self.QOr[3]
    self.lreset()
    nacc = self.lalloc((T,), F32)
    dacc = self.lalloc((T,), F32)
    sd = [self.lalloc((256,), BF16) for _ in range(6)]
    QD = self.lalloc((T,), BF16)
    KD = [self.lalloc((T,), BF16) for _ in range(2)]
    for kz in KD:
        self.S.op("dve", lambda hh, kz=kz: hh.memset(kz.ap, 0.0), [], kz.regs)
    VD = self.lalloc((16, 128), BF16)
    self.hn_alloc()
    self.attn_bufs()
    anti = self.cT["anti"]
    ones = self.cT["ones"]
    gv = VEC_QK + l * 16
    allh = lambda k: [self.hTr[k][c] for c in range(NCH)]
    for j in range(2):
        for g, dil in enumerate((1, 4, 16)):
            nbk = 16 // dil
            for e in range(2):
                self.load_strip(sd[g * 2 + e], self.din["gd"], g * 4 + 2 * j + e, 256, "sd%d" % (g * 2 + e))

            def cb(ti, c, bk, m):
                cs = slice(c * 512, (c + 1) * 512)
                if ti == 0:
                    self.head_norm(bk, 128, gv + 4, VEC_EPS + 1, 1.0, QD.ap[:, cs], QD.regs)
                else:
                    self.head_norm(bk, 128, gv + 5, VEC_EPS, 1.0 / 64, None, KD[0].regs + KD[1].regs,
                                   split=(KD[0].ap[0:64, cs], KD[1].ap[64:128, cs]))
            self.proj_fm(w, [(O_DQ + g * 256 + j * 128, 128), (O_DK + g * 256 + j * 128, 128)], cb)

            def tok_ap(k, blk):
                r, n = blk // nbk, blk % nbk
                base = r + dil * n * 128
                return self.hT[:, k, base:base + 127 * dil + 1:dil], allh(k)
            self.proj_tm(w, [(O_DV + g * 256 + j * 128, 128)],
                         lambda blk, bk, o: self.copy("act", VD.ap[:, blk, :], self.PS[:, bk, 0:128], [self.PSR[bk]], VD.regs),
                         tok_ap=tok_ap)
            for e in range(2):
                rows = slice(64 * e, 64 * e + 64)
                st = sd[g * 2 + e]
                for r in range(dil):
                    for n in range(nbk):
                        bo = self.bankA()
                        bd = self.bankA()
                        qb = r + dil * n * 128
                        qsl = slice(qb, qb + 127 * dil + 1, dil)
                        kbs = ([n - 1] if n > 0 else []) + [n]
                        for i, kn in enumerate(kbs):
                            kb = r + dil * kn * 128
                            off = 128 if kn != n else 0
                            bs = self.bank()
                            self.mm(self.PS[:, bs, 0:128], KD[e].ap[:, kb:kb + 127 * dil + 1:dil], QD.ap[:, qsl], True, False,
                                    KD[e].regs + QD.regs, [self.PSR[bs]])
                            self.mm(self.PS[:, bs, 0:128], anti, st.ap[:, off:off + 128], False, True,
                                    st.regs + [self.creg], [self.PSR[bs]])
                            p = self.Pb[self.pi % 2]
                            self.pi += 1
                            self.act(p.ap[:, 0:128], self.PS[:, bs, 0:128], AF.Exp, [self.PSR[bs]], p.regs)
                            last = i == len(kbs) - 1
                            self.mm(self.PS[:, bo, 0:128], VD.ap[:, r * nbk + kn, :], p.ap[:, 0:128], i == 0, last,
                                    VD.regs + p.regs, [self.PSR[bo]])
                            self.mm(self.PS[:, bd, 0:128], ones, p.ap[:, 0:128], i == 0, last, p.regs + [self.creg],
                                    [self.PSR[bd]])
                        if g == 0:
                            self.copy("act", nacc.ap[rows, qsl], self.PS[rows, bo, 0:128], [self.PSR[bo]], nacc.regs)
                            self.copy("dve", dacc.ap[rows, qsl], self.PS[rows, bd, 0:128], [self.PSR[bd]], dacc.regs)
                        else:
                            self.tt("dve", nacc.ap[rows, qsl], self.PS[rows, bo, 0:128], nacc.ap[rows, qsl], ALU.add,
                                    [self.PSR[bo]] + nacc.regs, nacc.regs)
                            self.tt("dve", dacc.ap[rows, qsl], self.PS[rows, bd, 0:128], dacc.ap[rows, qsl], ALU.add,
                                    [self.PSR[bd]] + dacc.regs, dacc.regs)
        rc = self.rcb
        for c in range(NCH):
            cs = slice(c * 512, (c + 1) * 512)
            self.recip(rc.ap, dacc.ap[:, cs], dacc.regs, rc.regs)
            self.tt("dve", QO[:, j, cs], nacc.ap[:, cs], rc.ap, ALU.mult, nacc.regs + rc.regs, [QOr[j][c]])


NIT = 18
S0 = 64.0


def _mix_A(self, l):
    w = self.w_in[l]
    QO, QOr = self.QO[0], self.QOr[0]
    self.lreset()
    KA = [self.lalloc((T,), BF16) for _ in range(2)]
    for kz in KA:
        self.S.op("dve", lambda hh, kz=kz: hh.memset(kz.ap, 0.0), [], kz.regs)
    VA = self.lalloc((16, 128), BF16)
    IQ = self.lalloc((T,), BF16)
    IQ3 = self.lalloc((T,), BF16)
    IK3 = self.lalloc((T,), BF16)
    wab = self.lalloc((16, 4), F32)
    wsg = self.lalloc((16, 4), F32)
    cm = self.lalloc((128,), F32)
    self.dma("sp", cm.ap, self.din["c_cm"], "ccm", [], cm.regs)
    mark = self.loc_cur
    self.hn_alloc()
    gv = VEC_QK + l * 16
    self.proj_fm(w, [(O_AQ, 256)], lambda ti, c, bk, m: self.head_norm(
        bk, 128, gv + 0, VEC_EPS + 1, 1.0, QO[:, ti, c * 512:(c + 1) * 512], [QOr[ti][c]]))
    self.proj_fm(w, [(O_AK, 64), (O_AK, 64)], lambda ti, c, bk, m: self.head_norm(
        bk, 128, gv + 1, VEC_EPS, 1.0 / 64, None, KA[0].regs + KA[1].regs,
        split=(KA[0].ap[0:64, c * 512:(c + 1) * 512], KA[1].ap[64:128, c * 512:(c + 1) * 512])))

    def cb_i(ti, c, bk, m):
        cs = slice(c * 512, (c + 1) * 512)
        dst = (IQ, IQ3, IK3)[ti]
        self.copy("act" if ti % 2 else "dve", dst.ap[0:m, cs], self.PS[0:m, bk, :], [self.PSR[bk]], dst.regs)
    self.proj_fm(w, [(O_IQ, 128), (O_IK, 32), (O_IK, 32), (O_IK, 32)], cb_i, tiles=[(0, 96), (96, 32), (128, 96)])

    def cb_v(tt, bk, o):
        pr = [self.PSR[bk]]
        self.copy("act", VA.ap[:, tt, 0:64], self.PS[:, bk, 0:64], pr, VA.regs)
        self.copy("dve", VA.ap[:, tt, 64:128], self.PS[:, bk, 0:64], pr, VA.regs)
        self.act(wab.ap[:, tt, :], self.PS[:, bk, 64:68], AF.Abs, pr, wab.regs)
        self.act(wsg.ap[:, tt, :], self.PS[:, bk, 64:68], AF.Sign, pr, wsg.regs)
    self.proj_tm(w, [(O_AV, 64), (O_IW, 4)], cb_v)
    self.loc_cur = mark
    self.attn_bufs()
    score = self.lalloc((T,), F32)
    rt = [self.lalloc((512,), F32) for _ in range(2)]
    mbt = [self.lalloc((T,), BF16) for _ in range(2)]
    strips = [self.lalloc((T,), BF16) for _ in range(2)]
    small = self.lalloc((16,), F32)
    thr = small.ap[:, 0:1]
    cnt = small.ap[:, 1:2]
    g2 = small.ap[:, 2:3]
    anti = self.cT["anti"]
    ident = self.cT["ident"]
    ri = 0
    si = 0
    for qp in range(8):
        for i in range(2):
            qt = 2 * qp + i
            if qt < 2:
                continue
            nk = (qt + 1) * 128
            qsl = slice(qt * 128, (qt + 1) * 128)
            for s0 in range(0, nk, 512):
                sn = min(512, nk - s0)
                for h in range(4):
                    bz = self.bank()
                    if h < 3:
                        lt, rh = IQ.ap[32 * h:32 * h + 32, qsl], IK3.ap[32 * h:32 * h + 32, s0:s0 + sn]
                        rd = IQ.regs + IK3.regs
                    else:
                        lt, rh = IQ3.ap[0:32, qsl], IK3.ap[0:32, s0:s0 + sn]
                        rd = IQ3.regs + IK3.regs
                    self.mm(self.PS[:, bz, 0:sn], lt, rh, True, True, rd, [self.PSR[bz]])
                    r = rt[ri % 2]
                    ri += 1
                    self.act(r.ap[:, 0:sn], self.PS[:, bz, 0:sn], AF.Relu, [self.PSR[bz]] + wab.regs, r.regs,
                             scale=wab.ap[:, qt, h:h + 1])
                    if h == 0:
                        self.ts("dve", score.ap[:, s0:s0 + sn], r.ap[:, 0:sn], wsg.ap[:, qt, 0:1], ALU.mult,
                                r.regs + wsg.regs, score.regs)
                    else:
                        self.stt(score.ap[:, s0:s0 + sn], r.ap[:, 0:sn], wsg.ap[:, qt, h:h + 1], score.ap[:, s0:s0 + sn],
                                 ALU.mult, ALU.add, r.regs + wsg.regs + score.regs, score.regs)
            self.tt("dve", score.ap[:, qsl], score.ap[:, qsl], cm.ap, ALU.add, score.regs + cm.regs, score.regs)
            self.S.op("dve", lambda hh: hh.memset(thr, 0.0), [], small.regs)
            mb = mbt[i]
            for it in range(NIT):
                step = S0 / (2 ** it)
                self.ts("dve", mb.ap[:, 0:nk], score.ap[:, 0:nk], thr, ALU.is_gt, score.regs + small.regs,
                        mb.regs + small.regs, op1=ALU.add, accum=cnt)
                self.ts("dve", g2, cnt, 255.5, ALU.is_ge, small.regs, small.regs, s2=2.0 * step, op1=ALU.mult)
                self.stt(thr, g2, -step, thr, ALU.add, ALU.add, small.regs, small.regs)
            self.ts("dve", thr, thr, -S0 / (2 ** (NIT - 1)), ALU.add, small.regs, small.regs)
            self.ts("dve", mb.ap[:, 0:nk], score.ap[:, 0:nk], thr, ALU.is_le, score.regs + small.regs, mb.regs,
                    s2=NEG, op1=ALU.mult)
        q0 = qp * 256
        for h in range(4):
            j, e = h // 2, h % 2
            rs_ = slice(64 * e, 64 * e + 64)
            st = strips[si % 2]
            self.load_strip(st, self.din["gext"], h, q0 + 256, "strip%d" % (si % 2))
            si += 1

            def qk_fn(sb, qs, n):
                return [(0, n, KA[e].ap[:, sb * 128:(sb + 1) * 128], QO[:, j, qs:qs + n], KA[e].regs + [QOr[j][qp // 2]])]

            def bias_fn(sb, qs, n):
                off = qs - sb * 128
                out = [(0, n, anti, st.ap[:, off:off + n], st.regs + [self.creg])]
                if qp > 0:
                    for i in range(2):
                        t0 = q0 + i * 128
                        if t0 >= qs:
                            out.append((t0 - qs, 128, mbt[i].ap[:, sb * 128:(sb + 1) * 128], ident,
                                        mbt[i].regs + [self.creg]))
                return out

            def v_fn(sb):
                return VA.ap[:, sb, :], VA.regs
            self.attn_seg(q0, 256, 2 * (qp + 1), qk_fn, bias_fn, v_fn, e, QO[rs_, j, q0:q0 + 256], [QOr[j][qp // 2]])


Builder.mix_D = _mix_D
Builder.mix_A = _mix_A


_CACHE = {}


def get_nc(nseq, stages, dbg=None):
    key = (nseq, stages, dbg)
    if key not in _CACHE:
        b = Builder(nseq, stages, dbg)
        _CACHE[key] = b
        b.nc_built = b.build_wrapped()
    return _CACHE[key]


def _build_wrapped(self):
    return self.build()


Builder.build_wrapped = _build_wrapped


def make_inmap(inp):
    shared = {
        "w_ffn_in": np.ascontiguousarray(np.asarray(inp["w_ffn_in"], np.float32)),
        "w_ffn_out": np.ascontiguousarray(np.asarray(inp["w_ffn_out"], np.float32)),
        "vecs": make_vecs(inp),
    }
    for k, v in host_consts().items():
        shared["c_" + k] = v
    for k, v in host_consts2().items():
        shared["c_" + k] = v
    for k in ("w_in", "w_branch", "w_out", "w_mla_uq", "w_mla_ukv"):
        shared[k] = np.ascontiguousarray(np.asarray(inp[k], np.float32))
    shared["gext"], shared["gd"] = make_gext(inp)
    return shared


def kernel(**inp):
    ncores = 8
    nseq = 2
    b = get_nc(nseq, 99)
    x = np.ascontiguousarray(np.asarray(inp["x"], np.float32))
    shared = make_inmap(inp)
    in_maps = []
    for i in range(ncores):
        m = dict(shared)
        m["x"] = x[i * nseq:(i + 1) * nseq]
        in_maps.append(m)
    res = run_bass_kernel_spmd(b.nc_built, in_maps, core_ids=list(range(ncores)))
    return np.concatenate([r["y"] for r in res.results], axis=0)
```

```python
import numpy as np
import math
from contextlib import ExitStack
import concourse.bass as bass
import concourse.mybir as mybir
from concourse.bass_utils import run_bass_kernel_spmd

F32 = mybir.dt.float32
BF16 = mybir.dt.bfloat16
AF = mybir.ActivationFunctionType
ALU = mybir.AluOpType

D = 1024
T = 2048
DFF = 2816
INW = 8388
NCH = 4
NEG = -30000.0
EPS = 1e-6

O_AQ, O_AK, O_AV, O_IQ, O_IK, O_IW = 0, 256, 320, 384, 512, 544
O_BQ, O_BK, O_BV = 548, 804, 1060
O_CQ, O_CKV, O_CKR = 1316, 1700, 1956
O_DQ, O_DK, O_DV = 1988, 2756, 3524
O_G = 4292

PAGE = 1024
ARENA_BYTES = 224000


class Reg:
    __slots__ = ("w", "r", "excl")

    def __init__(self, excl=False):
        self.w = None
        self.r = {}
        self.excl = excl


class TT:
    __slots__ = ("ap", "regs")

    def __init__(self, ap, regs):
        self.ap = ap
        self.regs = regs


ENGS = ["pe", "act", "dve", "pool", "sp"]


class Sched:
    def __init__(self):
        self.ops = {e: [] for e in ENGS}
        self.cnt = {e: 0 for e in ENGS}
        self.waited = {e: {} for e in ENGS}
        self.dma_tot = {}

    def _waits(self, eng, reads, writes, k):
        need = {}
        for r in reads:
            t = r.w
            if t is None:
                continue
            key, val = t
            if key == eng and eng == "pe":
                continue
            if need.get(key, 0) < val:
                need[key] = val
        for w in writes:
            toks = list(w.r.values())
            if w.w is not None:
                toks.append(w.w)
            for key, val in toks:
                if key == eng and eng == "pe":
                    continue
                if need.get(key, 0) < val:
                    need[key] = val
        out = []
        wd = self.waited[eng]
        for key, val in need.items():
            if wd.get(key, 0) >= val:
                continue
            wd[key] = val
            out.append((key, val))
        return out

    def op(self, eng, fn, reads=(), writes=(), mode=None):
        if any(r.excl for r in reads):
            writes = list(writes) + [r for r in reads if r.excl]
            reads = [r for r in reads if not r.excl]
        k = self.cnt[eng] + 1
        waits = self._waits(eng, reads, writes, k)
        self.cnt[eng] = k
        self.ops[eng].append((0, fn, waits, mode))
        tok = (eng, k)
        for r in reads:
            r.r[eng] = tok
        for w in writes:
            w.w = tok
            w.r = {}

    def dma(self, queue, fn, stream, reads=(), writes=()):
        waits = self._waits(queue, reads, writes, 1 << 60)
        tot = self.dma_tot.get(stream, 0) + 16
        self.dma_tot[stream] = tot
        self.ops[queue].append((1, fn, waits, stream))
        key = ("D", stream)
        tok = (key, tot)
        for r in reads:
            r.r[key] = tok
        for w in writes:
            w.w = tok
            w.r = {}

    def final_waits(self, eng, streams):
        waits = []
        for s in streams:
            if s in self.dma_tot:
                waits.append((("D", s), self.dma_tot[s]))
        self.ops[eng].append((2, None, waits, None))

    def emit(self, nc):
        with ExitStack() as es:
            sems = {}
            for e in ENGS:
                sems[e] = es.enter_context(nc.semaphore("s_" + e))
            for i, s in enumerate(self.dma_tot):
                sems[("D", s)] = es.enter_context(nc.semaphore("d%d" % i))
            block = es.enter_context(nc.Block())
            names = {"pe": "tensor", "act": "scalar", "dve": "vector", "pool": "gpsimd", "sp": "sync"}

            def mk(e):
                def body(h):
                    se = sems[e]
                    last_mode = (128, 128, 0)
                    for kind, fn, waits, stream in self.ops[e]:
                        for key, val in waits:
                            h.wait_ge(sems[key], val)
                        if kind == 0:
                            if e == "pe":
                                md = stream if stream is not None else (128, 128, 0)
                                if md != last_mode:
                                    h.drain()
                                    self.ndrain = getattr(self, "ndrain", 0) + 1
                                    last_mode = md
                            fn(h).then_inc(se, 1)
                        elif kind == 1:
                            fn(h).then_inc(sems[("D", stream)], 16)
                return body

            for e in ENGS:
                getattr(block, names[e])(mk(e))


def rel_bucket_np(dist):
    n = np.maximum(dist, 0)
    max_exact = 16
    nf = np.maximum(n, 1).astype(np.float32)
    log_b = max_exact + (np.log(nf / np.float32(max_exact)) / np.float32(math.log(2048 / max_exact))
                         * np.float32(32 - max_exact)).astype(np.int32)
    return np.where(n < max_exact, n, np.minimum(log_b, 31))


def host_consts():
    c = {}
    eye = np.eye(128, dtype=np.float32)
    c["ident"] = eye
    c["anti"] = eye[::-1].copy()
    c["ones"] = np.ones((128, 128), np.float32)
    bd = np.zeros((128, 128), np.float32)
    bd[:64, :64] = 1
    bd[64:, 64:] = 1
    c["bd64"] = bd
    return c


def host_consts2():
    c = {}
    c["sel"] = np.kron(np.eye(32, dtype=np.float32), np.ones((1, 128), np.float32))
    bm = np.zeros((8, 4, 8), np.float32)
    for own in range(8):
        bm[own, :, own:] = -1e30
    c["bmk"] = np.broadcast_to(bm.reshape(1, 256), (128, 256)).copy()
    E = np.zeros((64, 128), np.float32)
    for k in range(64):
        E[k, (k // 32) * 64:(k // 32) * 64 + 64] = 1
    c["E"] = E
    cmm = np.where(np.arange(128)[None, :] <= np.arange(128)[:, None], 0.0, -1e30).astype(np.float32)
    c["cm"] = cmm
    c["F"] = E.T.copy()
    G = np.zeros((64, 64), np.float32)
    G[:32, :32] = 1
    G[32:, 32:] = 1
    c["G"] = G
    rot = np.zeros((64, 64), np.float32)
    for b in range(2):
        for m in range(16):
            rot[b * 32 + m + 16, b * 32 + m] = -1.0
            rot[b * 32 + m, b * 32 + m + 16] = 1.0
    c["rot"] = rot
    freqs = 10000.0 ** (-np.arange(16, dtype=np.float32) / 16)
    ang = np.arange(T, dtype=np.float32)[None, :] * np.tile(freqs, 4)[:, None].astype(np.float32)
    c["cos"] = np.cos(ang).astype(np.float32)
    c["sin"] = np.sin(ang).astype(np.float32)
    return c


class Builder:
    def __init__(self, nseq=2, stages=99, dbg=None):
        self.nseq = nseq
        self.stages = stages
        self.dbg = dbg
        self.S = Sched()
        self.nc = bass.Bass("TRN2", target_bir_lowering=False, dynamic_dma_scratch_size=4096)
        self.din = {}
        self.wslot_i = 0
        self.bank_i = 0
        self.bankA_i = 0

    def dram_in(self, name, shape, dt=F32):
        t = self.nc.dram_tensor(name, list(shape), dt, kind="ExternalInput").ap()
        self.din[name] = t
        return t

    def carve(self, off, free_shape, dt):
        n = 1
        for s in free_shape:
            n *= s
        nb = n * (4 if dt == F32 else 2)
        assert off % 4 == 0 and off + nb <= ARENA_BYTES, (off, nb)
        ap = self.arena[:, off // 2:(off + nb) // 2]
        if dt == F32:
            ap = ap.bitcast(F32)
        if len(free_shape) == 2:
            ap = ap.rearrange("p (a b) -> p a b", a=free_shape[0])
        elif len(free_shape) == 3:
            ap = ap.rearrange("p (a b c) -> p a b c", a=free_shape[0], b=free_shape[1])
        return ap, nb

    def lalloc(self, free_shape, dt):
        ap, nb = self.carve(self.loc_base + self.loc_cur, free_shape, dt)
        p0 = self.loc_cur // PAGE
        self.loc_cur += (nb + PAGE - 1) // PAGE * PAGE
        p1 = self.loc_cur // PAGE
        assert self.loc_base + self.loc_cur <= ARENA_BYTES, ("local overflow", self.loc_cur)
        return TT(ap, self.loc_pages[p0:p1])

    def lreset(self):
        self.loc_cur = 0

    def bank(self):
        b = self.bank_i
        self.bank_i = (b + 1) % 4
        return b

    def bankA(self):
        b = self.bankA_i
        self.bankA_i = (b + 1) % 4
        return 4 + b

    def mm(self, out, lhsT, rhs, start, stop, reads, writes):
        ru = lambda v: 32 if v <= 32 else (64 if v <= 64 else 128)
        kk = lhsT.shape[0]
        mmm = 1
        for d in lhsT.shape[1:]:
            mmm *= d
        self.S.op("pe", lambda h: h.matmul(out, lhsT, rhs, start=start, stop=stop), reads, writes,
                  mode=(ru(kk), ru(mmm), lhsT.offset // (lhsT.tensor.shape[1] * 32) if kk < 128 else 0))

    def act(self, out, in_, func, reads, writes, scale=1.0, bias=0.0, accum=None):
        if accum is None:
            self.S.op("act", lambda h: h.activation(out=out, in_=in_, func=func, scale=scale, bias=bias),
                      reads, writes)
        else:
            self.S.op("act", lambda h: h.activation(out=out, in_=in_, func=func, scale=scale, bias=bias,
                                                      accum_out=accum), reads, writes)

    def ts(self, eng, out, in0, s1, op0, reads, writes, s2=None, op1=None, accum=None):
        def fn(h):
            kw = {}
            if op1 is not None:
                kw["op1"] = op1
            if accum is not None:
                kw["accum_out"] = accum
            return h.tensor_scalar(out=out, in0=in0, scalar1=s1, scalar2=s2, op0=op0, **kw)
        self.S.op(eng, fn, reads, writes)

    def stt(self, out, in0, scalar, in1, op0, op1, reads, writes):
        self.S.op("dve", lambda h: h.scalar_tensor_tensor(out=out, in0=in0, scalar=scalar, in1=in1,
                                                            op0=op0, op1=op1), reads, writes)

    def tt(self, eng, out, in0, in1, op, reads, writes):
        self.S.op(eng, lambda h: h.tensor_tensor(out=out, in0=in0, in1=in1, op=op), reads, writes)

    def copy(self, eng, out, in_, reads, writes):
        if eng == "act":
            self.S.op("act", lambda h: h.copy(out=out, in_=in_), reads, writes)
        else:
            self.S.op(eng, lambda h: h.tensor_copy(out=out, in_=in_), reads, writes)

    def recip(self, out, in_, reads, writes):
        self.S.op("dve", lambda h: h.reciprocal(out=out, in_=in_), reads, writes)

    def dma(self, queue, out, in_, stream, reads, writes):
        if queue == "pool":
            self.S.dma(queue, lambda h: h.dma_start(out=out, in_=in_, max_dma_last_dim=2048), stream, reads, writes)
        else:
            self.S.dma(queue, lambda h: h.dma_start(out=out, in_=in_), stream, reads, writes)

    def wload(self, pieces, nk):
        s = self.wslot_i
        self.wslot_i = (s + 1) % len(self.wslots)
        ap_full, reg = self.wslots[s]
        tot = max(o + n for _, o, n in pieces)
        assert nk * tot * 2 <= 8192, (nk, tot)
        view = ap_full[:, 0:nk * tot].rearrange("p (k c) -> p k c", k=nk)
        for src, o, n in pieces:
            srcv = src.rearrange("(k p) c -> p k c", p=128)
            self.dma("pool", view[:, :, o:o + n], srcv, ("w", s), [], [reg])
        return view, [reg]

    def build(self):
        nc = self.nc
        ns = self.nseq
        x_in = self.dram_in("x", (ns, T, D))
        wfi = self.dram_in("w_ffn_in", (2, 2, D, 2 * DFF))
        wfo = self.dram_in("w_ffn_out", (2, 2, DFF, D))
        vecs = self.dram_in("vecs", (128, NVEC))
        self.w_in = self.dram_in("w_in", (2, D, INW))
        self.w_branch = self.dram_in("w_branch", (2, 4, 256, D))
        self.w_out = self.dram_in("w_out", (2, D, D))
        self.w_uq = self.dram_in("w_mla_uq", (2, 384, 384))
        self.w_ukv = self.dram_in("w_mla_ukv", (2, 256, 512))
        self.dram_in("gext", (9, NG))
        self.dram_in("gd", (12, NGD))
        for k, v in host_consts2().items():
            self.dram_in("c_" + k, v.shape)
        if self.dbg:
            self.dbg_out = self.nc.dram_tensor("dbg", [4, 128, 2, T], BF16, kind="ExternalOutput").ap()
        cst = {k: self.dram_in("c_" + k, v.shape) for k, v in host_consts().items()}
        self.y_out = nc.dram_tensor("y", [ns, T, D], F32, kind="ExternalOutput").ap()
        with ExitStack() as es:
            arena_t = es.enter_context(nc.sbuf_tensor("arena", [128, ARENA_BYTES // 2], BF16))
            self.arena = arena_t[:, :]
            ps_t = es.enter_context(nc.psum_tensor("ps", [128, 8, 512], F32))
            self.PS = ps_t
            self.PSR = [Reg(excl=True) for _ in range(8)]
            off = 0
            self.xT, nb = self.carve(off, (8, T), F32); off += nb
            self.xTr = [[Reg() for _ in range(NCH)] for _ in range(8)]
            self.hT, nb = self.carve(off, (8, T), BF16); off += nb
            self.hTr = [[Reg() for _ in range(NCH)] for _ in range(8)]
            self.QO = []
            self.QOr = []
            for n in range(4):
                q, nb = self.carve(off, (2, T), BF16); off += nb
                self.QO.append(q)
                self.QOr.append([[Reg() for _ in range(NCH)] for _ in range(2)])
            self.wslots = []
            for s in range(3):
                w, nb = self.carve(off, (4096,), BF16); off += nb
                self.wslots.append((w, Reg()))
            self.cT = {}
            creg = Reg()
            self.creg = creg
            for k, v in host_consts().items():
                ap, nb = self.carve(off, (v.shape[1],), BF16); off += nb
                self.cT[k] = ap
                self.dma("pool", ap, cst[k], "const", [], [creg])
            ap, nb = self.carve(off, (128,), F32); off += nb
            self.identf = ap
            self.dma("sp", ap, cst["ident"], "constf", [], [creg])
            ap, nb = self.carve(off, (NVEC,), F32); off += nb
            self.vecs = ap
            self.eps_ap = ap[:, VEC_EPS:VEC_EPS + 1]
            self.dma("sp", ap, vecs, "constf", [], [creg])
            off = (off + PAGE - 1) // PAGE * PAGE
            self.loc_base = off
            npages = (ARENA_BYTES - off) // PAGE
            self.loc_pages = [Reg() for _ in range(npages)]
            self.loc_cur = 0
            print("persistent bytes", off, "local pages", npages)

            for b in range(ns):
                self.load_x(x_in, b)
                st = 0
                for l in range(2):
                    for f in range(2):
                        if f == 1:
                            if st < self.stages:
                                self.mixer(l)
                                if self.dbg:
                                    st = 1000
                            st += 1
                        if st < self.stages:
                            self.ffn(wfi[l, f], wfo[l, f], VEC_NG + (l * 3 + (0 if f == 0 else 2)) * 8)
                        st += 1
                self.store_x(b)
            self.S.final_waits("sp", ["out0", "out1"])
            self.S.emit(nc)
        return nc

    def load_x(self, x_in, b):
        self.lreset()
        stg = [self.lalloc((4, D), F32) for _ in range(2)]
        for c in range(NCH):
            s = stg[c % 2]
            src = x_in[b, c * 512:(c + 1) * 512, :].rearrange("(a p) d -> p a d", p=128)
            self.dma("sp", s.ap, src, "xin%d" % (c % 2), [], s.regs)
            for j in range(8):
                bk = self.bank()
                for a in range(4):
                    o = self.PS[:, bk, a * 128:(a + 1) * 128]
                    i = s.ap[:, a, j * 128:(j + 1) * 128]
                    self.S.op("pe", lambda h, o=o, i=i: h.transpose(o, i, self.identf),
                              s.regs + [self.creg], [self.PSR[bk]])
                dst = self.xT[:, j, c * 512:(c + 1) * 512]
                self.copy("act" if j % 2 else "dve", dst, self.PS[:, bk, :], [self.PSR[bk]], [self.xTr[j][c]])

    def store_x(self, b):
        self.lreset()
        stg = [self.lalloc((4, D), F32) for _ in range(2)]
        for c in range(NCH):
            s = stg[c % 2]
            for a in range(4):
                for jj in range(2):
                    bk = self.bank()
                    for j4 in range(4):
                        j = jj * 4 + j4
                        o = self.PS[:, bk, j4 * 128:(j4 + 1) * 128]
                        i = self.xT[:, j, c * 512 + a * 128: c * 512 + (a + 1) * 128]
                        self.S.op("pe", lambda h, o=o, i=i: h.transpose(o, i, self.identf),
                                  [self.xTr[j][c], self.creg], [self.PSR[bk]])
                    dst = s.ap[:, a, jj * 512:(jj + 1) * 512]
                    self.copy("act" if jj % 2 else "dve", dst, self.PS[:, bk, :], [self.PSR[bk]], s.regs)
            dstd = self.y_out[b, c * 512:(c + 1) * 512, :].rearrange("(a p) d -> p a d", p=128)
            self.dma("sp", dstd, s.ap, "out%d" % (c % 2), s.regs, [])

    def rmsnorm_x(self, gain_col):
        sq = [self.lalloc((512,), BF16) for _ in range(2)]
        rs = self.lalloc((512,), F32)
        ones = self.cT["ones"]
        for c in range(NCH):
            cs = slice(c * 512, (c + 1) * 512)
            bk = self.bank()
            for j in range(8):
                q = sq[j % 2]
                self.act(q.ap, self.xT[:, j, cs], AF.Square, [self.xTr[j][c]], q.regs)
                self.mm(self.PS[:, bk, :], ones, q.ap, j == 0, j == 7, q.regs + [self.creg], [self.PSR[bk]])
            self.act(rs.ap, self.PS[:, bk, :], AF.Ln, [self.PSR[bk]], rs.regs, scale=1.0 / D, bias=self.eps_ap)
            self.act(rs.ap, rs.ap, AF.Exp, rs.regs, rs.regs, scale=-0.5)
            for j in range(8):
                g = self.vecs[:, gain_col + j:gain_col + j + 1]
                self.stt(self.hT[:, j, cs], self.xT[:, j, cs], g, rs.ap, ALU.mult, ALU.mult,
                         [self.xTr[j][c], self.creg] + rs.regs, [self.hTr[j][c]])

    def ffn(self, w_in, w_out, gain_col):
        self.lreset()
        self.rmsnorm_x(gain_col)
        gT = self.lalloc((8, T), BF16)
        gp = lambda i, c: gT.regs[i * 4 + c: i * 4 + c + 1]
        sl = [self.lalloc((512,), F32) for _ in range(2)]
        sli = 0
        for (g0, nf) in ((0, 8), (8, 8), (16, 6)):
            for fb in range(0, nf, 2):
                f0 = (g0 + fb) * 128
                wv, wr = self.wload([(w_in[:, f0:f0 + 256], 0, 256),
                                     (w_in[:, DFF + f0:DFF + f0 + 256], 256, 256)], 8)
                for c in range(NCH):
                    cs = slice(c * 512, (c + 1) * 512)
                    for ft in range(2):
                        bg = self.bank()
                        bu = self.bankA()
                        for k in range(8):
                            self.mm(self.PS[:, bg, :], wv[:, k, ft * 128:(ft + 1) * 128], self.hT[:, k, cs],
                                    k == 0, k == 7, wr + [self.hTr[k][c]], [self.PSR[bg]])
                        for k in range(8):
                            self.mm(self.PS[:, bu, :], wv[:, k, 256 + ft * 128:256 + (ft + 1) * 128],
                                    self.hT[:, k, cs], k == 0, k == 7, wr + [self.hTr[k][c]], [self.PSR[bu]])
                        s = sl[sli % 2]
                        sli += 1
                        self.act(s.ap, self.PS[:, bg, :], AF.Silu, [self.PSR[bg]], s.regs)
                        self.tt("dve", gT.ap[:, fb + ft, cs], s.ap, self.PS[:, bu, :], ALU.mult,
                                s.regs + [self.PSR[bu]], gp(fb + ft, c))
            for dq in range(2):
                wv, wr = self.wload([(w_out[g0 * 128:(g0 + nf) * 128, dq * 512:(dq + 1) * 512], 0, 512)], nf)
                for c in range(NCH):
                    cs = slice(c * 512, (c + 1) * 512)
                    for dt in range(4):
                        d = dq * 4 + dt
                        bk = self.bank()
                        for i in range(nf):
                            self.mm(self.PS[:, bk, :], wv[:, i, dt * 128:(dt + 1) * 128], gT.ap[:, i, cs],
                                    i == 0, i == nf - 1, wr + gp(i, c), [self.PSR[bk]])
                        self.stt(self.xT[:, d, cs], self.PS[:, bk, :], 0.5, self.xT[:, d, cs], ALU.mult, ALU.add,
                                 [self.PSR[bk], self.xTr[d][c]], [self.xTr[d][c]])


VEC_NG = 0
VEC_EPS = 48
VEC_BG = 56
VEC_QK = 120
NVEC = 160
NG = 2175
NGD = 383


def make_vecs(inp):
    v = np.zeros((128, NVEC), np.float32)
    ng = np.asarray(inp["norm_gain"], np.float32)
    v[:, VEC_NG:VEC_NG + 48] = ng.reshape(6, 8, 128).transpose(2, 0, 1).reshape(128, 48)
    v[:, VEC_EPS] = EPS
    v[:, VEC_EPS + 1] = EPS * 64
    v[:, VEC_EPS + 2] = EPS * 96
    bg = np.asarray(inp["b_gate"], np.float32)
    v[:, VEC_BG:VEC_BG + 64] = bg.reshape(8, 8, 128).transpose(2, 0, 1).reshape(128, 64)
    t2 = lambda a: np.concatenate([a, a])
    for l in range(2):
        o = VEC_QK + l * 16
        v[:, o + 0] = t2(np.asarray(inp["qk_gain_a"])[l, 0])
        v[:, o + 1] = t2(np.asarray(inp["qk_gain_a"])[l, 1])
        v[:, o + 2] = t2(np.asarray(inp["qk_gain_b"])[l, 0])
        v[:, o + 3] = t2(np.asarray(inp["qk_gain_b"])[l, 1])
        v[:, o + 4] = t2(np.asarray(inp["qk_gain_d"])[l, 0])
        v[:, o + 5] = t2(np.asarray(inp["qk_gain_d"])[l, 1])
        qc = np.asarray(inp["qk_gain_c"], np.float32)
        v[:, o + 6] = t2(qc[l, 0, :64])
        v[:, o + 7] = t2(qc[l, 1, :64])
        v[:64, o + 8] = t2(qc[l, 0, 64:])
        v[:64, o + 9] = t2(qc[l, 1, 64:])
        v[:, o + 10:o + 13] = np.asarray(inp["mla_norm_q"], np.float32)[l].reshape(3, 128).T
        v[:, o + 13:o + 15] = np.asarray(inp["mla_norm_kv"], np.float32)[l].reshape(2, 128).T
    return v


def make_gext(inp):
    rb = np.asarray(inp["rel_bias"], np.float32)
    dist = np.arange(NG) - 127
    bk = rel_bucket_np(dist)
    g = np.full((9, NG), NEG, np.float32)
    for h in range(8):
        g[h, 127:] = rb[h, bk[127:]]
    g[8, 127:] = 0.0
    gd = np.full((12, NGD), NEG, np.float32)
    dd = np.arange(NGD) - 127
    for gi, dil in enumerate((1, 4, 16)):
        ok = (dd >= 0) & (dd <= 128)
        b2 = rel_bucket_np(dd * dil)
        for h in range(4):
            gd[gi * 4 + h, ok] = rb[8 + gi * 4 + h, b2[ok]]
    return g, gd


def _hn_alloc(self):
    self.hn_sq = [self.lalloc((512,), BF16) for _ in range(2)]
    self.hn_tmp = [self.lalloc((512,), F32) for _ in range(2)]
    self.hn_rs = self.lalloc((512,), F32)
    self.hn_i = 0


def _vcol(self, c):
    return self.vecs[:, c:c + 1]


def _proj_fm(self, w, pieces, cb, tiles=None):
    sp = []
    o = 0
    for c0, n in pieces:
        sp.append((w[:, c0:c0 + n], o, n))
        o += n
    wv, wr = self.wload(sp, 8)
    if tiles is None:
        tiles = [(i * 128, min(128, o - i * 128)) for i in range((o + 127) // 128)]
    for c in range(NCH):
        cs = slice(c * 512, (c + 1) * 512)
        for ti, (tc0, m) in enumerate(tiles):
            bk = self.bank()
            for k in range(8):
                self.mm(self.PS[0:m, bk, :], wv[:, k, tc0:tc0 + m], self.hT[:, k, cs], k == 0, k == 7,
                        wr + [self.hTr[k][c]], [self.PSR[bk]])
            cb(ti, c, bk, m)


def _proj_tm(self, w, pieces, cb, tok_ap=None):
    sp = []
    o = 0
    for c0, n in pieces:
        sp.append((w[:, c0:c0 + n], o, n))
        o += n
    wv, wr = self.wload(sp, 8)
    for tt in range(16):
        bk = self.bank()
        for k in range(8):
            if tok_ap is None:
                lt = self.hT[:, k, tt * 128:(tt + 1) * 128]
                rd = [self.hTr[k][tt // 4]]
            else:
                lt, rd = tok_ap(k, tt)
            self.mm(self.PS[:, bk, 0:o], lt, wv[:, k, 0:o], k == 0, k == 7, wr + rd, [self.PSR[bk]])
        cb(tt, bk, o)


def _head_norm(self, bk, m, gain_col, eps_col, ss_scale, dst_ap, dst_regs, blk="bd64", split=None):
    i = self.hn_i
    self.hn_i += 1
    sq = self.hn_sq[i % 2]
    tmp = self.hn_tmp[i % 2]
    rs = self.hn_rs
    pr = [self.PSR[bk]]
    self.act(sq.ap[0:m, :], self.PS[0:m, bk, :], AF.Square, pr, sq.regs)
    self.copy("act", tmp.ap[0:m, :], self.PS[0:m, bk, :], pr, tmp.regs)
    b2 = self.bankA()
    self.mm(self.PS[0:m, b2, :], self.cT[blk][0:m, 0:m], sq.ap[0:m, :], True, True, sq.regs + [self.creg], [self.PSR[b2]])
    self.act(rs.ap[0:m, :], self.PS[0:m, b2, :], AF.Ln, [self.PSR[b2]], rs.regs, scale=ss_scale,
             bias=self.vecs[0:m, eps_col:eps_col + 1])
    self.act(rs.ap[0:m, :], rs.ap[0:m, :], AF.Exp, rs.regs, rs.regs, scale=-0.5)
    if split is not None:
        for e, d in enumerate(split):
            r = slice(64 * e, 64 * e + 64)
            self.stt(d, tmp.ap[r, :], self.vecs[r, gain_col:gain_col + 1], rs.ap[r, :], ALU.mult, ALU.mult,
                     tmp.regs + rs.regs + [self.creg], dst_regs)
        return
    self.stt(dst_ap, tmp.ap[0:m, :], self.vecs[0:m, gain_col:gain_col + 1], rs.ap[0:m, :], ALU.mult, ALU.mult,
             tmp.regs + rs.regs + [self.creg], dst_regs)


def _load_strip(self, dst, src_dram, row, ncols, stream):
    base = src_dram[row:row + 1, 0:ncols]
    ap = bass.AP(tensor=base.tensor, offset=base.offset, ap=[[1, 128], [1, ncols]])
    self.dma("pool", dst.ap[:, 0:ncols], ap, stream, [], dst.regs)


def _attn_seg(self, q0, nq, nsb, qk_fn, bias_fn, v_fn, e, dst_ap, dst_regs):
    bo = self.bankA()
    bd = self.bankA()
    ones = self.cT["ones"]
    for sb in range(nsb):
        qs = max(q0, sb * 128)
        n = q0 + nq - qs
        c0 = qs - q0
        bs = self.bank()
        mms = qk_fn(sb, qs, n) + bias_fn(sb, qs, n)
        for i, (o0, on, lt, rh, rd) in enumerate(mms):
            self.mm(self.PS[:, bs, o0:o0 + on], lt, rh, i == 0, i == len(mms) - 1, rd, [self.PSR[bs]])
        p = self.Pb[self.pi % 2]
        self.pi += 1
        self.act(p.ap[:, 0:n], self.PS[:, bs, 0:n], AF.Exp, [self.PSR[bs]], p.regs)
        vl, vr = v_fn(sb)
        self.mm(self.PS[:, bo, c0:c0 + n], vl, p.ap[:, 0:n], sb == 0, sb == nsb - 1, vr + p.regs, [self.PSR[bo]])
        self.mm(self.PS[:, bd, c0:c0 + n], ones, p.ap[:, 0:n], sb == 0, sb == nsb - 1, p.regs + [self.creg],
                [self.PSR[bd]])
    r = slice(64 * e, 64 * e + 64)
    rc = self.rcb
    self.act(rc.ap[r, 0:nq], self.PS[r, bd, 0:nq], AF.Ln, [self.PSR[bd]], rc.regs)
    self.act(rc.ap[r, 0:nq], rc.ap[r, 0:nq], AF.Exp, rc.regs, rc.regs, scale=-1.0)
    self.tt("dve", dst_ap, self.PS[r, bo, 0:nq], rc.ap[r, 0:nq], ALU.mult, [self.PSR[bo]] + rc.regs, dst_regs)


def _attn_bufs(self):
    self.Pb = [self.lalloc((512,), BF16) for _ in range(2)]
    self.pi = 0
    self.rcb = self.lalloc((512,), F32)


def _mix_B(self, l):
    w = self.w_in[l]
    QO, QOr = self.QO[1], self.QOr[1]
    self.lreset()
    KB = self.lalloc((2, T), BF16)
    KZ = [self.lalloc((T,), BF16) for _ in range(4)]
    VB = self.lalloc((16, 256), BF16)
    maskT = self.lalloc((T,), BF16)
    SEL = self.lalloc((4096,), BF16)
    for kz in KZ:
        self.S.op("dve", lambda hh, kz=kz: hh.memset(kz.ap, 0.0), [], kz.regs)
    self.S.op("dve", lambda hh: hh.memset(maskT.ap, 0.0), [], maskT.regs)
    self.S.op("dve", lambda hh: hh.memset(SEL.ap, 0.0), [], SEL.regs)
    self.dma("pool", SEL.ap[0:32, :], self.din["c_sel"], "misc0", [], SEL.regs)
    bmk = self.lalloc((256,), F32)
    self.dma("sp", bmk.ap, self.din["c_bmk"], "misc1", [], bmk.regs)
    small = self.lalloc((256,), F32)
    kms = self.lalloc((2, 8), BF16)
    mb = self.lalloc((32,), BF16)
    mark = self.loc_cur
    self.hn_alloc()
    gq = VEC_QK + l * 16
    self.proj_fm(w, [(O_BQ, 256)], lambda ti, c, bk, m: self.head_norm(
        bk, 128, gq + 2, VEC_EPS + 1, 1.0, QO[:, ti, c * 512:(c + 1) * 512], [QOr[ti][c]]))
    def cb_bk(ti, c, bk, m):
        cs = slice(c * 512, (c + 1) * 512)
        self.head_norm(bk, 128, gq + 3, VEC_EPS, 1.0 / 64, None, KB.regs + KZ[2 * ti].regs + KZ[2 * ti + 1].regs,
                       split=(KZ[2 * ti].ap[0:64, cs], KZ[2 * ti + 1].ap[64:128, cs]))
        self.copy("act", KB.ap[0:64, ti, cs], KZ[2 * ti].ap[0:64, cs], KZ[2 * ti].regs, KB.regs)
        self.copy("act", KB.ap[64:128, ti, cs], KZ[2 * ti + 1].ap[64:128, cs], KZ[2 * ti + 1].regs, KB.regs)
    self.proj_fm(w, [(O_BK, 256)], cb_bk)
    self.proj_tm(w, [(O_BV, 256)], lambda tt, bk, o: self.copy(
        "act", VB.ap[:, tt, :], self.PS[:, bk, 0:256], [self.PSR[bk]], VB.regs))
    import os
    CUT = int(os.environ.get("BCUT", "9"))
    if CUT <= 1:
        return
    for j in range(2):
        for n in range(8):
            o = small.ap[:, j * 8 + n:j * 8 + n + 1]
            i = KB.ap[:, j, n * 256:(n + 1) * 256]
            self.S.op("dve", lambda h, o=o, i=i: h.reduce_sum(out=o, in_=i, axis=mybir.AxisListType.X),
                      KB.regs, small.regs)
    self.copy("dve", kms.ap, small.ap[:, 0:16].rearrange("p (a b) -> p a b", a=2), small.regs, kms.regs)
    gm = small.ap[:, 16:48]
    mx = small.ap[:, 48:80]
    C2 = int(os.environ.get("BCUT2", "9"))
    if C2 <= 0:
        return
    for qt in range(16):
        own = qt // 2
        bk = self.bank()
        for h in range(4):
            j, e = h // 2, h % 2
            self.mm(self.PS[:, bk, h * 8:(h + 1) * 8], QO[64 * e:64 * e + 64, j, qt * 128:(qt + 1) * 128],
                    kms.ap[64 * e:64 * e + 64, j, :], True, True, [QOr[j][qt // 4]] + kms.regs, [self.PSR[bk]])
        self.tt("dve", gm, self.PS[:, bk, 0:32], bmk.ap[:, own * 32:(own + 1) * 32], ALU.add,
                [self.PSR[bk]] + bmk.regs, small.regs)
        if C2 <= 1:
            continue
        for h in range(4):
            o = mx[:, h * 8:(h + 1) * 8]
            i = gm[:, h * 8:(h + 1) * 8]
            self.S.op("dve", lambda hh, o=o, i=i: hh.max(out=o, in_=i), small.regs, small.regs)
        if C2 <= 2:
            continue
        for h in range(4):
            self.ts("dve", mb.ap[:, h * 8:(h + 1) * 8], gm[:, h * 8:(h + 1) * 8], mx[:, h * 8 + 2:h * 8 + 3],
                    ALU.is_lt, small.regs, mb.regs, s2=NEG, op1=ALU.mult)
        if C2 <= 3:
            continue
        mv = mb.ap.rearrange("p (a b) -> p a b", a=4)[:, :, own:8]
        self.S.op("dve", lambda hh, mv=mv: hh.memset(mv, 0.0), [], mb.regs)
        if C2 <= 4:
            continue
        bt = self.bank()
        po = self.PS[0:32, bt, 0:128]
        self.mm(po, mb.ap, self.cT["ident"], True, True, mb.regs + [self.creg], [self.PSR[bt]])
        if C2 <= 5:
            continue
        self.copy("act", maskT.ap[0:32, qt * 128:(qt + 1) * 128], po, [self.PSR[bt]], maskT.regs)
    if CUT <= 2:
        return
    self.loc_cur = mark
    self.attn_bufs()
    strips = [self.lalloc((T,), BF16) for _ in range(2)]
    anti = self.cT["anti"]
    for h in range(4):
        if CUT <= 3 and h >= 1:
            break
        j, e = h // 2, h % 2
        st = strips[h % 2]
        self.load_strip(st, self.din["gext"], 4 + h, T, "strip%d" % (h % 2))
        rs_ = slice(64 * e, 64 * e + 64)
        for c in range(NCH):
            def qk_fn(sb, qs, n):
                return [(0, n, KZ[h].ap[:, sb * 128:(sb + 1) * 128], QO[:, j, qs:qs + n],
                         KZ[h].regs + [QOr[j][c]])]

            def bias_fn(sb, qs, n):
                off = qs - sb * 128
                nb = sb // 2
                return [(0, n, anti, st.ap[:, off:off + n], st.regs + [self.creg]),
                        (0, n, SEL.ap[:, (h * 8 + nb) * 128:(h * 8 + nb + 1) * 128], maskT.ap[:, qs:qs + n],
                         SEL.regs + maskT.regs)]

            def v_fn(sb):
                return VB.ap[:, sb, j * 128:(j + 1) * 128], VB.regs
            self.attn_seg(c * 512, 512, 4 * (c + 1), qk_fn, bias_fn, v_fn, e,
                          QO[rs_, j, c * 512:(c + 1) * 512], [QOr[j][c]])


for _n, _f in list(globals().items()):
    if _n.startswith("_") and callable(_f) and _n[1:] in (
            "hn_alloc", "vcol", "proj_fm", "proj_tm", "head_norm", "load_strip", "attn_seg", "attn_bufs", "mix_B"):
        setattr(Builder, _n[1:], _f)

def _merge(self, l):
    w = self.w_in[l]
    wbr = self.w_branch[l]
    wo = self.w_out[l]
    self.lreset()
    mT = self.lalloc((8, T), BF16)
    mp = lambda d, c: mT.regs[d * 4 + c:d * 4 + c + 1]
    gs = [self.lalloc((512,), F32) for _ in range(2)]
    acc = [self.lalloc((512,), F32) for _ in range(2)]
    tmp = [self.lalloc((512,), F32) for _ in range(2)]
    gi = 0
    for dp in range(4):
        d0 = dp * 256
        wg = []
        for half in range(2):
            wg.append(self.wload([(w[:, O_G + (2 * half) * 1024 + d0:O_G + (2 * half) * 1024 + d0 + 256], 0, 256),
                                  (w[:, O_G + (2 * half + 1) * 1024 + d0:O_G + (2 * half + 1) * 1024 + d0 + 256], 256, 256)], 8))
        wb, wbr_r = self.wload([(wbr[n, :, d0:d0 + 256], n * 256, 256) for n in range(4)], 2)
        for c in range(NCH):
            cs = slice(c * 512, (c + 1) * 512)
            for dt in range(2):
                d = dp * 2 + dt
                a = acc[(c * 2 + dt) % 2]
                for n in range(4):
                    wv, wr = wg[n // 2]
                    bg = self.bank()
                    for k in range(8):
                        self.mm(self.PS[:, bg, :], wv[:, k, (n % 2) * 256 + dt * 128:(n % 2) * 256 + (dt + 1) * 128],
                                self.hT[:, k, cs], k == 0, k == 7, wr + [self.hTr[k][c]], [self.PSR[bg]])
                    g = gs[gi % 2]
                    t = tmp[gi % 2]
                    gi += 1
                    self.act(g.ap, self.PS[:, bg, :], AF.Sigmoid, [self.PSR[bg]], g.regs,
                             bias=self.vcol(VEC_BG + (l * 4 + n) * 8 + d))
                    bm = self.bankA()
                    for jj in range(2):
                        self.mm(self.PS[:, bm, :], wb[:, jj, n * 256 + dt * 128:n * 256 + (dt + 1) * 128],
                                self.QO[n][:, jj, cs], jj == 0, jj == 1, wbr_r + [self.QOr[n][jj][c]], [self.PSR[bm]])
                    if n == 0:
                        self.tt("dve", a.ap, g.ap, self.PS[:, bm, :], ALU.mult, g.regs + [self.PSR[bm]], a.regs)
                    else:
                        self.tt("dve", t.ap, g.ap, self.PS[:, bm, :], ALU.mult, g.regs + [self.PSR[bm]], t.regs)
                        if n < 3:
                            self.tt("dve", a.ap, a.ap, t.ap, ALU.add, a.regs + t.regs, a.regs)
                        else:
                            self.tt("dve", mT.ap[:, d, cs], a.ap, t.ap, ALU.add, a.regs + t.regs, mp(d, c))
    for dq in range(2):
        wv, wr = self.wload([(wo[:, dq * 512:(dq + 1) * 512], 0, 512)], 8)
        for c in range(NCH):
            cs = slice(c * 512, (c + 1) * 512)
            for dt in range(4):
                d = dq * 4 + dt
                bk = self.bank()
                for k in range(8):
                    self.mm(self.PS[:, bk, :], wv[:, k, dt * 128:(dt + 1) * 128], mT.ap[:, k, cs], k == 0, k == 7,
                            wr + mp(k, c), [self.PSR[bk]])
                self.tt("dve", self.xT[:, d, cs], self.PS[:, bk, :], self.xT[:, d, cs], ALU.add,
                        [self.PSR[bk], self.xTr[d][c]], [self.xTr[d][c]])


def _mixer(self, l):
    self.lreset()
    self.rmsnorm_x(VEC_NG + (l * 3 + 1) * 8)
    en = self.dbg if self.dbg else "ABCD"
    if "A" in en:
        self.mix_A(l)
    if "B" in en:
        self.mix_B(l)
    if "C" in en:
        self.mix_C(l)
    if "D" in en:
        self.mix_D(l)
    if self.dbg:
        for n in range(4):
            if "ABCD"[n] in en:
                rr = [r for pr in self.QOr[n] for r in pr]
                self.dma("sp", self.dbg_out[n], self.QO[n], "out0", rr, [])
        return
    self.merge(l)


Builder.merge = _merge
Builder.mixer = _mixer


def _mix_C(self, l):
    w = self.w_in[l]
    QO, QOr = self.QO[2], self.QOr[2]
    self.lreset()
    QR = self.lalloc((2, T), BF16)
    KC = self.lalloc((2, T), BF16)
    KR = self.lalloc((2, T), BF16)
    VC = self.lalloc((16, 256), BF16)
    Em = self.lalloc((128,), BF16)
    Fm = self.lalloc((64,), BF16)
    Gm = self.lalloc((64,), BF16)
    ROT = self.lalloc((64,), F32)
    stc = self.lalloc((512,), BF16)
    self.dma("pool", Em.ap[0:64, :], self.din["c_E"], "cE", [], Em.regs)
    self.dma("pool", Fm.ap, self.din["c_F"], "cF", [], Fm.regs)
    self.dma("pool", Gm.ap[0:64, :], self.din["c_G"], "cG", [], Gm.regs)
    self.dma("sp", ROT.ap[0:64, :], self.din["c_rot"], "cROT", [], ROT.regs)
    self.load_strip(stc, self.din["gext"], 8, 512, "strip0")
    mark = self.loc_cur
    cqg = self.lalloc((3, 512), BF16)
    rl = self.lalloc((512,), F32)
    u_n = self.lalloc((512,), F32)
    u_r = self.lalloc((512,), F32)
    u_k = self.lalloc((512,), F32)
    sq_n = self.lalloc((512,), BF16)
    sq_r = self.lalloc((512,), BF16)
    rs_n = self.lalloc((512,), F32)
    rs_r = self.lalloc((512,), F32)
    cos = self.lalloc((512,), F32)
    sin = self.lalloc((512,), F32)
    t1 = self.lalloc((512,), F32)
    t2 = self.lalloc((512,), F32)
    rtm = self.lalloc((8,), F32)
    ones = self.cT["ones"]
    bd64 = self.cT["bd64"]
    gv = VEC_QK + l * 16
    H = slice(0, 64)
    st = {}

    def headnorm_rope(src_r, gain_n, gain_r, eps_col, ss_scale, dst_n, dst_n_regs, dst_r, dst_r_regs, c):
        cs = slice(c * 512, (c + 1) * 512)
        self.act(sq_n.ap, u_n.ap, AF.Square, u_n.regs, sq_n.regs)
        self.act(sq_r.ap[H, :], src_r.ap[H, :], AF.Square, src_r.regs, sq_r.regs)
        bsn = self.bankA()
        self.mm(self.PS[:, bsn, :], bd64, sq_n.ap, True, False, sq_n.regs + [self.creg], [self.PSR[bsn]])
        self.mm(self.PS[:, bsn, :], Em.ap[H, :], sq_r.ap[H, :], False, True, sq_r.regs + Em.regs, [self.PSR[bsn]])
        bsr = self.bankA()
        self.mm(self.PS[H, bsr, :], Fm.ap[:, 0:64], sq_n.ap, True, False, sq_n.regs + Fm.regs, [self.PSR[bsr]])
        self.mm(self.PS[H, bsr, :], Gm.ap[H, 0:64], sq_r.ap[H, :], False, True, sq_r.regs + Gm.regs, [self.PSR[bsr]])
        self.act(rs_n.ap, self.PS[:, bsn, :], AF.Ln, [self.PSR[bsn]], rs_n.regs, scale=ss_scale, bias=self.vcol(eps_col))
        self.act(rs_n.ap, rs_n.ap, AF.Exp, rs_n.regs, rs_n.regs, scale=-0.5)
        self.act(rs_r.ap[H, :], self.PS[H, bsr, :], AF.Ln, [self.PSR[bsr]], rs_r.regs, scale=ss_scale,
                 bias=self.vecs[H, eps_col:eps_col + 1])
        self.act(rs_r.ap[H, :], rs_r.ap[H, :], AF.Exp, rs_r.regs, rs_r.regs, scale=-0.5)
        self.stt(dst_n, u_n.ap, self.vcol(gain_n), rs_n.ap, ALU.mult, ALU.mult, u_n.regs + rs_n.regs + [self.creg], dst_n_regs)
        self.stt(t1.ap[H, :], src_r.ap[H, :], self.vecs[H, gain_r:gain_r + 1], rs_r.ap[H, :], ALU.mult, ALU.mult,
                 src_r.regs + rs_r.regs + [self.creg], t1.regs)
        import os
        if int(os.environ.get("CQ", "9")) <= 3:
            return
        bp = self.bank()
        self.mm(self.PS[H, bp, :], ROT.ap[H, 0:64], t1.ap[H, :], True, True, ROT.regs + t1.regs, [self.PSR[bp]])
        self.tt("dve", t2.ap[H, :], self.PS[H, bp, :], sin.ap[H, :], ALU.mult, [self.PSR[bp]] + sin.regs, t2.regs)
        self.tt("dve", t1.ap[H, :], t1.ap[H, :], cos.ap[H, :], ALU.mult, t1.regs + cos.regs, t1.regs)
        self.tt("dve", dst_r, t1.ap[H, :], t2.ap[H, :], ALU.add, t1.regs + t2.regs, dst_r_regs)

    def load_cs(c):
        self.dma("sp", cos.ap[H, :], self.din["c_cos"][:, c * 512:(c + 1) * 512], "ccos", [], cos.regs)
        self.dma("sp", sin.ap[H, :], self.din["c_sin"][:, c * 512:(c + 1) * 512], "csin", [], sin.regs)

    wuq = self.w_uq[l]
    pcs = []
    for h in range(4):
        pcs.append((wuq[:, h * 96:h * 96 + 64], h * 64, 64))
        pcs.append((wuq[:, h * 96 + 64:h * 96 + 96], 256 + h * 32, 32))
    import os
    CD = int(os.environ.get("CD", "9"))
    if CD <= 0:
        return
    wq, wq_r = self.wload(pcs, 3)
    if CD <= 1:
        load_cs(0)
        return

    def cb_q(ti, c, bk, m):
        cs = slice(c * 512, (c + 1) * 512)
        if ti == 0:
            st["ss"] = self.bankA()
            load_cs(c)
        bss = st["ss"]
        self.act(sq_n.ap, self.PS[:, bk, :], AF.Square, [self.PSR[bk]], sq_n.regs)
        self.mm(self.PS[:, bss, :], ones, sq_n.ap, ti == 0, ti == 2, sq_n.regs + [self.creg], [self.PSR[bss]])
        self.ts("dve", cqg.ap[:, ti, :], self.PS[:, bk, :], self.vcol(gv + 10 + ti), ALU.mult,
                [self.PSR[bk], self.creg], cqg.regs)
        if ti < 2:
            return
        import os
        CQ = int(os.environ.get("CQ", "9"))
        if CQ <= 1:
            return
        self.act(rl.ap, self.PS[:, bss, :], AF.Ln, [self.PSR[bss]], rl.regs, scale=1.0 / 384, bias=self.vcol(VEC_EPS))
        self.act(rl.ap, rl.ap, AF.Exp, rl.regs, rl.regs, scale=-0.5)
        for j in range(2):
            bn = self.bank()
            for k in range(3):
                self.mm(self.PS[:, bn, :], wq[:, k, j * 128:(j + 1) * 128], cqg.ap[:, k, :], k == 0, k == 2,
                        wq_r + cqg.regs, [self.PSR[bn]])
            self.tt("dve", u_n.ap, self.PS[:, bn, :], rl.ap, ALU.mult, [self.PSR[bn]] + rl.regs, u_n.regs)
            br = self.bank()
            for k in range(3):
                self.mm(self.PS[H, br, :], wq[:, k, 256 + j * 64:256 + (j + 1) * 64], cqg.ap[:, k, :], k == 0, k == 2,
                        wq_r + cqg.regs, [self.PSR[br]])
            self.tt("dve", u_r.ap[H, :], self.PS[H, br, :], rl.ap[H, :], ALU.mult, [self.PSR[br]] + rl.regs, u_r.regs)
            if CQ <= 2:
                continue
            headnorm_rope(u_r, gv + 6, gv + 8, VEC_EPS + 2, 1.0, QO[:, j, cs], [QOr[j][c]],
                          QR.ap[H, j, cs], QR.regs, c)

    self.proj_fm(w, [(O_CQ, 384)], cb_q)
    import os
    CC = int(os.environ.get("CCUT", "9"))
    if CC <= 1:
        return

    wukv = self.w_ukv[l]
    pcs = []
    for h in range(4):
        pcs.append((wukv[:, h * 128:h * 128 + 64], h * 64, 64))
        pcs.append((wukv[:, h * 128 + 64:h * 128 + 128], 256 + h * 64, 64))
    wk, wk_r = self.wload(pcs, 2)
    ckvg = cqg

    def cb_k(ti, c, bk, m):
        cs = slice(c * 512, (c + 1) * 512)
        if ti == 0:
            st["ss"] = self.bankA()
            st["tm"] = self.bankA()
            load_cs(c)
        bss, btm = st["ss"], st["tm"]
        if ti < 2:
            self.act(sq_n.ap, self.PS[:, bk, :], AF.Square, [self.PSR[bk]], sq_n.regs)
            self.mm(self.PS[:, bss, :], ones, sq_n.ap, ti == 0, ti == 1, sq_n.regs + [self.creg], [self.PSR[bss]])
            for a in range(4):
                self.mm(self.PS[:, btm, a:a + 1], sq_n.ap[:, a * 128:(a + 1) * 128], ones[:, 0:1],
                        ti == 0 and a == 0, ti == 1 and a == 3, sq_n.regs + [self.creg], [self.PSR[btm]])
            self.ts("dve", ckvg.ap[:, ti, :], self.PS[:, bk, :], self.vcol(gv + 13 + ti), ALU.mult,
                    [self.PSR[bk], self.creg], ckvg.regs)
            return
        self.copy("act", u_k.ap[H, :], self.PS[H, bk, :], [self.PSR[bk]], u_k.regs)
        self.act(rl.ap, self.PS[:, bss, :], AF.Ln, [self.PSR[bss]], rl.regs, scale=1.0 / 256, bias=self.vcol(VEC_EPS))
        self.act(rl.ap, rl.ap, AF.Exp, rl.regs, rl.regs, scale=-0.5)
        self.act(rtm.ap[:, 0:4], self.PS[:, btm, 0:4], AF.Ln, [self.PSR[btm]], rtm.regs, scale=1.0 / 256,
                 bias=self.vcol(VEC_EPS))
        self.act(rtm.ap[:, 0:4], rtm.ap[:, 0:4], AF.Exp, rtm.regs, rtm.regs, scale=-0.5)
        for j in range(2):
            bn = self.bank()
            for k in range(2):
                self.mm(self.PS[:, bn, :], wk[:, k, j * 128:(j + 1) * 128], ckvg.ap[:, k, :], k == 0, k == 1,
                        wk_r + ckvg.regs, [self.PSR[bn]])
            self.tt("dve", u_n.ap, self.PS[:, bn, :], rl.ap, ALU.mult, [self.PSR[bn]] + rl.regs, u_n.regs)
            headnorm_rope(u_k, gv + 7, gv + 9, VEC_EPS, 1.0 / 96, KC.ap[:, j, cs], KC.regs,
                          KR.ap[H, j, cs], KR.regs, c)
        for a in range(4):
            bv = self.bank()
            for k in range(2):
                self.mm(self.PS[:, bv, 0:256], ckvg.ap[:, k, a * 128:(a + 1) * 128], wk[:, k, 256:512], k == 0, k == 1,
                        wk_r + ckvg.regs, [self.PSR[bv]])
            self.ts("dve", VC.ap[:, c * 4 + a, :], self.PS[:, bv, 0:256], rtm.ap[:, a:a + 1], ALU.mult,
                    [self.PSR[bv]] + rtm.regs, VC.regs)

    self.proj_fm(w, [(O_CKV, 256), (O_CKR, 32), (O_CKR, 32)], cb_k, tiles=[(0, 128), (128, 128), (256, 64)])

    if CC <= 2:
        return
    self.loc_cur = mark
    self.attn_bufs()
    anti = self.cT["anti"]
    for h in range(4):
        j, e = h // 2, h % 2
        rs_ = slice(64 * e, 64 * e + 64)
        rr = slice(32 * e, 32 * e + 32)
        for c in range(NCH):
            def qk_fn(sb, qs, n):
                return [(0, n, KC.ap[rs_, j, sb * 128:(sb + 1) * 128], QO[rs_, j, qs:qs + n], KC.regs + [QOr[j][c]]),
                        (0, n, KR.ap[rr, j, sb * 128:(sb + 1) * 128], QR.ap[rr, j, qs:qs + n], KR.regs + QR.regs)]

            def bias_fn(sb, qs, n):
                if qs != sb * 128:
                    return []
                return [(0, n, anti, stc.ap[:, 0:n], stc.regs + [self.creg])]

            def v_fn(sb):
                return VC.ap[:, sb, j * 128:(j + 1) * 128], VC.regs
            self.attn_seg(c * 512, 512, 4 * (c + 1), qk_fn, bias_fn, v_fn, e,
                          QO[rs_, j, c * 512:(c + 1) * 512], [QOr[j][c]])


Builder.mix_C = _mix_C


def _mix_D(self, l):
    w = self.w_in[l]
    QO, QOr = self.QO[3], self.QOr[3]
    self.lreset()
    nacc = self.lalloc((T,), F32)
    dacc = self.lalloc((T,), F32)
    sd = [self.lalloc((256,), BF16) for _ in range(6)]
    QD = self.lalloc((T,), BF16)
    KD = [self.lalloc((T,), BF16) for _ in range(2)]
    for kz in KD:
        self.S.op("dve", lambda hh, kz=kz: hh.memset(kz.ap, 0.0), [], kz.regs)
    VD = self.lalloc((16, 128), BF16)
    self.hn_alloc()
    self.attn_bufs()
    anti = self.cT["anti"]
    ones = self.cT["ones"]
    gv = VEC_QK + l * 16
    allh = lambda k: [self.hTr[k][c] for c in range(NCH)]
    for j in range(2):
        for g, dil in enumerate((1, 4, 16)):
            nbk = 16 // dil
            for e in range(2):
                self.load_strip(sd[g * 2 + e], self.din["gd"], g * 4 + 2 * j + e, 256, "sd%d" % (g * 2 + e))

            def cb(ti, c, bk, m):
                cs = slice(c * 512, (c + 1) * 512)
                if ti == 0:
                    self.head_norm(bk, 128, gv + 4, VEC_EPS + 1, 1.0, QD.ap[:, cs], QD.regs)
                else:
                    self.head_norm(bk, 128, gv + 5, VEC_EPS, 1.0 / 64, None, KD[0].regs + KD[1].regs,
                                   split=(KD[0].ap[0:64, cs], KD[1].ap[64:128, cs]))
            self.proj_fm(w, [(O_DQ + g * 256 + j * 128, 128), (O_DK + g * 256 + j * 128, 128)], cb)

            def tok_ap(k, blk):
                r, n = blk // nbk, blk % nbk
                base = r + dil * n * 128
                return self.hT[:, k, base:base + 127 * dil + 1:dil], allh(k)
            self.proj_tm(w, [(O_DV + g * 256 + j * 128, 128)],
                         lambda blk, bk, o: self.copy("act", VD.ap[:, blk, :], self.PS[:, bk, 0:128], [self.PSR[bk]], VD.regs),
                         tok_ap=tok_ap)
            for e in range(2):
                rows = slice(64 * e, 64 * e + 64)
                st = sd[g * 2 + e]
                for r in range(dil):
                    for n in range(nbk):
                        bo = self.bankA()
                        bd = self.bankA()
                        qb = r + dil * n * 128
                        qsl = slice(qb, qb + 127 * dil + 1, dil)
                        kbs = ([n - 1] if n > 0 else []) + [n]
                        for i, kn in enumerate(kbs):
                            kb = r + dil * kn * 128
                            off = 128 if kn != n else 0
                            bs = self.bank()
                            self.mm(self.PS[:, bs, 0:128], KD[e].ap[:, kb:kb + 127 * dil + 1:dil], QD.ap[:, qsl], True, False,
                                    KD[e].regs + QD.regs, [self.PSR[bs]])
                            self.mm(self.PS[:, bs, 0:128], anti, st.ap[:, off:off + 128], False, True,
                                    st.regs + [self.creg], [self.PSR[bs]])
                            p = self.Pb[self.pi % 2]
                            self.pi += 1
                            self.act(p.ap[:, 0:128], self.PS[:, bs, 0:128], AF.Exp, [self.PSR[bs]], p.regs)
                            last = i == len(kbs) - 1
                            self.mm(self.PS[:, bo, 0:128], VD.ap[:, r * nbk + kn, :], p.ap[:, 0:128], i == 0, last,
                                    VD.regs + p.regs, [self.PSR[bo]])
                            self.mm(self.PS[:, bd, 0:128], ones, p.ap[:, 0:128], i == 0, last, p.regs + [self.creg],
                                    [self.PSR[bd]])
                        if g == 0:
                            self.copy("act", nacc.ap[rows, qsl], self.PS[rows, bo, 0:128], [self.PSR[bo]], nacc.regs)
                            self.copy("dve", dacc.ap[rows, qsl], self.PS[rows, bd, 0:128], [self.PSR[bd]], dacc.regs)
                        else:
                            self.tt("dve", nacc.ap[rows, qsl], self.PS[rows, bo, 0:128], nacc.ap[rows, qsl], ALU.add,
                                    [self.PSR[bo]] + nacc.regs, nacc.regs)
                            self.tt("dve", dacc.ap[rows, qsl], self.PS[rows, bd, 0:128], dacc.ap[rows, qsl], ALU.add,
                                    [self.PSR[bd]] + dacc.regs, dacc.regs)
        rc = self.rcb
        for c in range(NCH):
            cs = slice(c * 512, (c + 1) * 512)
            self.act(rc.ap, dacc.ap[:, cs], AF.Ln, dacc.regs, rc.regs)
            self.act(rc.ap, rc.ap, AF.Exp, rc.regs, rc.regs, scale=-1.0)
            self.tt("dve", QO[:, j, cs], nacc.ap[:, cs], rc.ap, ALU.mult, nacc.regs + rc.regs, [QOr[j][c]])


NIT = 18
S0 = 64.0


def _mix_A(self, l):
    w = self.w_in[l]
    QO, QOr = self.QO[0], self.QOr[0]
    self.lreset()
    KA = [self.lalloc((T,), BF16) for _ in range(2)]
    for kz in KA:
        self.S.op("dve", lambda hh, kz=kz: hh.memset(kz.ap, 0.0), [], kz.regs)
    VA = self.lalloc((16, 128), BF16)
    IQ = self.lalloc((T,), BF16)
    IQ3 = self.lalloc((T,), BF16)
    IK3 = self.lalloc((T,), BF16)
    wab = self.lalloc((16, 4), F32)
    wsg = self.lalloc((16, 4), F32)
    cm = self.lalloc((128,), F32)
    self.dma("sp", cm.ap, self.din["c_cm"], "ccm", [], cm.regs)
    mark = self.loc_cur
    self.hn_alloc()
    gv = VEC_QK + l * 16
    self.proj_fm(w, [(O_AQ, 256)], lambda ti, c, bk, m: self.head_norm(
        bk, 128, gv + 0, VEC_EPS + 1, 1.0, QO[:, ti, c * 512:(c + 1) * 512], [QOr[ti][c]]))
    self.proj_fm(w, [(O_AK, 64), (O_AK, 64)], lambda ti, c, bk, m: self.head_norm(
        bk, 128, gv + 1, VEC_EPS, 1.0 / 64, None, KA[0].regs + KA[1].regs,
        split=(KA[0].ap[0:64, c * 512:(c + 1) * 512], KA[1].ap[64:128, c * 512:(c + 1) * 512])))

    def cb_i(ti, c, bk, m):
        cs = slice(c * 512, (c + 1) * 512)
        dst = (IQ, IQ3, IK3)[ti]
        self.copy("act" if ti % 2 else "dve", dst.ap[0:m, cs], self.PS[0:m, bk, :], [self.PSR[bk]], dst.regs)
    self.proj_fm(w, [(O_IQ, 128), (O_IK, 32), (O_IK, 32), (O_IK, 32)], cb_i, tiles=[(0, 96), (96, 32), (128, 96)])

    def cb_v(tt, bk, o):
        pr = [self.PSR[bk]]
        self.copy("act", VA.ap[:, tt, 0:64], self.PS[:, bk, 0:64], pr, VA.regs)
        self.copy("dve", VA.ap[:, tt, 64:128], self.PS[:, bk, 0:64], pr, VA.regs)
        self.act(wab.ap[:, tt, :], self.PS[:, bk, 64:68], AF.Abs, pr, wab.regs)
        self.act(wsg.ap[:, tt, :], self.PS[:, bk, 64:68], AF.Sign, pr, wsg.regs)
    self.proj_tm(w, [(O_AV, 64), (O_IW, 4)], cb_v)
    self.loc_cur = mark
    self.attn_bufs()
    score = self.lalloc((T,), F32)
    rt = [self.lalloc((512,), F32) for _ in range(2)]
    mbt = [self.lalloc((T,), BF16) for _ in range(2)]
    strips = [self.lalloc((T,), BF16) for _ in range(2)]
    small = self.lalloc((16,), F32)
    thr = small.ap[:, 0:1]
    cnt = small.ap[:, 1:2]
    g2 = small.ap[:, 2:3]
    anti = self.cT["anti"]
    ident = self.cT["ident"]
    ri = 0
    si = 0
    for qp in range(8):
        for i in range(2):
            qt = 2 * qp + i
            if qt < 2:
                continue
            nk = (qt + 1) * 128
            qsl = slice(qt * 128, (qt + 1) * 128)
            for s0 in range(0, nk, 512):
                sn = min(512, nk - s0)
                for h in range(4):
                    bz = self.bank()
                    if h < 3:
                        lt, rh = IQ.ap[32 * h:32 * h + 32, qsl], IK3.ap[32 * h:32 * h + 32, s0:s0 + sn]
                        rd = IQ.regs + IK3.regs
                    else:
                        lt, rh = IQ3.ap[0:32, qsl], IK3.ap[0:32, s0:s0 + sn]
                        rd = IQ3.regs + IK3.regs
                    self.mm(self.PS[:, bz, 0:sn], lt, rh, True, True, rd, [self.PSR[bz]])
                    r = rt[ri % 2]
                    ri += 1
                    self.act(r.ap[:, 0:sn], self.PS[:, bz, 0:sn], AF.Relu, [self.PSR[bz]] + wab.regs, r.regs,
                             scale=wab.ap[:, qt, h:h + 1])
                    if h == 0:
                        self.ts("dve", score.ap[:, s0:s0 + sn], r.ap[:, 0:sn], wsg.ap[:, qt, 0:1], ALU.mult,
                                r.regs + wsg.regs, score.regs)
                    else:
                        self.stt(score.ap[:, s0:s0 + sn], r.ap[:, 0:sn], wsg.ap[:, qt, h:h + 1], score.ap[:, s0:s0 + sn],
                                 ALU.mult, ALU.add, r.regs + wsg.regs + score.regs, score.regs)
            self.tt("dve", score.ap[:, qsl], score.ap[:, qsl], cm.ap, ALU.add, score.regs + cm.regs, score.regs)
            self.S.op("dve", lambda hh: hh.memset(thr, 0.0), [], small.regs)
            mb = mbt[i]
            for it in range(NIT):
                step = S0 / (2 ** it)
                self.ts("dve", mb.ap[:, 0:nk], score.ap[:, 0:nk], thr, ALU.is_gt, score.regs + small.regs,
                        mb.regs + small.regs, op1=ALU.add, accum=cnt)
                self.ts("dve", g2, cnt, 255.5, ALU.is_ge, small.regs, small.regs, s2=2.0 * step, op1=ALU.mult)
                self.stt(thr, g2, -step, thr, ALU.add, ALU.add, small.regs, small.regs)
            self.ts("dve", thr, thr, -S0 / (2 ** (NIT - 1)), ALU.add, small.regs, small.regs)
            self.ts("dve", mb.ap[:, 0:nk], score.ap[:, 0:nk], thr, ALU.is_le, score.regs + small.regs, mb.regs,
                    s2=NEG, op1=ALU.mult)
        q0 = qp * 256
        for h in range(4):
            j, e = h // 2, h % 2
            rs_ = slice(64 * e, 64 * e + 64)
            st = strips[si % 2]
            self.load_strip(st, self.din["gext"], h, q0 + 256, "strip%d" % (si % 2))
            si += 1

            def qk_fn(sb, qs, n):
                return [(0, n, KA[e].ap[:, sb * 128:(sb + 1) * 128], QO[:, j, qs:qs + n], KA[e].regs + [QOr[j][qp // 2]])]

            def bias_fn(sb, qs, n):
                off = qs - sb * 128
                out = [(0, n, anti, st.ap[:, off:off + n], st.regs + [self.creg])]
                if qp > 0:
                    for i in range(2):
                        t0 = q0 + i * 128
                        if t0 >= qs:
                            out.append((t0 - qs, 128, mbt[i].ap[:, sb * 128:(sb + 1) * 128], ident,
                                        mbt[i].regs + [self.creg]))
                return out

            def v_fn(sb):
                return VA.ap[:, sb, :], VA.regs
            self.attn_seg(q0, 256, 2 * (qp + 1), qk_fn, bias_fn, v_fn, e, QO[rs_, j, q0:q0 + 256], [QOr[j][qp // 2]])


Builder.mix_D = _mix_D
Builder.mix_A = _mix_A


_CACHE = {}


def get_nc(nseq, stages, dbg=None):
    key = (nseq, stages, dbg)
    if key not in _CACHE:
        b = Builder(nseq, stages, dbg)
        _CACHE[key] = b
        b.nc_built = b.build_wrapped()
    return _CACHE[key]


def _build_wrapped(self):
    return self.build()


Builder.build_wrapped = _build_wrapped


def make_inmap(inp):
    shared = {
        "w_ffn_in": np.ascontiguousarray(np.asarray(inp["w_ffn_in"], np.float32)),
        "w_ffn_out": np.ascontiguousarray(np.asarray(inp["w_ffn_out"], np.float32)),
        "vecs": make_vecs(inp),
    }
    for k, v in host_consts().items():
        shared["c_" + k] = v
    for k, v in host_consts2().items():
        shared["c_" + k] = v
    for k in ("w_in", "w_branch", "w_out", "w_mla_uq", "w_mla_ukv"):
        shared[k] = np.ascontiguousarray(np.asarray(inp[k], np.float32))
    shared["gext"], shared["gd"] = make_gext(inp)
    return shared


def kernel(**inp):
    ncores = 8
    nseq = 2
    b = get_nc(nseq, 99)
    x = np.ascontiguousarray(np.asarray(inp["x"], np.float32))
    shared = make_inmap(inp)
    in_maps = []
    for i in range(ncores):
        m = dict(shared)
        m["x"] = x[i * nseq:(i + 1) * nseq]
        in_maps.append(m)
    res = run_bass_kernel_spmd(b.nc_built, in_maps, core_ids=list(range(ncores)))
    return np.concatenate([r["y"] for r in res.results], axis=0)
```

```python
import numpy as np
import math
from contextlib import ExitStack
import concourse.bass as bass
import concourse.mybir as mybir
from concourse.bass_utils import run_bass_kernel_spmd

F32 = mybir.dt.float32
BF16 = mybir.dt.bfloat16
AF = mybir.ActivationFunctionType
ALU = mybir.AluOpType

D = 1024
T = 2048
DFF = 2816
INW = 8388
NCH = 4
NEG = -30000.0
EPS = 1e-6

O_AQ, O_AK, O_AV, O_IQ, O_IK, O_IW = 0, 256, 320, 384, 512, 544
O_BQ, O_BK, O_BV = 548, 804, 1060
O_CQ, O_CKV, O_CKR = 1316, 1700, 1956
O_DQ, O_DK, O_DV = 1988, 2756, 3524
O_G = 4292

PAGE = 1024
ARENA_BYTES = 224000


class Reg:
    __slots__ = ("w", "r", "excl")

    def __init__(self, excl=False):
        self.w = None
        self.r = {}
        self.excl = excl


class TT:
    __slots__ = ("ap", "regs")

    def __init__(self, ap, regs):
        self.ap = ap
        self.regs = regs


ENGS = ["pe", "act", "dve", "pool", "sp"]


class Sched:
    def __init__(self):
        self.ops = {e: [] for e in ENGS}
        self.cnt = {e: 0 for e in ENGS}
        self.waited = {e: {} for e in ENGS}
        self.dma_tot = {}

    def _waits(self, eng, reads, writes, k):
        need = {}
        for r in reads:
            t = r.w
            if t is None:
                continue
            key, val = t
            if key == eng and eng == "pe":
                continue
            if need.get(key, 0) < val:
                need[key] = val
        for w in writes:
            toks = list(w.r.values())
            if w.w is not None:
                toks.append(w.w)
            for key, val in toks:
                if key == eng and eng == "pe":
                    continue
                if need.get(key, 0) < val:
                    need[key] = val
        out = []
        wd = self.waited[eng]
        for key, val in need.items():
            if wd.get(key, 0) >= val:
                continue
            wd[key] = val
            out.append((key, val))
        return out

    def op(self, eng, fn, reads=(), writes=(), mode=None):
        if any(r.excl for r in reads):
            writes = list(writes) + [r for r in reads if r.excl]
            reads = [r for r in reads if not r.excl]
        k = self.cnt[eng] + 1
        waits = self._waits(eng, reads, writes, k)
        self.cnt[eng] = k
        self.ops[eng].append((0, fn, waits, mode))
        tok = (eng, k)
        for r in reads:
            r.r[eng] = tok
        for w in writes:
            w.w = tok
            w.r = {}

    def dma(self, queue, fn, stream, reads=(), writes=()):
        waits = self._waits(queue, reads, writes, 1 << 60)
        tot = self.dma_tot.get(stream, 0) + 16
        self.dma_tot[stream] = tot
        self.ops[queue].append((1, fn, waits, stream))
        key = ("D", stream)
        tok = (key, tot)
        for r in reads:
            r.r[key] = tok
        for w in writes:
            w.w = tok
            w.r = {}

    def final_waits(self, eng, streams):
        waits = []
        for s in streams:
            if s in self.dma_tot:
                waits.append((("D", s), self.dma_tot[s]))
        self.ops[eng].append((2, None, waits, None))

    def emit(self, nc):
        with ExitStack() as es:
            sems = {}
            for e in ENGS:
                sems[e] = es.enter_context(nc.semaphore("s_" + e))
            for i, s in enumerate(self.dma_tot):
                sems[("D", s)] = es.enter_context(nc.semaphore("d%d" % i))
            block = es.enter_context(nc.Block())
            names = {"pe": "tensor", "act": "scalar", "dve": "vector", "pool": "gpsimd", "sp": "sync"}

            def mk(e):
                def body(h):
                    se = sems[e]
                    last_mode = (128, 128, 0)
                    for kind, fn, waits, stream in self.ops[e]:
                        for key, val in waits:
                            h.wait_ge(sems[key], val)
                        if kind == 0:
                            if e == "pe":
                                md = stream if stream is not None else (128, 128, 0)
                                if md != last_mode:
                                    h.drain()
                                    self.ndrain = getattr(self, "ndrain", 0) + 1
                                    last_mode = md
                            fn(h).then_inc(se, 1)
                        elif kind == 1:
                            fn(h).then_inc(sems[("D", stream)], 16)
                return body

            for e in ENGS:
                getattr(block, names[e])(mk(e))


def rel_bucket_np(dist):
    n = np.maximum(dist, 0)
    max_exact = 16
    nf = np.maximum(n, 1).astype(np.float32)
    log_b = max_exact + (np.log(nf / np.float32(max_exact)) / np.float32(math.log(2048 / max_exact))
                         * np.float32(32 - max_exact)).astype(np.int32)
    return np.where(n < max_exact, n, np.minimum(log_b, 31))


def host_consts():
    c = {}
    eye = np.eye(128, dtype=np.float32)
    c["ident"] = eye
    c["anti"] = eye[::-1].copy()
    c["ones"] = np.ones((128, 128), np.float32)
    bd = np.zeros((128, 128), np.float32)
    bd[:64, :64] = 1
    bd[64:, 64:] = 1
    c["bd64"] = bd
    return c


def host_consts2():
    c = {}
    c["sel"] = np.kron(np.eye(32, dtype=np.float32), np.ones((1, 128), np.float32))
    bm = np.zeros((8, 4, 8), np.float32)
    for own in range(8):
        bm[own, :, own:] = -1e30
    c["bmk"] = np.broadcast_to(bm.reshape(1, 256), (128, 256)).copy()
    E = np.zeros((64, 128), np.float32)
    for k in range(64):
        E[k, (k // 32) * 64:(k // 32) * 64 + 64] = 1
    c["E"] = E
    cmm = np.where(np.arange(128)[None, :] <= np.arange(128)[:, None], 0.0, -1e30).astype(np.float32)
    c["cm"] = cmm
    c["F"] = E.T.copy()
    G = np.zeros((64, 64), np.float32)
    G[:32, :32] = 1
    G[32:, 32:] = 1
    c["G"] = G
    rot = np.zeros((64, 64), np.float32)
    for b in range(2):
        for m in range(16):
            rot[b * 32 + m + 16, b * 32 + m] = -1.0
            rot[b * 32 + m, b * 32 + m + 16] = 1.0
    c["rot"] = rot
    freqs = 10000.0 ** (-np.arange(16, dtype=np.float32) / 16)
    ang = np.arange(T, dtype=np.float32)[None, :] * np.tile(freqs, 4)[:, None].astype(np.float32)
    c["cos"] = np.cos(ang).astype(np.float32)
    c["sin"] = np.sin(ang).astype(np.float32)
    return c


class Builder:
    def __init__(self, nseq=2, stages=99, dbg=None):
        self.nseq = nseq
        self.stages = stages
        self.dbg = dbg
        self.S = Sched()
        self.nc = bass.Bass("TRN2", target_bir_lowering=False, dynamic_dma_scratch_size=4096)
        self.din = {}
        self.wslot_i = 0
        self.bank_i = 0
        self.bankA_i = 0

    def dram_in(self, name, shape, dt=F32):
        t = self.nc.dram_tensor(name, list(shape), dt, kind="ExternalInput").ap()
        self.din[name] = t
        return t

    def carve(self, off, free_shape, dt):
        n = 1
        for s in free_shape:
            n *= s
        nb = n * (4 if dt == F32 else 2)
        assert off % 4 == 0 and off + nb <= ARENA_BYTES, (off, nb)
        ap = self.arena[:, off // 2:(off + nb) // 2]
        if dt == F32:
            ap = ap.bitcast(F32)
        if len(free_shape) == 2:
            ap = ap.rearrange("p (a b) -> p a b", a=free_shape[0])
        elif len(free_shape) == 3:
            ap = ap.rearrange("p (a b c) -> p a b c", a=free_shape[0], b=free_shape[1])
        return ap, nb

    def lalloc(self, free_shape, dt):
        ap, nb = self.carve(self.loc_base + self.loc_cur, free_shape, dt)
        p0 = self.loc_cur // PAGE
        self.loc_cur += (nb + PAGE - 1) // PAGE * PAGE
        p1 = self.loc_cur // PAGE
        assert self.loc_base + self.loc_cur <= ARENA_BYTES, ("local overflow", self.loc_cur)
        return TT(ap, self.loc_pages[p0:p1])

    def lreset(self):
        self.loc_cur = 0

    def bank(self):
        b = self.bank_i
        self.bank_i = (b + 1) % 4
        return b

    def bankA(self):
        b = self.bankA_i
        self.bankA_i = (b + 1) % 4
        return 4 + b

    def mm(self, out, lhsT, rhs, start, stop, reads, writes):
        ru = lambda v: 32 if v <= 32 else (64 if v <= 64 else 128)
        kk = lhsT.shape[0]
        mmm = 1
        for d in lhsT.shape[1:]:
            mmm *= d
        self.S.op("pe", lambda h: h.matmul(out, lhsT, rhs, start=start, stop=stop), reads, writes,
                  mode=(ru(kk), ru(mmm), lhsT.offset // (lhsT.tensor.shape[1] * 32) if kk < 128 else 0))

    def act(self, out, in_, func, reads, writes, scale=1.0, bias=0.0, accum=None):
        if accum is None:
            self.S.op("act", lambda h: h.activation(out=out, in_=in_, func=func, scale=scale, bias=bias),
                      reads, writes)
        else:
            self.S.op("act", lambda h: h.activation(out=out, in_=in_, func=func, scale=scale, bias=bias,
                                                      accum_out=accum), reads, writes)

    def ts(self, eng, out, in0, s1, op0, reads, writes, s2=None, op1=None, accum=None):
        def fn(h):
            kw = {}
            if op1 is not None:
                kw["op1"] = op1
            if accum is not None:
                kw["accum_out"] = accum
            return h.tensor_scalar(out=out, in0=in0, scalar1=s1, scalar2=s2, op0=op0, **kw)
        self.S.op(eng, fn, reads, writes)

    def stt(self, out, in0, scalar, in1, op0, op1, reads, writes):
        self.S.op("dve", lambda h: h.scalar_tensor_tensor(out=out, in0=in0, scalar=scalar, in1=in1,
                                                            op0=op0, op1=op1), reads, writes)

    def tt(self, eng, out, in0, in1, op, reads, writes):
        self.S.op(eng, lambda h: h.tensor_tensor(out=out, in0=in0, in1=in1, op=op), reads, writes)

    def copy(self, eng, out, in_, reads, writes):
        if eng == "act":
            self.S.op("act", lambda h: h.copy(out=out, in_=in_), reads, writes)
        else:
            self.S.op(eng, lambda h: h.tensor_copy(out=out, in_=in_), reads, writes)

    def recip(self, out, in_, reads, writes):
        self.S.op("dve", lambda h: h.reciprocal(out=out, in_=in_), reads, writes)

    def dma(self, queue, out, in_, stream, reads, writes):
        if queue == "pool":
            self.S.dma(queue, lambda h: h.dma_start(out=out, in_=in_, max_dma_last_dim=2048), stream, reads, writes)
        else:
            self.S.dma(queue, lambda h: h.dma_start(out=out, in_=in_), stream, reads, writes)

    def wload(self, pieces, nk):
        s = self.wslot_i
        self.wslot_i = (s + 1) % len(self.wslots)
        ap_full, reg = self.wslots[s]
        tot = max(o + n for _, o, n in pieces)
        assert nk * tot * 2 <= 8192, (nk, tot)
        view = ap_full[:, 0:nk * tot].rearrange("p (k c) -> p k c", k=nk)
        for src, o, n in pieces:
            srcv = src.rearrange("(k p) c -> p k c", p=128)
            self.dma("pool", view[:, :, o:o + n], srcv, ("w", s), [], [reg])
        return view, [reg]

    def build(self):
        nc = self.nc
        ns = self.nseq
        x_in = self.dram_in("x", (ns, T, D))
        wfi = self.dram_in("w_ffn_in", (2, 2, D, 2 * DFF))
        wfo = self.dram_in("w_ffn_out", (2, 2, DFF, D))
        vecs = self.dram_in("vecs", (128, NVEC))
        self.w_in = self.dram_in("w_in", (2, D, INW))
        self.w_branch = self.dram_in("w_branch", (2, 4, 256, D))
        self.w_out = self.dram_in("w_out", (2, D, D))
        self.w_uq = self.dram_in("w_mla_uq", (2, 384, 384))
        self.w_ukv = self.dram_in("w_mla_ukv", (2, 256, 512))
        self.dram_in("gext", (9, NG))
        self.dram_in("gd", (12, NGD))
        for k, v in host_consts2().items():
            self.dram_in("c_" + k, v.shape)
        if self.dbg:
            self.dbg_out = self.nc.dram_tensor("dbg", [4, 128, 2, T], BF16, kind="ExternalOutput").ap()
        cst = {k: self.dram_in("c_" + k, v.shape) for k, v in host_consts().items()}
        self.y_out = nc.dram_tensor("y", [ns, T, D], F32, kind="ExternalOutput").ap()
        with ExitStack() as es:
            arena_t = es.enter_context(nc.sbuf_tensor("arena", [128, ARENA_BYTES // 2], BF16))
            self.arena = arena_t[:, :]
            ps_t = es.enter_context(nc.psum_tensor("ps", [128, 8, 512], F32))
            self.PS = ps_t
            self.PSR = [Reg(excl=True) for _ in range(8)]
            off = 0
            self.xT, nb = self.carve(off, (8, T), F32); off += nb
            self.xTr = [[Reg() for _ in range(NCH)] for _ in range(8)]
            self.hT, nb = self.carve(off, (8, T), BF16); off += nb
            self.hTr = [[Reg() for _ in range(NCH)] for _ in range(8)]
            self.QO = []
            self.QOr = []
            for n in range(4):
                q, nb = self.carve(off, (2, T), BF16); off += nb
                self.QO.append(q)
                self.QOr.append([[Reg() for _ in range(NCH)] for _ in range(2)])
            self.wslots = []
            for s in range(3):
                w, nb = self.carve(off, (4096,), BF16); off += nb
                self.wslots.append((w, Reg()))
            self.cT = {}
            creg = Reg()
            self.creg = creg
            for k, v in host_consts().items():
                ap, nb = self.carve(off, (v.shape[1],), BF16); off += nb
                self.cT[k] = ap
                self.dma("pool", ap, cst[k], "const", [], [creg])
            ap, nb = self.carve(off, (128,), F32); off += nb
            self.identf = ap
            self.dma("sp", ap, cst["ident"], "constf", [], [creg])
            ap, nb = self.carve(off, (NVEC,), F32); off += nb
            self.vecs = ap
            self.eps_ap = ap[:, VEC_EPS:VEC_EPS + 1]
            self.dma("sp", ap, vecs, "constf", [], [creg])
            off = (off + PAGE - 1) // PAGE * PAGE
            self.loc_base = off
            npages = (ARENA_BYTES - off) // PAGE
            self.loc_pages = [Reg() for _ in range(npages)]
            self.loc_cur = 0
            print("persistent bytes", off, "local pages", npages)

            for b in range(ns):
                self.load_x(x_in, b)
                st = 0
                for l in range(2):
                    for f in range(2):
                        if f == 1:
                            if st < self.stages:
                                self.mixer(l)
                                if self.dbg:
                                    st = 1000
                            st += 1
                        if st < self.stages:
                            self.ffn(wfi[l, f], wfo[l, f], VEC_NG + (l * 3 + (0 if f == 0 else 2)) * 8)
                        st += 1
                self.store_x(b)
            self.S.final_waits("sp", ["out0", "out1"])
            self.S.emit(nc)
        return nc

    def load_x(self, x_in, b):
        self.lreset()
        stg = [self.lalloc((4, D), F32) for _ in range(2)]
        for c in range(NCH):
            s = stg[c % 2]
            src = x_in[b, c * 512:(c + 1) * 512, :].rearrange("(a p) d -> p a d", p=128)
            self.dma("sp", s.ap, src, "xin%d" % (c % 2), [], s.regs)
            for j in range(8):
                bk = self.bank()
                for a in range(4):
                    o = self.PS[:, bk, a * 128:(a + 1) * 128]
                    i = s.ap[:, a, j * 128:(j + 1) * 128]
                    self.S.op("pe", lambda h, o=o, i=i: h.transpose(o, i, self.identf),
                              s.regs + [self.creg], [self.PSR[bk]])
                dst = self.xT[:, j, c * 512:(c + 1) * 512]
                self.copy("act" if j % 2 else "dve", dst, self.PS[:, bk, :], [self.PSR[bk]], [self.xTr[j][c]])

    def store_x(self, b):
        self.lreset()
        stg = [self.lalloc((4, D), F32) for _ in range(2)]
        for c in range(NCH):
            s = stg[c % 2]
            for a in range(4):
                for jj in range(2):
                    bk = self.bank()
                    for j4 in range(4):
                        j = jj * 4 + j4
                        o = self.PS[:, bk, j4 * 128:(j4 + 1) * 128]
                        i = self.xT[:, j, c * 512 + a * 128: c * 512 + (a + 1) * 128]
                        self.S.op("pe", lambda h, o=o, i=i: h.transpose(o, i, self.identf),
                                  [self.xTr[j][c], self.creg], [self.PSR[bk]])
                    dst = s.ap[:, a, jj * 512:(jj + 1) * 512]
                    self.copy("act" if jj % 2 else "dve", dst, self.PS[:, bk, :], [self.PSR[bk]], s.regs)
            dstd = self.y_out[b, c * 512:(c + 1) * 512, :].rearrange("(a p) d -> p a d", p=128)
            self.dma("sp", dstd, s.ap, "out%d" % (c % 2), s.regs, [])

    def rmsnorm_x(self, gain_col):
        sq = [self.lalloc((512,), BF16) for _ in range(2)]
        rs = self.lalloc((512,), F32)
        ones = self.cT["ones"]
        for c in range(NCH):
            cs = slice(c * 512, (c + 1) * 512)
            bk = self.bank()
            for j in range(8):
                q = sq[j % 2]
                self.act(q.ap, self.xT[:, j, cs], AF.Square, [self.xTr[j][c]], q.regs)
                self.mm(self.PS[:, bk, :], ones, q.ap, j == 0, j == 7, q.regs + [self.creg], [self.PSR[bk]])
            self.act(rs.ap, self.PS[:, bk, :], AF.Ln, [self.PSR[bk]], rs.regs, scale=1.0 / D, bias=self.eps_ap)
            self.act(rs.ap, rs.ap, AF.Exp, rs.regs, rs.regs, scale=-0.5)
            for j in range(8):
                g = self.vecs[:, gain_col + j:gain_col + j + 1]
                self.stt(self.hT[:, j, cs], self.xT[:, j, cs], g, rs.ap, ALU.mult, ALU.mult,
                         [self.xTr[j][c], self.creg] + rs.regs, [self.hTr[j][c]])

    def ffn(self, w_in, w_out, gain_col):
        self.lreset()
        self.rmsnorm_x(gain_col)
        gT = self.lalloc((8, T), BF16)
        gp = lambda i, c: gT.regs[i * 4 + c: i * 4 + c + 1]
        sl = [self.lalloc((512,), F32) for _ in range(2)]
        sli = 0
        for (g0, nf) in ((0, 8), (8, 8), (16, 6)):
            for fb in range(0, nf, 2):
                f0 = (g0 + fb) * 128
                wv, wr = self.wload([(w_in[:, f0:f0 + 256], 0, 256),
                                     (w_in[:, DFF + f0:DFF + f0 + 256], 256, 256)], 8)
                for c in range(NCH):
                    cs = slice(c * 512, (c + 1) * 512)
                    for ft in range(2):
                        bg = self.bank()
                        bu = self.bankA()
                        for k in range(8):
                            self.mm(self.PS[:, bg, :], wv[:, k, ft * 128:(ft + 1) * 128], self.hT[:, k, cs],
                                    k == 0, k == 7, wr + [self.hTr[k][c]], [self.PSR[bg]])
                        for k in range(8):
                            self.mm(self.PS[:, bu, :], wv[:, k, 256 + ft * 128:256 + (ft + 1) * 128],
                                    self.hT[:, k, cs], k == 0, k == 7, wr + [self.hTr[k][c]], [self.PSR[bu]])
                        s = sl[sli % 2]
                        sli += 1
                        self.act(s.ap, self.PS[:, bg, :], AF.Silu, [self.PSR[bg]], s.regs)
                        self.tt("dve", gT.ap[:, fb + ft, cs], s.ap, self.PS[:, bu, :], ALU.mult,
                                s.regs + [self.PSR[bu]], gp(fb + ft, c))
            for dq in range(2):
                wv, wr = self.wload([(w_out[g0 * 128:(g0 + nf) * 128, dq * 512:(dq + 1) * 512], 0, 512)], nf)
                for c in range(NCH):
                    cs = slice(c * 512, (c + 1) * 512)
                    for dt in range(4):
                        d = dq * 4 + dt
                        bk = self.bank()
                        for i in range(nf):
                            self.mm(self.PS[:, bk, :], wv[:, i, dt * 128:(dt + 1) * 128], gT.ap[:, i, cs],
                                    i == 0, i == nf - 1, wr + gp(i, c), [self.PSR[bk]])
                        self.stt(self.xT[:, d, cs], self.PS[:, bk, :], 0.5, self.xT[:, d, cs], ALU.mult, ALU.add,
                                 [self.PSR[bk], self.xTr[d][c]], [self.xTr[d][c]])


VEC_NG = 0
VEC_EPS = 48
VEC_BG = 56
VEC_QK = 120
NVEC = 160
NG = 2175
NGD = 383


def make_vecs(inp):
    v = np.zeros((128, NVEC), np.float32)
    ng = np.asarray(inp["norm_gain"], np.float32)
    v[:, VEC_NG:VEC_NG + 48] = ng.reshape(6, 8, 128).transpose(2, 0, 1).reshape(128, 48)
    v[:, VEC_EPS] = EPS
    v[:, VEC_EPS + 1] = EPS * 64
    v[:, VEC_EPS + 2] = EPS * 96
    bg = np.asarray(inp["b_gate"], np.float32)
    v[:, VEC_BG:VEC_BG + 64] = bg.reshape(8, 8, 128).transpose(2, 0, 1).reshape(128, 64)
    t2 = lambda a: np.concatenate([a, a])
    for l in range(2):
        o = VEC_QK + l * 16
        v[:, o + 0] = t2(np.asarray(inp["qk_gain_a"])[l, 0])
        v[:, o + 1] = t2(np.asarray(inp["qk_gain_a"])[l, 1])
        v[:, o + 2] = t2(np.asarray(inp["qk_gain_b"])[l, 0])
        v[:, o + 3] = t2(np.asarray(inp["qk_gain_b"])[l, 1])
        v[:, o + 4] = t2(np.asarray(inp["qk_gain_d"])[l, 0])
        v[:, o + 5] = t2(np.asarray(inp["qk_gain_d"])[l, 1])
        qc = np.asarray(inp["qk_gain_c"], np.float32)
        v[:, o + 6] = t2(qc[l, 0, :64])
        v[:, o + 7] = t2(qc[l, 1, :64])
        v[:64, o + 8] = t2(qc[l, 0, 64:])
        v[:64, o + 9] = t2(qc[l, 1, 64:])
        v[:, o + 10:o + 13] = np.asarray(inp["mla_norm_q"], np.float32)[l].reshape(3, 128).T
        v[:, o + 13:o + 15] = np.asarray(inp["mla_norm_kv"], np.float32)[l].reshape(2, 128).T
    return v


def make_gext(inp):
    rb = np.asarray(inp["rel_bias"], np.float32)
    dist = np.arange(NG) - 127
    bk = rel_bucket_np(dist)
    g = np.full((9, NG), NEG, np.float32)
    for h in range(8):
        g[h, 127:] = rb[h, bk[127:]]
    g[8, 127:] = 0.0
    gd = np.full((12, NGD), NEG, np.float32)
    dd = np.arange(NGD) - 127
    for gi, dil in enumerate((1, 4, 16)):
        ok = (dd >= 0) & (dd <= 128)
        b2 = rel_bucket_np(dd * dil)
        for h in range(4):
            gd[gi * 4 + h, ok] = rb[8 + gi * 4 + h, b2[ok]]
    return g, gd


def _hn_alloc(self):
    self.hn_sq = [self.lalloc((512,), BF16) for _ in range(2)]
    self.hn_tmp = [self.lalloc((512,), F32) for _ in range(2)]
    self.hn_rs = self.lalloc((512,), F32)
    self.hn_i = 0


def _vcol(self, c):
    return self.vecs[:, c:c + 1]


def _proj_fm(self, w, pieces, cb, tiles=None):
    sp = []
    o = 0
    for c0, n in pieces:
        sp.append((w[:, c0:c0 + n], o, n))
        o += n
    wv, wr = self.wload(sp, 8)
    if tiles is None:
        tiles = [(i * 128, min(128, o - i * 128)) for i in range((o + 127) // 128)]
    for c in range(NCH):
        cs = slice(c * 512, (c + 1) * 512)
        for ti, (tc0, m) in enumerate(tiles):
            bk = self.bank()
            for k in range(8):
                self.mm(self.PS[0:m, bk, :], wv[:, k, tc0:tc0 + m], self.hT[:, k, cs], k == 0, k == 7,
                        wr + [self.hTr[k][c]], [self.PSR[bk]])
            cb(ti, c, bk, m)


def _proj_tm(self, w, pieces, cb, tok_ap=None):
    sp = []
    o = 0
    for c0, n in pieces:
        sp.append((w[:, c0:c0 + n], o, n))
        o += n
    wv, wr = self.wload(sp, 8)
    for tt in range(16):
        bk = self.bank()
        for k in range(8):
            if tok_ap is None:
                lt = self.hT[:, k, tt * 128:(tt + 1) * 128]
                rd = [self.hTr[k][tt // 4]]
            else:
                lt, rd = tok_ap(k, tt)
            self.mm(self.PS[:, bk, 0:o], lt, wv[:, k, 0:o], k == 0, k == 7, wr + rd, [self.PSR[bk]])
        cb(tt, bk, o)


def _head_norm(self, bk, m, gain_col, eps_col, ss_scale, dst_ap, dst_regs, blk="bd64", split=None):
    i = self.hn_i
    self.hn_i += 1
    sq = self.hn_sq[i % 2]
    tmp = self.hn_tmp[i % 2]
    rs = self.hn_rs
    pr = [self.PSR[bk]]
    self.act(sq.ap[0:m, :], self.PS[0:m, bk, :], AF.Square, pr, sq.regs)
    self.copy("act", tmp.ap[0:m, :], self.PS[0:m, bk, :], pr, tmp.regs)
    b2 = self.bankA()
    self.mm(self.PS[0:m, b2, :], self.cT[blk][0:m, 0:m], sq.ap[0:m, :], True, True, sq.regs + [self.creg], [self.PSR[b2]])
    self.act(rs.ap[0:m, :], self.PS[0:m, b2, :], AF.Ln, [self.PSR[b2]], rs.regs, scale=ss_scale,
             bias=self.vecs[0:m, eps_col:eps_col + 1])
    self.act(rs.ap[0:m, :], rs.ap[0:m, :], AF.Exp, rs.regs, rs.regs, scale=-0.5)
    if split is not None:
        for e, d in enumerate(split):
            r = slice(64 * e, 64 * e + 64)
            self.stt(d, tmp.ap[r, :], self.vecs[r, gain_col:gain_col + 1], rs.ap[r, :], ALU.mult, ALU.mult,
                     tmp.regs + rs.regs + [self.creg], dst_regs)
        return
    self.stt(dst_ap, tmp.ap[0:m, :], self.vecs[0:m, gain_col:gain_col + 1], rs.ap[0:m, :], ALU.mult, ALU.mult,
             tmp.regs + rs.regs + [self.creg], dst_regs)


def _load_strip(self, dst, src_dram, row, ncols, stream):
    base = src_dram[row:row + 1, 0:ncols]
    ap = bass.AP(tensor=base.tensor, offset=base.offset, ap=[[1, 128], [1, ncols]])
    self.dma("pool", dst.ap[:, 0:ncols], ap, stream, [], dst.regs)


def _attn_seg(self, q0, nq, nsb, qk_fn, bias_fn, v_fn, e, dst_ap, dst_regs):
    bo = self.bankA()
    bd = self.bankA()
    ones = self.cT["ones"]
    for i0 in range(0, nsb, 2):
        info = []
        for sb in range(i0, min(nsb, i0 + 2)):
            qs = max(q0, sb * 128)
            n = q0 + nq - qs
            c0 = qs - q0
            bs = self.bank()
            info.append((sb, n, c0, bs, qk_fn(sb, qs, n) + bias_fn(sb, qs, n)))
        for idx in range(max(len(x[4]) for x in info)):
            for (sb, n, c0, bs, mms) in info:
                if idx < len(mms):
                    o0, on, lt, rh, rd = mms[idx]
                    self.mm(self.PS[:, bs, o0:o0 + on], lt, rh, idx == 0, idx == len(mms) - 1, rd, [self.PSR[bs]])
        pl = []
        for (sb, n, c0, bs, mms) in info:
            p = self.Pb[self.pi % 2]
            self.pi += 1
            self.act(p.ap[:, 0:n], self.PS[:, bs, 0:n], AF.Exp, [self.PSR[bs]], p.regs)
            pl.append(p)
        for (sb, n, c0, bs, mms), p in zip(info, pl):
            vl, vr = v_fn(sb)
            self.mm(self.PS[:, bo, c0:c0 + n], vl, p.ap[:, 0:n], sb == 0, sb == nsb - 1, vr + p.regs, [self.PSR[bo]])
            self.mm(self.PS[:, bd, c0:c0 + n], ones, p.ap[:, 0:n], sb == 0, sb == nsb - 1, p.regs + [self.creg],
                    [self.PSR[bd]])
    r = slice(64 * e, 64 * e + 64)
    rc = self.rcb
    self.act(rc.ap[r, 0:nq], self.PS[r, bd, 0:nq], AF.Ln, [self.PSR[bd]], rc.regs)
    self.act(rc.ap[r, 0:nq], rc.ap[r, 0:nq], AF.Exp, rc.regs, rc.regs, scale=-1.0)
    self.tt("dve", dst_ap, self.PS[r, bo, 0:nq], rc.ap[r, 0:nq], ALU.mult, [self.PSR[bo]] + rc.regs, dst_regs)


def _attn_bufs(self):
    self.Pb = [self.lalloc((512,), BF16) for _ in range(2)]
    self.pi = 0
    self.rcb = self.lalloc((512,), F32)


def _mix_B(self, l):
    w = self.w_in[l]
    QO, QOr = self.QO[1], self.QOr[1]
    self.lreset()
    KB = self.lalloc((2, T), BF16)
    KZ = [self.lalloc((T,), BF16) for _ in range(4)]
    VB = self.lalloc((16, 256), BF16)
    maskT = self.lalloc((T,), BF16)
    SEL = self.lalloc((4096,), BF16)
    for kz in KZ:
        self.S.op("dve", lambda hh, kz=kz: hh.memset(kz.ap, 0.0), [], kz.regs)
    self.S.op("dve", lambda hh: hh.memset(maskT.ap, 0.0), [], maskT.regs)
    self.S.op("dve", lambda hh: hh.memset(SEL.ap, 0.0), [], SEL.regs)
    self.dma("pool", SEL.ap[0:32, :], self.din["c_sel"], "misc0", [], SEL.regs)
    bmk = self.lalloc((256,), F32)
    self.dma("sp", bmk.ap, self.din["c_bmk"], "misc1", [], bmk.regs)
    small = self.lalloc((256,), F32)
    kms = self.lalloc((2, 8), BF16)
    mb = self.lalloc((32,), BF16)
    mark = self.loc_cur
    self.hn_alloc()
    gq = VEC_QK + l * 16
    self.proj_fm(w, [(O_BQ, 256)], lambda ti, c, bk, m: self.head_norm(
        bk, 128, gq + 2, VEC_EPS + 1, 1.0, QO[:, ti, c * 512:(c + 1) * 512], [QOr[ti][c]]))
    def cb_bk(ti, c, bk, m):
        cs = slice(c * 512, (c + 1) * 512)
        self.head_norm(bk, 128, gq + 3, VEC_EPS, 1.0 / 64, None, KB.regs + KZ[2 * ti].regs + KZ[2 * ti + 1].regs,
                       split=(KZ[2 * ti].ap[0:64, cs], KZ[2 * ti + 1].ap[64:128, cs]))
        self.copy("act", KB.ap[0:64, ti, cs], KZ[2 * ti].ap[0:64, cs], KZ[2 * ti].regs, KB.regs)
        self.copy("act", KB.ap[64:128, ti, cs], KZ[2 * ti + 1].ap[64:128, cs], KZ[2 * ti + 1].regs, KB.regs)
    self.proj_fm(w, [(O_BK, 256)], cb_bk)
    self.proj_tm(w, [(O_BV, 256)], lambda tt, bk, o: self.copy(
        "act", VB.ap[:, tt, :], self.PS[:, bk, 0:256], [self.PSR[bk]], VB.regs))
    import os
    CUT = int(os.environ.get("BCUT", "9"))
    if CUT <= 1:
        return
    for j in range(2):
        for n in range(8):
            o = small.ap[:, j * 8 + n:j * 8 + n + 1]
            i = KB.ap[:, j, n * 256:(n + 1) * 256]
            self.S.op("dve", lambda h, o=o, i=i: h.reduce_sum(out=o, in_=i, axis=mybir.AxisListType.X),
                      KB.regs, small.regs)
    self.copy("dve", kms.ap, small.ap[:, 0:16].rearrange("p (a b) -> p a b", a=2), small.regs, kms.regs)
    gm = small.ap[:, 16:48]
    mx = small.ap[:, 48:80]
    C2 = int(os.environ.get("BCUT2", "9"))
    if C2 <= 0:
        return
    for qt in range(16):
        own = qt // 2
        bk = self.bank()
        for h in range(4):
            j, e = h // 2, h % 2
            self.mm(self.PS[:, bk, h * 8:(h + 1) * 8], QO[64 * e:64 * e + 64, j, qt * 128:(qt + 1) * 128],
                    kms.ap[64 * e:64 * e + 64, j, :], True, True, [QOr[j][qt // 4]] + kms.regs, [self.PSR[bk]])
        self.tt("dve", gm, self.PS[:, bk, 0:32], bmk.ap[:, own * 32:(own + 1) * 32], ALU.add,
                [self.PSR[bk]] + bmk.regs, small.regs)
        if C2 <= 1:
            continue
        for h in range(4):
            o = mx[:, h * 8:(h + 1) * 8]
            i = gm[:, h * 8:(h + 1) * 8]
            self.S.op("dve", lambda hh, o=o, i=i: hh.max(out=o, in_=i), small.regs, small.regs)
        if C2 <= 2:
            continue
        for h in range(4):
            self.ts("dve", mb.ap[:, h * 8:(h + 1) * 8], gm[:, h * 8:(h + 1) * 8], mx[:, h * 8 + 2:h * 8 + 3],
                    ALU.is_lt, small.regs, mb.regs, s2=NEG, op1=ALU.mult)
        if C2 <= 3:
            continue
        mv = mb.ap.rearrange("p (a b) -> p a b", a=4)[:, :, own:8]
        self.S.op("dve", lambda hh, mv=mv: hh.memset(mv, 0.0), [], mb.regs)
        if C2 <= 4:
            continue
        bt = self.bank()
        po = self.PS[0:32, bt, 0:128]
        self.mm(po, mb.ap, self.cT["ident"], True, True, mb.regs + [self.creg], [self.PSR[bt]])
        if C2 <= 5:
            continue
        self.copy("act", maskT.ap[0:32, qt * 128:(qt + 1) * 128], po, [self.PSR[bt]], maskT.regs)
    if CUT <= 2:
        return
    self.loc_cur = mark
    self.attn_bufs()
    strips = [self.lalloc((T,), BF16) for _ in range(2)]
    anti = self.cT["anti"]
    for h in range(4):
        if CUT <= 3 and h >= 1:
            break
        j, e = h // 2, h % 2
        st = strips[h % 2]
        self.load_strip(st, self.din["gext"], 4 + h, T, "strip%d" % (h % 2))
        rs_ = slice(64 * e, 64 * e + 64)
        for c in range(NCH):
            def qk_fn(sb, qs, n):
                return [(0, n, KZ[h].ap[:, sb * 128:(sb + 1) * 128], QO[:, j, qs:qs + n],
                         KZ[h].regs + [QOr[j][c]])]

            def bias_fn(sb, qs, n):
                off = qs - sb * 128
                nb = sb // 2
                return [(0, n, anti, st.ap[:, off:off + n], st.regs + [self.creg]),
                        (0, n, SEL.ap[:, (h * 8 + nb) * 128:(h * 8 + nb + 1) * 128], maskT.ap[:, qs:qs + n],
                         SEL.regs + maskT.regs)]

            def v_fn(sb):
                return VB.ap[:, sb, j * 128:(j + 1) * 128], VB.regs
            self.attn_seg(c * 512, 512, 4 * (c + 1), qk_fn, bias_fn, v_fn, e,
                          QO[rs_, j, c * 512:(c + 1) * 512], [QOr[j][c]])


for _n, _f in list(globals().items()):
    if _n.startswith("_") and callable(_f) and _n[1:] in (
            "hn_alloc", "vcol", "proj_fm", "proj_tm", "head_norm", "load_strip", "attn_seg", "attn_bufs", "mix_B"):
        setattr(Builder, _n[1:], _f)

def _merge(self, l):
    w = self.w_in[l]
    wbr = self.w_branch[l]
    wo = self.w_out[l]
    self.lreset()
    mT = self.lalloc((8, T), BF16)
    mp = lambda d, c: mT.regs[d * 4 + c:d * 4 + c + 1]
    gs = [self.lalloc((512,), F32) for _ in range(2)]
    acc = [self.lalloc((512,), F32) for _ in range(2)]
    tmp = [self.lalloc((512,), F32) for _ in range(2)]
    gi = 0
    for dp in range(4):
        d0 = dp * 256
        wg = []
        for half in range(2):
            wg.append(self.wload([(w[:, O_G + (2 * half) * 1024 + d0:O_G + (2 * half) * 1024 + d0 + 256], 0, 256),
                                  (w[:, O_G + (2 * half + 1) * 1024 + d0:O_G + (2 * half + 1) * 1024 + d0 + 256], 256, 256)], 8))
        wb, wbr_r = self.wload([(wbr[n, :, d0:d0 + 256], n * 256, 256) for n in range(4)], 2)
        for c in range(NCH):
            cs = slice(c * 512, (c + 1) * 512)
            for dt in range(2):
                d = dp * 2 + dt
                a = acc[(c * 2 + dt) % 2]
                for n in range(4):
                    wv, wr = wg[n // 2]
                    bg = self.bank()
                    for k in range(8):
                        self.mm(self.PS[:, bg, :], wv[:, k, (n % 2) * 256 + dt * 128:(n % 2) * 256 + (dt + 1) * 128],
                                self.hT[:, k, cs], k == 0, k == 7, wr + [self.hTr[k][c]], [self.PSR[bg]])
                    g = gs[gi % 2]
                    t = tmp[gi % 2]
                    gi += 1
                    self.act(g.ap, self.PS[:, bg, :], AF.Sigmoid, [self.PSR[bg]], g.regs,
                             bias=self.vcol(VEC_BG + (l * 4 + n) * 8 + d))
                    bm = self.bankA()
                    for jj in range(2):
                        self.mm(self.PS[:, bm, :], wb[:, jj, n * 256 + dt * 128:n * 256 + (dt + 1) * 128],
                                self.QO[n][:, jj, cs], jj == 0, jj == 1, wbr_r + [self.QOr[n][jj][c]], [self.PSR[bm]])
                    if n == 0:
                        self.tt("dve", a.ap, g.ap, self.PS[:, bm, :], ALU.mult, g.regs + [self.PSR[bm]], a.regs)
                    else:
                        self.tt("dve", t.ap, g.ap, self.PS[:, bm, :], ALU.mult, g.regs + [self.PSR[bm]], t.regs)
                        if n < 3:
                            self.tt("dve", a.ap, a.ap, t.ap, ALU.add, a.regs + t.regs, a.regs)
                        else:
                            self.tt("dve", mT.ap[:, d, cs], a.ap, t.ap, ALU.add, a.regs + t.regs, mp(d, c))
    for dq in range(2):
        wv, wr = self.wload([(wo[:, dq * 512:(dq + 1) * 512], 0, 512)], 8)
        for c in range(NCH):
            cs = slice(c * 512, (c + 1) * 512)
            for dt in range(4):
                d = dq * 4 + dt
                bk = self.bank()
                for k in range(8):
                    self.mm(self.PS[:, bk, :], wv[:, k, dt * 128:(dt + 1) * 128], mT.ap[:, k, cs], k == 0, k == 7,
                            wr + mp(k, c), [self.PSR[bk]])
                self.tt("dve", self.xT[:, d, cs], self.PS[:, bk, :], self.xT[:, d, cs], ALU.add,
                        [self.PSR[bk], self.xTr[d][c]], [self.xTr[d][c]])


def _mixer(self, l):
    self.lreset()
    self.rmsnorm_x(VEC_NG + (l * 3 + 1) * 8)
    en = self.dbg if self.dbg else "ABCD"
    if "A" in en:
        self.mix_A(l)
    if "B" in en:
        self.mix_B(l)
    if "C" in en:
        self.mix_C(l)
    if "D" in en:
        self.mix_D(l)
    if self.dbg:
        for n in range(4):
            if "ABCD"[n] in en:
                rr = [r for pr in self.QOr[n] for r in pr]
                self.dma("sp", self.dbg_out[n], self.QO[n], "out0", rr, [])
        return
    self.merge(l)


Builder.merge = _merge
Builder.mixer = _mixer


def _mix_C(self, l):
    w = self.w_in[l]
    QO, QOr = self.QO[2], self.QOr[2]
    self.lreset()
    QR = self.lalloc((2, T), BF16)
    KC = self.lalloc((2, T), BF16)
    KR = self.lalloc((2, T), BF16)
    VC = self.lalloc((16, 256), BF16)
    Em = self.lalloc((128,), BF16)
    Fm = self.lalloc((64,), BF16)
    Gm = self.lalloc((64,), BF16)
    ROT = self.lalloc((64,), F32)
    stc = self.lalloc((512,), BF16)
    self.dma("pool", Em.ap[0:64, :], self.din["c_E"], "cE", [], Em.regs)
    self.dma("pool", Fm.ap, self.din["c_F"], "cF", [], Fm.regs)
    self.dma("pool", Gm.ap[0:64, :], self.din["c_G"], "cG", [], Gm.regs)
    self.dma("sp", ROT.ap[0:64, :], self.din["c_rot"], "cROT", [], ROT.regs)
    self.load_strip(stc, self.din["gext"], 8, 512, "strip0")
    mark = self.loc_cur
    cqg = self.lalloc((3, 512), BF16)
    rl = self.lalloc((512,), F32)
    u_n = self.lalloc((512,), F32)
    u_r = self.lalloc((512,), F32)
    u_k = self.lalloc((512,), F32)
    sq_n = self.lalloc((512,), BF16)
    sq_r = self.lalloc((512,), BF16)
    rs_n = self.lalloc((512,), F32)
    rs_r = self.lalloc((512,), F32)
    cos = self.lalloc((512,), F32)
    sin = self.lalloc((512,), F32)
    t1 = self.lalloc((512,), F32)
    t2 = self.lalloc((512,), F32)
    rtm = self.lalloc((8,), F32)
    ones = self.cT["ones"]
    bd64 = self.cT["bd64"]
    gv = VEC_QK + l * 16
    H = slice(0, 64)
    st = {}

    def headnorm_rope(src_r, gain_n, gain_r, eps_col, ss_scale, dst_n, dst_n_regs, dst_r, dst_r_regs, c):
        cs = slice(c * 512, (c + 1) * 512)
        self.act(sq_n.ap, u_n.ap, AF.Square, u_n.regs, sq_n.regs)
        self.act(sq_r.ap[H, :], src_r.ap[H, :], AF.Square, src_r.regs, sq_r.regs)
        bsn = self.bankA()
        self.mm(self.PS[:, bsn, :], bd64, sq_n.ap, True, False, sq_n.regs + [self.creg], [self.PSR[bsn]])
        self.mm(self.PS[:, bsn, :], Em.ap[H, :], sq_r.ap[H, :], False, True, sq_r.regs + Em.regs, [self.PSR[bsn]])
        bsr = self.bankA()
        self.mm(self.PS[H, bsr, :], Fm.ap[:, 0:64], sq_n.ap, True, False, sq_n.regs + Fm.regs, [self.PSR[bsr]])
        self.mm(self.PS[H, bsr, :], Gm.ap[H, 0:64], sq_r.ap[H, :], False, True, sq_r.regs + Gm.regs, [self.PSR[bsr]])
        self.act(rs_n.ap, self.PS[:, bsn, :], AF.Ln, [self.PSR[bsn]], rs_n.regs, scale=ss_scale, bias=self.vcol(eps_col))
        self.act(rs_n.ap, rs_n.ap, AF.Exp, rs_n.regs, rs_n.regs, scale=-0.5)
        self.act(rs_r.ap[H, :], self.PS[H, bsr, :], AF.Ln, [self.PSR[bsr]], rs_r.regs, scale=ss_scale,
                 bias=self.vecs[H, eps_col:eps_col + 1])
        self.act(rs_r.ap[H, :], rs_r.ap[H, :], AF.Exp, rs_r.regs, rs_r.regs, scale=-0.5)
        self.stt(dst_n, u_n.ap, self.vcol(gain_n), rs_n.ap, ALU.mult, ALU.mult, u_n.regs + rs_n.regs + [self.creg], dst_n_regs)
        self.stt(t1.ap[H, :], src_r.ap[H, :], self.vecs[H, gain_r:gain_r + 1], rs_r.ap[H, :], ALU.mult, ALU.mult,
                 src_r.regs + rs_r.regs + [self.creg], t1.regs)
        import os
        if int(os.environ.get("CQ", "9")) <= 3:
            return
        bp = self.bank()
        self.mm(self.PS[H, bp, :], ROT.ap[H, 0:64], t1.ap[H, :], True, True, ROT.regs + t1.regs, [self.PSR[bp]])
        self.tt("dve", t2.ap[H, :], self.PS[H, bp, :], sin.ap[H, :], ALU.mult, [self.PSR[bp]] + sin.regs, t2.regs)
        self.tt("dve", t1.ap[H, :], t1.ap[H, :], cos.ap[H, :], ALU.mult, t1.regs + cos.regs, t1.regs)
        self.tt("dve", dst_r, t1.ap[H, :], t2.ap[H, :], ALU.add, t1.regs + t2.regs, dst_r_regs)

    def load_cs(c):
        self.dma("sp", cos.ap[H, :], self.din["c_cos"][:, c * 512:(c + 1) * 512], "ccos", [], cos.regs)
        self.dma("sp", sin.ap[H, :], self.din["c_sin"][:, c * 512:(c + 1) * 512], "csin", [], sin.regs)

    wuq = self.w_uq[l]
    pcs = []
    for h in range(4):
        pcs.append((wuq[:, h * 96:h * 96 + 64], h * 64, 64))
        pcs.append((wuq[:, h * 96 + 64:h * 96 + 96], 256 + h * 32, 32))
    import os
    CD = int(os.environ.get("CD", "9"))
    if CD <= 0:
        return
    wq, wq_r = self.wload(pcs, 3)
    if CD <= 1:
        load_cs(0)
        return

    def cb_q(ti, c, bk, m):
        cs = slice(c * 512, (c + 1) * 512)
        if ti == 0:
            st["ss"] = self.bankA()
            load_cs(c)
        bss = st["ss"]
        self.act(sq_n.ap, self.PS[:, bk, :], AF.Square, [self.PSR[bk]], sq_n.regs)
        self.mm(self.PS[:, bss, :], ones, sq_n.ap, ti == 0, ti == 2, sq_n.regs + [self.creg], [self.PSR[bss]])
        self.ts("dve", cqg.ap[:, ti, :], self.PS[:, bk, :], self.vcol(gv + 10 + ti), ALU.mult,
                [self.PSR[bk], self.creg], cqg.regs)
        if ti < 2:
            return
        import os
        CQ = int(os.environ.get("CQ", "9"))
        if CQ <= 1:
            return
        self.act(rl.ap, self.PS[:, bss, :], AF.Ln, [self.PSR[bss]], rl.regs, scale=1.0 / 384, bias=self.vcol(VEC_EPS))
        self.act(rl.ap, rl.ap, AF.Exp, rl.regs, rl.regs, scale=-0.5)
        for j in range(2):
            bn = self.bank()
            for k in range(3):
                self.mm(self.PS[:, bn, :], wq[:, k, j * 128:(j + 1) * 128], cqg.ap[:, k, :], k == 0, k == 2,
                        wq_r + cqg.regs, [self.PSR[bn]])
            self.tt("dve", u_n.ap, self.PS[:, bn, :], rl.ap, ALU.mult, [self.PSR[bn]] + rl.regs, u_n.regs)
            br = self.bank()
            for k in range(3):
                self.mm(self.PS[H, br, :], wq[:, k, 256 + j * 64:256 + (j + 1) * 64], cqg.ap[:, k, :], k == 0, k == 2,
                        wq_r + cqg.regs, [self.PSR[br]])
            self.tt("dve", u_r.ap[H, :], self.PS[H, br, :], rl.ap[H, :], ALU.mult, [self.PSR[br]] + rl.regs, u_r.regs)
            if CQ <= 2:
                continue
            headnorm_rope(u_r, gv + 6, gv + 8, VEC_EPS + 2, 1.0, QO[:, j, cs], [QOr[j][c]],
                          QR.ap[H, j, cs], QR.regs, c)

    self.proj_fm(w, [(O_CQ, 384)], cb_q)
    import os
    CC = int(os.environ.get("CCUT", "9"))
    if CC <= 1:
        return

    wukv = self.w_ukv[l]
    pcs = []
    for h in range(4):
        pcs.append((wukv[:, h * 128:h * 128 + 64], h * 64, 64))
        pcs.append((wukv[:, h * 128 + 64:h * 128 + 128], 256 + h * 64, 64))
    wk, wk_r = self.wload(pcs, 2)
    ckvg = cqg

    def cb_k(ti, c, bk, m):
        cs = slice(c * 512, (c + 1) * 512)
        if ti == 0:
            st["ss"] = self.bankA()
            st["tm"] = self.bankA()
            load_cs(c)
        bss, btm = st["ss"], st["tm"]
        if ti < 2:
            self.act(sq_n.ap, self.PS[:, bk, :], AF.Square, [self.PSR[bk]], sq_n.regs)
            self.mm(self.PS[:, bss, :], ones, sq_n.ap, ti == 0, ti == 1, sq_n.regs + [self.creg], [self.PSR[bss]])
            for a in range(4):
                self.mm(self.PS[:, btm, a:a + 1], sq_n.ap[:, a * 128:(a + 1) * 128], ones[:, 0:1],
                        ti == 0 and a == 0, ti == 1 and a == 3, sq_n.regs + [self.creg], [self.PSR[btm]])
            self.ts("dve", ckvg.ap[:, ti, :], self.PS[:, bk, :], self.vcol(gv + 13 + ti), ALU.mult,
                    [self.PSR[bk], self.creg], ckvg.regs)
            return
        self.copy("act", u_k.ap[H, :], self.PS[H, bk, :], [self.PSR[bk]], u_k.regs)
        self.act(rl.ap, self.PS[:, bss, :], AF.Ln, [self.PSR[bss]], rl.regs, scale=1.0 / 256, bias=self.vcol(VEC_EPS))
        self.act(rl.ap, rl.ap, AF.Exp, rl.regs, rl.regs, scale=-0.5)
        self.act(rtm.ap[:, 0:4], self.PS[:, btm, 0:4], AF.Ln, [self.PSR[btm]], rtm.regs, scale=1.0 / 256,
                 bias=self.vcol(VEC_EPS))
        self.act(rtm.ap[:, 0:4], rtm.ap[:, 0:4], AF.Exp, rtm.regs, rtm.regs, scale=-0.5)
        for j in range(2):
            bn = self.bank()
            for k in range(2):
                self.mm(self.PS[:, bn, :], wk[:, k, j * 128:(j + 1) * 128], ckvg.ap[:, k, :], k == 0, k == 1,
                        wk_r + ckvg.regs, [self.PSR[bn]])
            self.tt("dve", u_n.ap, self.PS[:, bn, :], rl.ap, ALU.mult, [self.PSR[bn]] + rl.regs, u_n.regs)
            headnorm_rope(u_k, gv + 7, gv + 9, VEC_EPS, 1.0 / 96, KC.ap[:, j, cs], KC.regs,
                          KR.ap[H, j, cs], KR.regs, c)
        for a in range(4):
            bv = self.bank()
            for k in range(2):
                self.mm(self.PS[:, bv, 0:256], ckvg.ap[:, k, a * 128:(a + 1) * 128], wk[:, k, 256:512], k == 0, k == 1,
                        wk_r + ckvg.regs, [self.PSR[bv]])
            self.ts("dve", VC.ap[:, c * 4 + a, :], self.PS[:, bv, 0:256], rtm.ap[:, a:a + 1], ALU.mult,
                    [self.PSR[bv]] + rtm.regs, VC.regs)

    self.proj_fm(w, [(O_CKV, 256), (O_CKR, 32), (O_CKR, 32)], cb_k, tiles=[(0, 128), (128, 128), (256, 64)])

    if CC <= 2:
        return
    self.loc_cur = mark
    self.attn_bufs()
    anti = self.cT["anti"]
    for h in range(4):
        j, e = h // 2, h % 2
        rs_ = slice(64 * e, 64 * e + 64)
        rr = slice(32 * e, 32 * e + 32)
        for c in range(NCH):
            def qk_fn(sb, qs, n):
                return [(0, n, KC.ap[rs_, j, sb * 128:(sb + 1) * 128], QO[rs_, j, qs:qs + n], KC.regs + [QOr[j][c]]),
                        (0, n, KR.ap[rr, j, sb * 128:(sb + 1) * 128], QR.ap[rr, j, qs:qs + n], KR.regs + QR.regs)]

            def bias_fn(sb, qs, n):
                if qs != sb * 128:
                    return []
                return [(0, n, anti, stc.ap[:, 0:n], stc.regs + [self.creg])]

            def v_fn(sb):
                return VC.ap[:, sb, j * 128:(j + 1) * 128], VC.regs
            self.attn_seg(c * 512, 512, 4 * (c + 1), qk_fn, bias_fn, v_fn, e,
                          QO[rs_, j, c * 512:(c + 1) * 512], [QOr[j][c]])


Builder.mix_C = _mix_C


def _mix_D(self, l):
    w = self.w_in[l]
    QO, QOr = self.QO[3], self.QOr[3]
    self.lreset()
    nacc = self.lalloc((T,), F32)
    dacc = self.lalloc((T,), F32)
    sd = [self.lalloc((256,), BF16) for _ in range(6)]
    QD = self.lalloc((T,), BF16)
    KD = [self.lalloc((T,), BF16) for _ in range(2)]
    for kz in KD:
        self.S.op("dve", lambda hh, kz=kz: hh.memset(kz.ap, 0.0), [], kz.regs)
    VD = self.lalloc((16, 128), BF16)
    self.hn_alloc()
    self.attn_bufs()
    anti = self.cT["anti"]
    ones = self.cT["ones"]
    gv = VEC_QK + l * 16
    allh = lambda k: [self.hTr[k][c] for c in range(NCH)]
    for j in range(2):
        for g, dil in enumerate((1, 4, 16)):
            nbk = 16 // dil
            for e in range(2):
                self.load_strip(sd[g * 2 + e], self.din["gd"], g * 4 + 2 * j + e, 256, "sd%d" % (g * 2 + e))

            def cb(ti, c, bk, m):
                cs = slice(c * 512, (c + 1) * 512)
                if ti == 0:
                    self.head_norm(bk, 128, gv + 4, VEC_EPS + 1, 1.0, QD.ap[:, cs], QD.regs)
                else:
                    self.head_norm(bk, 128, gv + 5, VEC_EPS, 1.0 / 64, None, KD[0].regs + KD[1].regs,
                                   split=(KD[0].ap[0:64, cs], KD[1].ap[64:128, cs]))
            self.proj_fm(w, [(O_DQ + g * 256 + j * 128, 128), (O_DK + g * 256 + j * 128, 128)], cb)

            def tok_ap(k, blk):
                r, n = blk // nbk, blk % nbk
                base = r + dil * n * 128
                return self.hT[:, k, base:base + 127 * dil + 1:dil], allh(k)
            self.proj_tm(w, [(O_DV + g * 256 + j * 128, 128)],
                         lambda blk, bk, o: self.copy("act", VD.ap[:, blk, :], self.PS[:, bk, 0:128], [self.PSR[bk]], VD.regs),
                         tok_ap=tok_ap)
            for e in range(2):
                rows = slice(64 * e, 64 * e + 64)
                st = sd[g * 2 + e]
                for r in range(dil):
                    for n in range(nbk):
                        bo = self.bankA()
                        bd = self.bankA()
                        qb = r + dil * n * 128
                        qsl = slice(qb, qb + 127 * dil + 1, dil)
                        kbs = ([n - 1] if n > 0 else []) + [n]
                        for i, kn in enumerate(kbs):
                            kb = r + dil * kn * 128
                            off = 128 if kn != n else 0
                            bs = self.bank()
                            self.mm(self.PS[:, bs, 0:128], KD[e].ap[:, kb:kb + 127 * dil + 1:dil], QD.ap[:, qsl], True, False,
                                    KD[e].regs + QD.regs, [self.PSR[bs]])
                            self.mm(self.PS[:, bs, 0:128], anti, st.ap[:, off:off + 128], False, True,
                                    st.regs + [self.creg], [self.PSR[bs]])
                            p = self.Pb[self.pi % 2]
                            self.pi += 1
                            self.act(p.ap[:, 0:128], self.PS[:, bs, 0:128], AF.Exp, [self.PSR[bs]], p.regs)
                            last = i == len(kbs) - 1
                            self.mm(self.PS[:, bo, 0:128], VD.ap[:, r * nbk + kn, :], p.ap[:, 0:128], i == 0, last,
                                    VD.regs + p.regs, [self.PSR[bo]])
                            self.mm(self.PS[:, bd, 0:128], ones, p.ap[:, 0:128], i == 0, last, p.regs + [self.creg],
                                    [self.PSR[bd]])
                        if g == 0:
                            self.copy("act", nacc.ap[rows, qsl], self.PS[rows, bo, 0:128], [self.PSR[bo]], nacc.regs)
                            self.copy("dve", dacc.ap[rows, qsl], self.PS[rows, bd, 0:128], [self.PSR[bd]], dacc.regs)
                        else:
                            self.tt("dve", nacc.ap[rows, qsl], self.PS[rows, bo, 0:128], nacc.ap[rows, qsl], ALU.add,
                                    [self.PSR[bo]] + nacc.regs, nacc.regs)
                            self.tt("dve", dacc.ap[rows, qsl], self.PS[rows, bd, 0:128], dacc.ap[rows, qsl], ALU.add,
                                    [self.PSR[bd]] + dacc.regs, dacc.regs)
        rc = self.rcb
        for c in range(NCH):
            cs = slice(c * 512, (c + 1) * 512)
            self.act(rc.ap, dacc.ap[:, cs], AF.Ln, dacc.regs, rc.regs)
            self.act(rc.ap, rc.ap, AF.Exp, rc.regs, rc.regs, scale=-1.0)
            self.tt("dve", QO[:, j, cs], nacc.ap[:, cs], rc.ap, ALU.mult, nacc.regs + rc.regs, [QOr[j][c]])


NIT = 16
S0 = 32.0


def _mix_A(self, l):
    w = self.w_in[l]
    QO, QOr = self.QO[0], self.QOr[0]
    self.lreset()
    KA = [self.lalloc((T,), BF16) for _ in range(2)]
    for kz in KA:
        self.S.op("dve", lambda hh, kz=kz: hh.memset(kz.ap, 0.0), [], kz.regs)
    VA = self.lalloc((16, 128), BF16)
    IQ = self.lalloc((T,), BF16)
    IQ3 = self.lalloc((T,), BF16)
    IK3 = self.lalloc((T,), BF16)
    wab = self.lalloc((16, 4), F32)
    wsg = self.lalloc((16, 4), F32)
    cm = self.lalloc((128,), F32)
    self.dma("sp", cm.ap, self.din["c_cm"], "ccm", [], cm.regs)
    mark = self.loc_cur
    self.hn_alloc()
    gv = VEC_QK + l * 16
    self.proj_fm(w, [(O_AQ, 256)], lambda ti, c, bk, m: self.head_norm(
        bk, 128, gv + 0, VEC_EPS + 1, 1.0, QO[:, ti, c * 512:(c + 1) * 512], [QOr[ti][c]]))
    self.proj_fm(w, [(O_AK, 64), (O_AK, 64)], lambda ti, c, bk, m: self.head_norm(
        bk, 128, gv + 1, VEC_EPS, 1.0 / 64, None, KA[0].regs + KA[1].regs,
        split=(KA[0].ap[0:64, c * 512:(c + 1) * 512], KA[1].ap[64:128, c * 512:(c + 1) * 512])))

    def cb_i(ti, c, bk, m):
        cs = slice(c * 512, (c + 1) * 512)
        dst = (IQ, IQ3, IK3)[ti]
        self.copy("act" if ti % 2 else "dve", dst.ap[0:m, cs], self.PS[0:m, bk, :], [self.PSR[bk]], dst.regs)
    self.proj_fm(w, [(O_IQ, 128), (O_IK, 32), (O_IK, 32), (O_IK, 32)], cb_i, tiles=[(0, 96), (96, 32), (128, 96)])

    def cb_v(tt, bk, o):
        pr = [self.PSR[bk]]
        self.copy("act", VA.ap[:, tt, 0:64], self.PS[:, bk, 0:64], pr, VA.regs)
        self.copy("dve", VA.ap[:, tt, 64:128], self.PS[:, bk, 0:64], pr, VA.regs)
        self.act(wab.ap[:, tt, :], self.PS[:, bk, 64:68], AF.Abs, pr, wab.regs)
        self.act(wsg.ap[:, tt, :], self.PS[:, bk, 64:68], AF.Sign, pr, wsg.regs)
    self.proj_tm(w, [(O_AV, 64), (O_IW, 4)], cb_v)
    self.loc_cur = mark
    self.attn_bufs()
    score = self.lalloc((T,), F32)
    rt = [self.lalloc((512,), F32) for _ in range(2)]
    mbt = [self.lalloc((T,), BF16) for _ in range(2)]
    strips = [self.lalloc((T,), BF16) for _ in range(2)]
    small = self.lalloc((16,), F32)
    thr = small.ap[:, 0:1]
    cnt = small.ap[:, 1:2]
    g2 = small.ap[:, 2:3]
    anti = self.cT["anti"]
    ident = self.cT["ident"]
    ri = 0
    si = 0
    for qp in range(8):
        for i in range(2):
            qt = 2 * qp + i
            if qt < 2:
                continue
            nk = (qt + 1) * 128
            qsl = slice(qt * 128, (qt + 1) * 128)
            for s0 in range(0, nk, 512):
                sn = min(512, nk - s0)
                for h in range(4):
                    bz = self.bank()
                    if h < 3:
                        lt, rh = IQ.ap[32 * h:32 * h + 32, qsl], IK3.ap[32 * h:32 * h + 32, s0:s0 + sn]
                        rd = IQ.regs + IK3.regs
                    else:
                        lt, rh = IQ3.ap[0:32, qsl], IK3.ap[0:32, s0:s0 + sn]
                        rd = IQ3.regs + IK3.regs
                    self.mm(self.PS[:, bz, 0:sn], lt, rh, True, True, rd, [self.PSR[bz]])
                    r = rt[ri % 2]
                    ri += 1
                    self.act(r.ap[:, 0:sn], self.PS[:, bz, 0:sn], AF.Relu, [self.PSR[bz]] + wab.regs, r.regs,
                             scale=wab.ap[:, qt, h:h + 1])
                    if h == 0:
                        self.ts("dve", score.ap[:, s0:s0 + sn], r.ap[:, 0:sn], wsg.ap[:, qt, 0:1], ALU.mult,
                                r.regs + wsg.regs, score.regs)
                    else:
                        self.stt(score.ap[:, s0:s0 + sn], r.ap[:, 0:sn], wsg.ap[:, qt, h:h + 1], score.ap[:, s0:s0 + sn],
                                 ALU.mult, ALU.add, r.regs + wsg.regs + score.regs, score.regs)
            self.tt("dve", score.ap[:, qsl], score.ap[:, qsl], cm.ap, ALU.add, score.regs + cm.regs, score.regs)
            self.S.op("dve", lambda hh: hh.memset(thr, 0.0), [], small.regs)
            mb = mbt[i]
            for it in range(NIT):
                step = S0 / (2 ** it)
                self.ts("dve", mb.ap[:, 0:nk], score.ap[:, 0:nk], thr, ALU.is_gt, score.regs + small.regs,
                        mb.regs + small.regs, op1=ALU.add, accum=cnt)
                self.ts("dve", g2, cnt, 255.5, ALU.is_ge, small.regs, small.regs, s2=2.0 * step, op1=ALU.mult)
                self.stt(thr, g2, -step, thr, ALU.add, ALU.add, small.regs, small.regs)
            self.ts("dve", thr, thr, -S0 / (2 ** (NIT - 1)), ALU.add, small.regs, small.regs)
            self.ts("dve", mb.ap[:, 0:nk], score.ap[:, 0:nk], thr, ALU.is_le, score.regs + small.regs, mb.regs,
                    s2=NEG, op1=ALU.mult)
        q0 = qp * 256
        for h in range(4):
            j, e = h // 2, h % 2
            rs_ = slice(64 * e, 64 * e + 64)
            st = strips[si % 2]
            self.load_strip(st, self.din["gext"], h, q0 + 256, "strip%d" % (si % 2))
            si += 1

            def qk_fn(sb, qs, n):
                return [(0, n, KA[e].ap[:, sb * 128:(sb + 1) * 128], QO[:, j, qs:qs + n], KA[e].regs + [QOr[j][qp // 2]])]

            def bias_fn(sb, qs, n):
                off = qs - sb * 128
                out = [(0, n, anti, st.ap[:, off:off + n], st.regs + [self.creg])]
                if qp > 0:
                    for i in range(2):
                        t0 = q0 + i * 128
                        if t0 >= qs:
                            out.append((t0 - qs, 128, mbt[i].ap[:, sb * 128:(sb + 1) * 128], ident,
                                        mbt[i].regs + [self.creg]))
                return out

            def v_fn(sb):
                return VA.ap[:, sb, :], VA.regs
            self.attn_seg(q0, 256, 2 * (qp + 1), qk_fn, bias_fn, v_fn, e, QO[rs_, j, q0:q0 + 256], [QOr[j][qp // 2]])


Builder.mix_D = _mix_D
Builder.mix_A = _mix_A


_CACHE = {}


def get_nc(nseq, stages, dbg=None):
    key = (nseq, stages, dbg)
    if key not in _CACHE:
        b = Builder(nseq, stages, dbg)
        _CACHE[key] = b
        b.nc_built = b.build_wrapped()
    return _CACHE[key]


def _build_wrapped(self):
    return self.build()


Builder.build_wrapped = _build_wrapped


def make_inmap(inp):
    shared = {
        "w_ffn_in": np.ascontiguousarray(np.asarray(inp["w_ffn_in"], np.float32)),
        "w_ffn_out": np.ascontiguousarray(np.asarray(inp["w_ffn_out"], np.float32)),
        "vecs": make_vecs(inp),
    }
    for k, v in host_consts().items():
        shared["c_" + k] = v
    for k, v in host_consts2().items():
        shared["c_" + k] = v
    for k in ("w_in", "w_branch", "w_out", "w_mla_uq", "w_mla_ukv"):
        shared[k] = np.ascontiguousarray(np.asarray(inp[k], np.float32))
    shared["gext"], shared["gd"] = make_gext(inp)
    return shared


def kernel(**inp):
    ncores = 8
    nseq = 2
    b = get_nc(nseq, 99)
    x = np.ascontiguousarray(np.asarray(inp["x"], np.float32))
    shared = make_inmap(inp)
    in_maps = []
    for i in range(ncores):
        m = dict(shared)
        m["x"] = x[i * nseq:(i + 1) * nseq]
        in_maps.append(m)
    res = run_bass_kernel_spmd(b.nc_built, in_maps, core_ids=list(range(ncores)))
    return np.concatenate([r["y"] for r in res.results], axis=0)
```

```python
import numpy as np
import math
from contextlib import ExitStack
import concourse.bass as bass
import concourse.mybir as mybir
from concourse.bass_utils import run_bass_kernel_spmd

F32 = mybir.dt.float32
BF16 = mybir.dt.bfloat16
AF = mybir.ActivationFunctionType
ALU = mybir.AluOpType

D = 1024
T = 2048
DFF = 2816
INW = 8388
NCH = 4
NEG = -30000.0
EPS = 1e-6

O_AQ, O_AK, O_AV, O_IQ, O_IK, O_IW = 0, 256, 320, 384, 512, 544
O_BQ, O_BK, O_BV = 548, 804, 1060
O_CQ, O_CKV, O_CKR = 1316, 1700, 1956
O_DQ, O_DK, O_DV = 1988, 2756, 3524
O_G = 4292

PAGE = 1024
ARENA_BYTES = 224000


class Reg:
    __slots__ = ("w", "r", "excl")

    def __init__(self, excl=False):
        self.w = None
        self.r = {}
        self.excl = excl


class TT:
    __slots__ = ("ap", "regs")

    def __init__(self, ap, regs):
        self.ap = ap
        self.regs = regs


ENGS = ["pe", "act", "dve", "pool", "sp"]


class Sched:
    def __init__(self):
        self.ops = {e: [] for e in ENGS}
        self.cnt = {e: 0 for e in ENGS}
        self.waited = {e: {} for e in ENGS}
        self.dma_tot = {}

    def _waits(self, eng, reads, writes, k):
        need = {}
        for r in reads:
            t = r.w
            if t is None:
                continue
            key, val = t
            if key == eng and eng == "pe":
                continue
            if need.get(key, 0) < val:
                need[key] = val
        for w in writes:
            toks = list(w.r.values())
            if w.w is not None:
                toks.append(w.w)
            for key, val in toks:
                if key == eng and eng == "pe":
                    continue
                if need.get(key, 0) < val:
                    need[key] = val
        out = []
        wd = self.waited[eng]
        for key, val in need.items():
            if wd.get(key, 0) >= val:
                continue
            wd[key] = val
            out.append((key, val))
        return out

    def op(self, eng, fn, reads=(), writes=(), mode=None):
        if any(r.excl for r in reads):
            writes = list(writes) + [r for r in reads if r.excl]
            reads = [r for r in reads if not r.excl]
        k = self.cnt[eng] + 1
        waits = self._waits(eng, reads, writes, k)
        self.cnt[eng] = k
        self.ops[eng].append((0, fn, waits, mode))
        tok = (eng, k)
        for r in reads:
            r.r[eng] = tok
        for w in writes:
            w.w = tok
            w.r = {}

    def dma(self, queue, fn, stream, reads=(), writes=()):
        waits = self._waits(queue, reads, writes, 1 << 60)
        tot = self.dma_tot.get(stream, 0) + 16
        self.dma_tot[stream] = tot
        self.ops[queue].append((1, fn, waits, stream))
        key = ("D", stream)
        tok = (key, tot)
        for r in reads:
            r.r[key] = tok
        for w in writes:
            w.w = tok
            w.r = {}

    def final_waits(self, eng, streams):
        waits = []
        for s in streams:
            if s in self.dma_tot:
                waits.append((("D", s), self.dma_tot[s]))
        self.ops[eng].append((2, None, waits, None))

    def emit(self, nc):
        with ExitStack() as es:
            sems = {}
            for e in ENGS:
                sems[e] = es.enter_context(nc.semaphore("s_" + e))
            for i, s in enumerate(self.dma_tot):
                sems[("D", s)] = es.enter_context(nc.semaphore("d%d" % i))
            block = es.enter_context(nc.Block())
            names = {"pe": "tensor", "act": "scalar", "dve": "vector", "pool": "gpsimd", "sp": "sync"}

            def mk(e):
                def body(h):
                    se = sems[e]
                    last_mode = (128, 128, 0)
                    for kind, fn, waits, stream in self.ops[e]:
                        for key, val in waits:
                            h.wait_ge(sems[key], val)
                        if kind == 0:
                            if e == "pe":
                                md = stream if stream is not None else (128, 128, 0)
                                if md != last_mode:
                                    h.drain()
                                    self.ndrain = getattr(self, "ndrain", 0) + 1
                                    last_mode = md
                            fn(h).then_inc(se, 1)
                        elif kind == 1:
                            fn(h).then_inc(sems[("D", stream)], 16)
                return body

            for e in ENGS:
                getattr(block, names[e])(mk(e))


def rel_bucket_np(dist):
    n = np.maximum(dist, 0)
    max_exact = 16
    nf = np.maximum(n, 1).astype(np.float32)
    log_b = max_exact + (np.log(nf / np.float32(max_exact)) / np.float32(math.log(2048 / max_exact))
                         * np.float32(32 - max_exact)).astype(np.int32)
    return np.where(n < max_exact, n, np.minimum(log_b, 31))


def host_consts():
    c = {}
    eye = np.eye(128, dtype=np.float32)
    c["ident"] = eye
    c["anti"] = eye[::-1].copy()
    c["ones"] = np.ones((128, 128), np.float32)
    bd = np.zeros((128, 128), np.float32)
    bd[:64, :64] = 1
    bd[64:, 64:] = 1
    c["bd64"] = bd
    return c


def host_consts2():
    c = {}
    c["sel"] = np.kron(np.eye(32, dtype=np.float32), np.ones((1, 128), np.float32))
    bm = np.zeros((8, 4, 8), np.float32)
    for own in range(8):
        bm[own, :, own:] = -1e30
    c["bmk"] = np.broadcast_to(bm.reshape(1, 256), (128, 256)).copy()
    E = np.zeros((64, 128), np.float32)
    for k in range(64):
        E[k, (k // 32) * 64:(k // 32) * 64 + 64] = 1
    c["E"] = E
    cmm = np.where(np.arange(128)[None, :] <= np.arange(128)[:, None], 0.0, -1e30).astype(np.float32)
    c["cm"] = cmm
    c["F"] = E.T.copy()
    G = np.zeros((64, 64), np.float32)
    G[:32, :32] = 1
    G[32:, 32:] = 1
    c["G"] = G
    rot = np.zeros((64, 64), np.float32)
    for b in range(2):
        for m in range(16):
            rot[b * 32 + m + 16, b * 32 + m] = -1.0
            rot[b * 32 + m, b * 32 + m + 16] = 1.0
    c["rot"] = rot
    freqs = 10000.0 ** (-np.arange(16, dtype=np.float32) / 16)
    ang = np.arange(T, dtype=np.float32)[None, :] * np.tile(freqs, 4)[:, None].astype(np.float32)
    c["cos"] = np.cos(ang).astype(np.float32)
    c["sin"] = np.sin(ang).astype(np.float32)
    return c


class Builder:
    def __init__(self, nseq=2, stages=99, dbg=None):
        self.nseq = nseq
        self.stages = stages
        self.dbg = dbg
        self.S = Sched()
        self.nc = bass.Bass("TRN2", target_bir_lowering=False, dynamic_dma_scratch_size=4096)
        self.din = {}
        self.wslot_i = 0
        self.bank_i = 0
        self.bankA_i = 0

    def dram_in(self, name, shape, dt=F32):
        t = self.nc.dram_tensor(name, list(shape), dt, kind="ExternalInput").ap()
        self.din[name] = t
        return t

    def carve(self, off, free_shape, dt):
        n = 1
        for s in free_shape:
            n *= s
        nb = n * (4 if dt == F32 else 2)
        assert off % 4 == 0 and off + nb <= ARENA_BYTES, (off, nb)
        ap = self.arena[:, off // 2:(off + nb) // 2]
        if dt == F32:
            ap = ap.bitcast(F32)
        if len(free_shape) == 2:
            ap = ap.rearrange("p (a b) -> p a b", a=free_shape[0])
        elif len(free_shape) == 3:
            ap = ap.rearrange("p (a b c) -> p a b c", a=free_shape[0], b=free_shape[1])
        return ap, nb

    def lalloc(self, free_shape, dt):
        ap, nb = self.carve(self.loc_base + self.loc_cur, free_shape, dt)
        p0 = self.loc_cur // PAGE
        self.loc_cur += (nb + PAGE - 1) // PAGE * PAGE
        p1 = self.loc_cur // PAGE
        assert self.loc_base + self.loc_cur <= ARENA_BYTES, ("local overflow", self.loc_cur)
        return TT(ap, self.loc_pages[p0:p1])

    def lreset(self):
        self.loc_cur = 0

    def bank(self):
        b = self.bank_i
        self.bank_i = (b + 1) % 4
        return b

    def bankA(self):
        b = self.bankA_i
        self.bankA_i = (b + 1) % 4
        return 4 + b

    def mm(self, out, lhsT, rhs, start, stop, reads, writes):
        ru = lambda v: 32 if v <= 32 else (64 if v <= 64 else 128)
        kk = lhsT.shape[0]
        mmm = 1
        for d in lhsT.shape[1:]:
            mmm *= d
        self.S.op("pe", lambda h: h.matmul(out, lhsT, rhs, start=start, stop=stop), reads, writes,
                  mode=(ru(kk), ru(mmm), lhsT.offset // (lhsT.tensor.shape[1] * 32) if kk < 128 else 0))

    def act(self, out, in_, func, reads, writes, scale=1.0, bias=0.0, accum=None):
        if accum is None:
            self.S.op("act", lambda h: h.activation(out=out, in_=in_, func=func, scale=scale, bias=bias),
                      reads, writes)
        else:
            self.S.op("act", lambda h: h.activation(out=out, in_=in_, func=func, scale=scale, bias=bias,
                                                      accum_out=accum), reads, writes)

    def ts(self, eng, out, in0, s1, op0, reads, writes, s2=None, op1=None, accum=None):
        def fn(h):
            kw = {}
            if op1 is not None:
                kw["op1"] = op1
            if accum is not None:
                kw["accum_out"] = accum
            return h.tensor_scalar(out=out, in0=in0, scalar1=s1, scalar2=s2, op0=op0, **kw)
        self.S.op(eng, fn, reads, writes)

    def stt(self, out, in0, scalar, in1, op0, op1, reads, writes):
        self.S.op("dve", lambda h: h.scalar_tensor_tensor(out=out, in0=in0, scalar=scalar, in1=in1,
                                                            op0=op0, op1=op1), reads, writes)

    def tt(self, eng, out, in0, in1, op, reads, writes):
        self.S.op(eng, lambda h: h.tensor_tensor(out=out, in0=in0, in1=in1, op=op), reads, writes)

    def copy(self, eng, out, in_, reads, writes):
        if eng == "act":
            self.S.op("act", lambda h: h.copy(out=out, in_=in_), reads, writes)
        else:
            self.S.op(eng, lambda h: h.tensor_copy(out=out, in_=in_), reads, writes)

    def recip(self, out, in_, reads, writes):
        self.S.op("dve", lambda h: h.reciprocal(out=out, in_=in_), reads, writes)

    def dma(self, queue, out, in_, stream, reads, writes):
        if queue == "pool":
            self.S.dma(queue, lambda h: h.dma_start(out=out, in_=in_, max_dma_last_dim=2048), stream, reads, writes)
        else:
            self.S.dma(queue, lambda h: h.dma_start(out=out, in_=in_), stream, reads, writes)

    def wload(self, pieces, nk):
        s = self.wslot_i
        self.wslot_i = (s + 1) % len(self.wslots)
        ap_full, reg = self.wslots[s]
        tot = max(o + n for _, o, n in pieces)
        assert nk * tot * 2 <= 8192, (nk, tot)
        view = ap_full[:, 0:nk * tot].rearrange("p (k c) -> p k c", k=nk)
        for src, o, n in pieces:
            srcv = src.rearrange("(k p) c -> p k c", p=128)
            self.dma("pool", view[:, :, o:o + n], srcv, ("w", s), [], [reg])
        return view, [reg]

    def build(self):
        nc = self.nc
        ns = self.nseq
        x_in = self.dram_in("x", (ns, T, D))
        wfi = self.dram_in("w_ffn_in", (2, 2, D, 2 * DFF))
        wfo = self.dram_in("w_ffn_out", (2, 2, DFF, D))
        vecs = self.dram_in("vecs", (128, NVEC))
        self.w_in = self.dram_in("w_in", (2, D, INW))
        self.w_branch = self.dram_in("w_branch", (2, 4, 256, D))
        self.w_out = self.dram_in("w_out", (2, D, D))
        self.w_uq = self.dram_in("w_mla_uq", (2, 384, 384))
        self.w_ukv = self.dram_in("w_mla_ukv", (2, 256, 512))
        self.dram_in("gext", (9, NG))
        self.dram_in("gd", (12, NGD))
        for k, v in host_consts2().items():
            self.dram_in("c_" + k, v.shape)
        if self.dbg:
            self.dbg_out = self.nc.dram_tensor("dbg", [4, 128, 2, T], BF16, kind="ExternalOutput").ap()
        cst = {k: self.dram_in("c_" + k, v.shape) for k, v in host_consts().items()}
        self.y_out = nc.dram_tensor("y", [ns, T, D], F32, kind="ExternalOutput").ap()
        with ExitStack() as es:
            arena_t = es.enter_context(nc.sbuf_tensor("arena", [128, ARENA_BYTES // 2], BF16))
            self.arena = arena_t[:, :]
            ps_t = es.enter_context(nc.psum_tensor("ps", [128, 8, 512], F32))
            self.PS = ps_t
            self.PSR = [Reg(excl=True) for _ in range(8)]
            off = 0
            self.xT, nb = self.carve(off, (8, T), F32); off += nb
            self.xTr = [[Reg() for _ in range(NCH)] for _ in range(8)]
            self.hT, nb = self.carve(off, (8, T), BF16); off += nb
            self.hTr = [[Reg() for _ in range(NCH)] for _ in range(8)]
            self.QO = []
            self.QOr = []
            for n in range(4):
                q, nb = self.carve(off, (2, T), BF16); off += nb
                self.QO.append(q)
                self.QOr.append([[Reg() for _ in range(NCH)] for _ in range(2)])
            self.wslots = []
            for s in range(3):
                w, nb = self.carve(off, (4096,), BF16); off += nb
                self.wslots.append((w, Reg()))
            self.cT = {}
            creg = Reg()
            self.creg = creg
            for k, v in host_consts().items():
                ap, nb = self.carve(off, (v.shape[1],), BF16); off += nb
                self.cT[k] = ap
                self.dma("pool", ap, cst[k], "const", [], [creg])
            ap, nb = self.carve(off, (128,), F32); off += nb
            self.identf = ap
            self.dma("sp", ap, cst["ident"], "constf", [], [creg])
            ap, nb = self.carve(off, (NVEC,), F32); off += nb
            self.vecs = ap
            self.eps_ap = ap[:, VEC_EPS:VEC_EPS + 1]
            self.dma("sp", ap, vecs, "constf", [], [creg])
            off = (off + PAGE - 1) // PAGE * PAGE
            self.loc_base = off
            npages = (ARENA_BYTES - off) // PAGE
            self.loc_pages = [Reg() for _ in range(npages)]
            self.loc_cur = 0
            print("persistent bytes", off, "local pages", npages)

            for b in range(ns):
                self.load_x(x_in, b)
                st = 0
                for l in range(2):
                    for f in range(2):
                        if f == 1:
                            if st < self.stages:
                                self.mixer(l)
                                if self.dbg:
                                    st = 1000
                            st += 1
                        if st < self.stages:
                            self.ffn(wfi[l, f], wfo[l, f], VEC_NG + (l * 3 + (0 if f == 0 else 2)) * 8)
                        st += 1
                self.store_x(b)
            self.S.final_waits("sp", ["out0", "out1"])
            self.S.emit(nc)
        return nc

    def load_x(self, x_in, b):
        self.lreset()
        stg = [self.lalloc((4, D), F32) for _ in range(2)]
        for c in range(NCH):
            s = stg[c % 2]
            src = x_in[b, c * 512:(c + 1) * 512, :].rearrange("(a p) d -> p a d", p=128)
            self.dma("sp", s.ap, src, "xin%d" % (c % 2), [], s.regs)
            for j in range(8):
                bk = self.bank()
                for a in range(4):
                    o = self.PS[:, bk, a * 128:(a + 1) * 128]
                    i = s.ap[:, a, j * 128:(j + 1) * 128]
                    self.S.op("pe", lambda h, o=o, i=i: h.transpose(o, i, self.identf),
                              s.regs + [self.creg], [self.PSR[bk]])
                dst = self.xT[:, j, c * 512:(c + 1) * 512]
                self.copy("act" if j % 2 else "dve", dst, self.PS[:, bk, :], [self.PSR[bk]], [self.xTr[j][c]])

    def store_x(self, b):
        self.lreset()
        stg = [self.lalloc((4, D), F32) for _ in range(2)]
        for c in range(NCH):
            s = stg[c % 2]
            for a in range(4):
                for jj in range(2):
                    bk = self.bank()
                    for j4 in range(4):
                        j = jj * 4 + j4
                        o = self.PS[:, bk, j4 * 128:(j4 + 1) * 128]
                        i = self.xT[:, j, c * 512 + a * 128: c * 512 + (a + 1) * 128]
                        self.S.op("pe", lambda h, o=o, i=i: h.transpose(o, i, self.identf),
                                  [self.xTr[j][c], self.creg], [self.PSR[bk]])
                    dst = s.ap[:, a, jj * 512:(jj + 1) * 512]
                    self.copy("act" if jj % 2 else "dve", dst, self.PS[:, bk, :], [self.PSR[bk]], s.regs)
            dstd = self.y_out[b, c * 512:(c + 1) * 512, :].rearrange("(a p) d -> p a d", p=128)
            self.dma("sp", dstd, s.ap, "out%d" % (c % 2), s.regs, [])

    def rmsnorm_x(self, gain_col):
        sq = [self.lalloc((512,), BF16) for _ in range(2)]
        rs = self.lalloc((512,), F32)
        ones = self.cT["ones"]
        for c in range(NCH):
            cs = slice(c * 512, (c + 1) * 512)
            bk = self.bank()
            for j in range(8):
                q = sq[j % 2]
                self.act(q.ap, self.xT[:, j, cs], AF.Square, [self.xTr[j][c]], q.regs)
                self.mm(self.PS[:, bk, :], ones, q.ap, j == 0, j == 7, q.regs + [self.creg], [self.PSR[bk]])
            self.act(rs.ap, self.PS[:, bk, :], AF.Ln, [self.PSR[bk]], rs.regs, scale=1.0 / D, bias=self.eps_ap)
            self.act(rs.ap, rs.ap, AF.Exp, rs.regs, rs.regs, scale=-0.5)
            for j in range(8):
                g = self.vecs[:, gain_col + j:gain_col + j + 1]
                self.stt(self.hT[:, j, cs], self.xT[:, j, cs], g, rs.ap, ALU.mult, ALU.mult,
                         [self.xTr[j][c], self.creg] + rs.regs, [self.hTr[j][c]])

    def ffn(self, w_in, w_out, gain_col):
        self.lreset()
        self.rmsnorm_x(gain_col)
        gT = self.lalloc((8, T), BF16)
        gp = lambda i, c: gT.regs[i * 4 + c: i * 4 + c + 1]
        sl = [self.lalloc((512,), F32) for _ in range(2)]
        sli = 0
        for (g0, nf) in ((0, 8), (8, 8), (16, 6)):
            for fb in range(0, nf, 2):
                f0 = (g0 + fb) * 128
                wv, wr = self.wload([(w_in[:, f0:f0 + 256], 0, 256),
                                     (w_in[:, DFF + f0:DFF + f0 + 256], 256, 256)], 8)
                for c in range(NCH):
                    cs = slice(c * 512, (c + 1) * 512)
                    for ft in range(2):
                        bg = self.bank()
                        bu = self.bankA()
                        for k in range(8):
                            self.mm(self.PS[:, bg, :], wv[:, k, ft * 128:(ft + 1) * 128], self.hT[:, k, cs],
                                    k == 0, k == 7, wr + [self.hTr[k][c]], [self.PSR[bg]])
                        for k in range(8):
                            self.mm(self.PS[:, bu, :], wv[:, k, 256 + ft * 128:256 + (ft + 1) * 128],
                                    self.hT[:, k, cs], k == 0, k == 7, wr + [self.hTr[k][c]], [self.PSR[bu]])
                        s = sl[sli % 2]
                        sli += 1
                        self.act(s.ap, self.PS[:, bg, :], AF.Silu, [self.PSR[bg]], s.regs)
                        self.tt("dve", gT.ap[:, fb + ft, cs], s.ap, self.PS[:, bu, :], ALU.mult,
                                s.regs + [self.PSR[bu]], gp(fb + ft, c))
            for dq in range(2):
                wv, wr = self.wload([(w_out[g0 * 128:(g0 + nf) * 128, dq * 512:(dq + 1) * 512], 0, 512)], nf)
                for c in range(NCH):
                    cs = slice(c * 512, (c + 1) * 512)
                    for dt in range(4):
                        d = dq * 4 + dt
                        bk = self.bank()
                        for i in range(nf):
                            self.mm(self.PS[:, bk, :], wv[:, i, dt * 128:(dt + 1) * 128], gT.ap[:, i, cs],
                                    i == 0, i == nf - 1, wr + gp(i, c), [self.PSR[bk]])
                        self.stt(self.xT[:, d, cs], self.PS[:, bk, :], 0.5, self.xT[:, d, cs], ALU.mult, ALU.add,
                                 [self.PSR[bk], self.xTr[d][c]], [self.xTr[d][c]])


VEC_NG = 0
VEC_EPS = 48
VEC_BG = 56
VEC_QK = 120
NVEC = 160
NG = 2175
NGD = 383


def make_vecs(inp):
    v = np.zeros((128, NVEC), np.float32)
    ng = np.asarray(inp["norm_gain"], np.float32)
    v[:, VEC_NG:VEC_NG + 48] = ng.reshape(6, 8, 128).transpose(2, 0, 1).reshape(128, 48)
    v[:, VEC_EPS] = EPS
    v[:, VEC_EPS + 1] = EPS * 64
    v[:, VEC_EPS + 2] = EPS * 96
    bg = np.asarray(inp["b_gate"], np.float32)
    v[:, VEC_BG:VEC_BG + 64] = bg.reshape(8, 8, 128).transpose(2, 0, 1).reshape(128, 64)
    t2 = lambda a: np.concatenate([a, a])
    for l in range(2):
        o = VEC_QK + l * 16
        v[:, o + 0] = t2(np.asarray(inp["qk_gain_a"])[l, 0])
        v[:, o + 1] = t2(np.asarray(inp["qk_gain_a"])[l, 1])
        v[:, o + 2] = t2(np.asarray(inp["qk_gain_b"])[l, 0])
        v[:, o + 3] = t2(np.asarray(inp["qk_gain_b"])[l, 1])
        v[:, o + 4] = t2(np.asarray(inp["qk_gain_d"])[l, 0])
        v[:, o + 5] = t2(np.asarray(inp["qk_gain_d"])[l, 1])
        qc = np.asarray(inp["qk_gain_c"], np.float32)
        v[:, o + 6] = t2(qc[l, 0, :64])
        v[:, o + 7] = t2(qc[l, 1, :64])
        v[:64, o + 8] = t2(qc[l, 0, 64:])
        v[:64, o + 9] = t2(qc[l, 1, 64:])
        v[:, o + 10:o + 13] = np.asarray(inp["mla_norm_q"], np.float32)[l].reshape(3, 128).T
        v[:, o + 13:o + 15] = np.asarray(inp["mla_norm_kv"], np.float32)[l].reshape(2, 128).T
    return v


def make_gext(inp):
    rb = np.asarray(inp["rel_bias"], np.float32)
    dist = np.arange(NG) - 127
    bk = rel_bucket_np(dist)
    g = np.full((9, NG), NEG, np.float32)
    for h in range(8):
        g[h, 127:] = rb[h, bk[127:]]
    g[8, 127:] = 0.0
    gd = np.full((12, NGD), NEG, np.float32)
    dd = np.arange(NGD) - 127
    for gi, dil in enumerate((1, 4, 16)):
        ok = (dd >= 0) & (dd <= 128)
        b2 = rel_bucket_np(dd * dil)
        for h in range(4):
            gd[gi * 4 + h, ok] = rb[8 + gi * 4 + h, b2[ok]]
    return g, gd


def _hn_alloc(self):
    self.hn_sq = [self.lalloc((512,), BF16) for _ in range(2)]
    self.hn_tmp = [self.lalloc((512,), F32) for _ in range(2)]
    self.hn_rs = self.lalloc((512,), F32)
    self.hn_i = 0


def _vcol(self, c):
    return self.vecs[:, c:c + 1]


def _proj_fm(self, w, pieces, cb, tiles=None):
    sp = []
    o = 0
    for c0, n in pieces:
        sp.append((w[:, c0:c0 + n], o, n))
        o += n
    wv, wr = self.wload(sp, 8)
    if tiles is None:
        tiles = [(i * 128, min(128, o - i * 128)) for i in range((o + 127) // 128)]
    for c in range(NCH):
        cs = slice(c * 512, (c + 1) * 512)
        for ti, (tc0, m) in enumerate(tiles):
            bk = self.bank()
            for k in range(8):
                self.mm(self.PS[0:m, bk, :], wv[:, k, tc0:tc0 + m], self.hT[:, k, cs], k == 0, k == 7,
                        wr + [self.hTr[k][c]], [self.PSR[bk]])
            cb(ti, c, bk, m)


def _proj_tm(self, w, pieces, cb, tok_ap=None):
    sp = []
    o = 0
    for c0, n in pieces:
        sp.append((w[:, c0:c0 + n], o, n))
        o += n
    wv, wr = self.wload(sp, 8)
    for tt in range(16):
        bk = self.bank()
        for k in range(8):
            if tok_ap is None:
                lt = self.hT[:, k, tt * 128:(tt + 1) * 128]
                rd = [self.hTr[k][tt // 4]]
            else:
                lt, rd = tok_ap(k, tt)
            self.mm(self.PS[:, bk, 0:o], lt, wv[:, k, 0:o], k == 0, k == 7, wr + rd, [self.PSR[bk]])
        cb(tt, bk, o)


def _head_norm(self, bk, m, gain_col, eps_col, ss_scale, dst_ap, dst_regs, blk="bd64", split=None):
    i = self.hn_i
    self.hn_i += 1
    sq = self.hn_sq[i % 2]
    tmp = self.hn_tmp[i % 2]
    rs = self.hn_rs
    pr = [self.PSR[bk]]
    self.act(sq.ap[0:m, :], self.PS[0:m, bk, :], AF.Square, pr, sq.regs)
    self.copy("act", tmp.ap[0:m, :], self.PS[0:m, bk, :], pr, tmp.regs)
    b2 = self.bankA()
    self.mm(self.PS[0:m, b2, :], self.cT[blk][0:m, 0:m], sq.ap[0:m, :], True, True, sq.regs + [self.creg], [self.PSR[b2]])
    self.act(rs.ap[0:m, :], self.PS[0:m, b2, :], AF.Ln, [self.PSR[b2]], rs.regs, scale=ss_scale,
             bias=self.vecs[0:m, eps_col:eps_col + 1])
    self.act(rs.ap[0:m, :], rs.ap[0:m, :], AF.Exp, rs.regs, rs.regs, scale=-0.5)
    if split is not None:
        for e, d in enumerate(split):
            r = slice(64 * e, 64 * e + 64)
            self.stt(d, tmp.ap[r, :], self.vecs[r, gain_col:gain_col + 1], rs.ap[r, :], ALU.mult, ALU.mult,
                     tmp.regs + rs.regs + [self.creg], dst_regs)
        return
    self.stt(dst_ap, tmp.ap[0:m, :], self.vecs[0:m, gain_col:gain_col + 1], rs.ap[0:m, :], ALU.mult, ALU.mult,
             tmp.regs + rs.regs + [self.creg], dst_regs)


def _load_strip(self, dst, src_dram, row, ncols, stream):
    base = src_dram[row:row + 1, 0:ncols]
    ap = bass.AP(tensor=base.tensor, offset=base.offset, ap=[[1, 128], [1, ncols]])
    self.dma("pool", dst.ap[:, 0:ncols], ap, stream, [], dst.regs)


def _attn_seg(self, q0, nq, nsb, qk_fn, bias_fn, v_fn, e, dst_ap, dst_regs):
    bo = self.bankA()
    bd = self.bankA()
    ones = self.cT["ones"]
    for i0 in range(0, nsb, 2):
        info = []
        for sb in range(i0, min(nsb, i0 + 2)):
            qs = max(q0, sb * 128)
            n = q0 + nq - qs
            c0 = qs - q0
            bs = self.bank()
            info.append((sb, n, c0, bs, qk_fn(sb, qs, n) + bias_fn(sb, qs, n)))
        for idx in range(max(len(x[4]) for x in info)):
            for (sb, n, c0, bs, mms) in info:
                if idx < len(mms):
                    o0, on, lt, rh, rd = mms[idx]
                    self.mm(self.PS[:, bs, o0:o0 + on], lt, rh, idx == 0, idx == len(mms) - 1, rd, [self.PSR[bs]])
        pl = []
        for (sb, n, c0, bs, mms) in info:
            p = self.Pb[self.pi % 2]
            self.pi += 1
            self.act(p.ap[:, 0:n], self.PS[:, bs, 0:n], AF.Exp, [self.PSR[bs]], p.regs)
            pl.append(p)
        for (sb, n, c0, bs, mms), p in zip(info, pl):
            vl, vr = v_fn(sb)
            self.mm(self.PS[:, bo, c0:c0 + n], vl, p.ap[:, 0:n], sb == 0, sb == nsb - 1, vr + p.regs, [self.PSR[bo]])
            self.mm(self.PS[:, bd, c0:c0 + n], ones, p.ap[:, 0:n], sb == 0, sb == nsb - 1, p.regs + [self.creg],
                    [self.PSR[bd]])
    r = slice(64 * e, 64 * e + 64)
    rc = self.rcb
    self.act(rc.ap[r, 0:nq], self.PS[r, bd, 0:nq], AF.Ln, [self.PSR[bd]], rc.regs)
    self.act(rc.ap[r, 0:nq], rc.ap[r, 0:nq], AF.Exp, rc.regs, rc.regs, scale=-1.0)
    self.tt("dve", dst_ap, self.PS[r, bo, 0:nq], rc.ap[r, 0:nq], ALU.mult, [self.PSR[bo]] + rc.regs, dst_regs)


def _attn_bufs(self):
    self.Pb = [self.lalloc((512,), BF16) for _ in range(2)]
    self.pi = 0
    self.rcb = self.lalloc((512,), F32)


def _mix_B(self, l):
    w = self.w_in[l]
    QO, QOr = self.QO[1], self.QOr[1]
    self.lreset()
    KB = self.lalloc((2, T), BF16)
    KZ = [self.lalloc((T,), BF16) for _ in range(4)]
    VB = self.lalloc((16, 256), BF16)
    maskT = self.lalloc((T,), BF16)
    SEL = self.lalloc((4096,), BF16)
    for kz in KZ:
        self.S.op("dve", lambda hh, kz=kz: hh.memset(kz.ap, 0.0), [], kz.regs)
    self.S.op("dve", lambda hh: hh.memset(maskT.ap, 0.0), [], maskT.regs)
    self.S.op("dve", lambda hh: hh.memset(SEL.ap, 0.0), [], SEL.regs)
    self.dma("pool", SEL.ap[0:32, :], self.din["c_sel"], "misc0", [], SEL.regs)
    bmk = self.lalloc((256,), F32)
    self.dma("sp", bmk.ap, self.din["c_bmk"], "misc1", [], bmk.regs)
    small = self.lalloc((256,), F32)
    kms = self.lalloc((2, 8), BF16)
    mb = self.lalloc((32,), BF16)
    mark = self.loc_cur
    self.hn_alloc()
    gq = VEC_QK + l * 16
    self.proj_fm(w, [(O_BQ, 256)], lambda ti, c, bk, m: self.head_norm(
        bk, 128, gq + 2, VEC_EPS + 1, 1.0, QO[:, ti, c * 512:(c + 1) * 512], [QOr[ti][c]]))
    def cb_bk(ti, c, bk, m):
        cs = slice(c * 512, (c + 1) * 512)
        self.head_norm(bk, 128, gq + 3, VEC_EPS, 1.0 / 64, None, KB.regs + KZ[2 * ti].regs + KZ[2 * ti + 1].regs,
                       split=(KZ[2 * ti].ap[0:64, cs], KZ[2 * ti + 1].ap[64:128, cs]))
        self.copy("act", KB.ap[0:64, ti, cs], KZ[2 * ti].ap[0:64, cs], KZ[2 * ti].regs, KB.regs)
        self.copy("act", KB.ap[64:128, ti, cs], KZ[2 * ti + 1].ap[64:128, cs], KZ[2 * ti + 1].regs, KB.regs)
    self.proj_fm(w, [(O_BK, 256)], cb_bk)
    self.proj_tm(w, [(O_BV, 256)], lambda tt, bk, o: self.copy(
        "act", VB.ap[:, tt, :], self.PS[:, bk, 0:256], [self.PSR[bk]], VB.regs))
    import os
    CUT = int(os.environ.get("BCUT", "9"))
    if CUT <= 1:
        return
    for j in range(2):
        for n in range(8):
            o = small.ap[:, j * 8 + n:j * 8 + n + 1]
            i = KB.ap[:, j, n * 256:(n + 1) * 256]
            self.S.op("dve", lambda h, o=o, i=i: h.reduce_sum(out=o, in_=i, axis=mybir.AxisListType.X),
                      KB.regs, small.regs)
    self.copy("dve", kms.ap, small.ap[:, 0:16].rearrange("p (a b) -> p a b", a=2), small.regs, kms.regs)
    gm = small.ap[:, 16:48]
    mx = small.ap[:, 48:80]
    C2 = int(os.environ.get("BCUT2", "9"))
    if C2 <= 0:
        return
    for qt in range(16):
        own = qt // 2
        bk = self.bank()
        for h in range(4):
            j, e = h // 2, h % 2
            self.mm(self.PS[:, bk, h * 8:(h + 1) * 8], QO[64 * e:64 * e + 64, j, qt * 128:(qt + 1) * 128],
                    kms.ap[64 * e:64 * e + 64, j, :], True, True, [QOr[j][qt // 4]] + kms.regs, [self.PSR[bk]])
        self.tt("dve", gm, self.PS[:, bk, 0:32], bmk.ap[:, own * 32:(own + 1) * 32], ALU.add,
                [self.PSR[bk]] + bmk.regs, small.regs)
        if C2 <= 1:
            continue
        for h in range(4):
            o = mx[:, h * 8:(h + 1) * 8]
            i = gm[:, h * 8:(h + 1) * 8]
            self.S.op("dve", lambda hh, o=o, i=i: hh.max(out=o, in_=i), small.regs, small.regs)
        if C2 <= 2:
            continue
        for h in range(4):
            self.ts("dve", mb.ap[:, h * 8:(h + 1) * 8], gm[:, h * 8:(h + 1) * 8], mx[:, h * 8 + 2:h * 8 + 3],
                    ALU.is_lt, small.regs, mb.regs, s2=NEG, op1=ALU.mult)
        if C2 <= 3:
            continue
        mv = mb.ap.rearrange("p (a b) -> p a b", a=4)[:, :, own:8]
        self.S.op("dve", lambda hh, mv=mv: hh.memset(mv, 0.0), [], mb.regs)
        if C2 <= 4:
            continue
        bt = self.bank()
        po = self.PS[0:32, bt, 0:128]
        self.mm(po, mb.ap, self.cT["ident"], True, True, mb.regs + [self.creg], [self.PSR[bt]])
        if C2 <= 5:
            continue
        self.copy("act", maskT.ap[0:32, qt * 128:(qt + 1) * 128], po, [self.PSR[bt]], maskT.regs)
    if CUT <= 2:
        return
    self.loc_cur = mark
    self.attn_bufs()
    strips = [self.lalloc((T,), BF16) for _ in range(2)]
    anti = self.cT["anti"]
    for h in range(4):
        if CUT <= 3 and h >= 1:
            break
        j, e = h // 2, h % 2
        st = strips[h % 2]
        self.load_strip(st, self.din["gext"], 4 + h, T, "strip%d" % (h % 2))
        rs_ = slice(64 * e, 64 * e + 64)
        for c in range(NCH):
            def qk_fn(sb, qs, n):
                return [(0, n, KZ[h].ap[:, sb * 128:(sb + 1) * 128], QO[:, j, qs:qs + n],
                         KZ[h].regs + [QOr[j][c]])]

            def bias_fn(sb, qs, n):
                off = qs - sb * 128
                nb = sb // 2
                return [(0, n, anti, st.ap[:, off:off + n], st.regs + [self.creg]),
                        (0, n, SEL.ap[:, (h * 8 + nb) * 128:(h * 8 + nb + 1) * 128], maskT.ap[:, qs:qs + n],
                         SEL.regs + maskT.regs)]

            def v_fn(sb):
                return VB.ap[:, sb, j * 128:(j + 1) * 128], VB.regs
            self.attn_seg(c * 512, 512, 4 * (c + 1), qk_fn, bias_fn, v_fn, e,
                          QO[rs_, j, c * 512:(c + 1) * 512], [QOr[j][c]])


for _n, _f in list(globals().items()):
    if _n.startswith("_") and callable(_f) and _n[1:] in (
            "hn_alloc", "vcol", "proj_fm", "proj_tm", "head_norm", "load_strip", "attn_seg", "attn_bufs", "mix_B"):
        setattr(Builder, _n[1:], _f)

def _merge(self, l):
    w = self.w_in[l]
    wbr = self.w_branch[l]
    wo = self.w_out[l]
    self.lreset()
    mT = self.lalloc((8, T), BF16)
    mp = lambda d, c: mT.regs[d * 4 + c:d * 4 + c + 1]
    gs = [self.lalloc((512,), F32) for _ in range(2)]
    acc = [self.lalloc((512,), F32) for _ in range(2)]
    tmp = [self.lalloc((512,), F32) for _ in range(2)]
    gi = 0
    for dp in range(4):
        d0 = dp * 256
        wg = []
        for half in range(2):
            wg.append(self.wload([(w[:, O_G + (2 * half) * 1024 + d0:O_G + (2 * half) * 1024 + d0 + 256], 0, 256),
                                  (w[:, O_G + (2 * half + 1) * 1024 + d0:O_G + (2 * half + 1) * 1024 + d0 + 256], 256, 256)], 8))
        wb, wbr_r = self.wload([(wbr[n, :, d0:d0 + 256], n * 256, 256) for n in range(4)], 2)
        for c in range(NCH):
            cs = slice(c * 512, (c + 1) * 512)
            for dt in range(2):
                d = dp * 2 + dt
                a = acc[(c * 2 + dt) % 2]
                for n in range(4):
                    wv, wr = wg[n // 2]
                    bg = self.bank()
                    for k in range(8):
                        self.mm(self.PS[:, bg, :], wv[:, k, (n % 2) * 256 + dt * 128:(n % 2) * 256 + (dt + 1) * 128],
                                self.hT[:, k, cs], k == 0, k == 7, wr + [self.hTr[k][c]], [self.PSR[bg]])
                    g = gs[gi % 2]
                    t = tmp[gi % 2]
                    gi += 1
                    self.act(g.ap, self.PS[:, bg, :], AF.Sigmoid, [self.PSR[bg]], g.regs,
                             bias=self.vcol(VEC_BG + (l * 4 + n) * 8 + d))
                    bm = self.bankA()
                    for jj in range(2):
                        self.mm(self.PS[:, bm, :], wb[:, jj, n * 256 + dt * 128:n * 256 + (dt + 1) * 128],
                                self.QO[n][:, jj, cs], jj == 0, jj == 1, wbr_r + [self.QOr[n][jj][c]], [self.PSR[bm]])
                    if n == 0:
                        self.tt("dve", a.ap, g.ap, self.PS[:, bm, :], ALU.mult, g.regs + [self.PSR[bm]], a.regs)
                    else:
                        self.tt("dve", t.ap, g.ap, self.PS[:, bm, :], ALU.mult, g.regs + [self.PSR[bm]], t.regs)
                        if n < 3:
                            self.tt("dve", a.ap, a.ap, t.ap, ALU.add, a.regs + t.regs, a.regs)
                        else:
                            self.tt("dve", mT.ap[:, d, cs], a.ap, t.ap, ALU.add, a.regs + t.regs, mp(d, c))
    for dq in range(2):
        wv, wr = self.wload([(wo[:, dq * 512:(dq + 1) * 512], 0, 512)], 8)
        for c in range(NCH):
            cs = slice(c * 512, (c + 1) * 512)
            for dt in range(4):
                d = dq * 4 + dt
                bk = self.bank()
                for k in range(8):
                    self.mm(self.PS[:, bk, :], wv[:, k, dt * 128:(dt + 1) * 128], mT.ap[:, k, cs], k == 0, k == 7,
                            wr + mp(k, c), [self.PSR[bk]])
                self.tt("dve", self.xT[:, d, cs], self.PS[:, bk, :], self.xT[:, d, cs], ALU.add,
                        [self.PSR[bk], self.xTr[d][c]], [self.xTr[d][c]])


def _mixer(self, l):
    self.lreset()
    self.rmsnorm_x(VEC_NG + (l * 3 + 1) * 8)
    en = self.dbg if self.dbg else "ABCD"
    if "A" in en:
        self.mix_A(l)
    if "B" in en:
        self.mix_B(l)
    if "C" in en:
        self.mix_C(l)
    if "D" in en:
        self.mix_D(l)
    if self.dbg:
        for n in range(4):
            if "ABCD"[n] in en:
                rr = [r for pr in self.QOr[n] for r in pr]
                self.dma("sp", self.dbg_out[n], self.QO[n], "out0", rr, [])
        return
    self.merge(l)


Builder.merge = _merge
Builder.mixer = _mixer


def _mix_C(self, l):
    w = self.w_in[l]
    QO, QOr = self.QO[2], self.QOr[2]
    self.lreset()
    QR = self.lalloc((2, T), BF16)
    KC = self.lalloc((2, T), BF16)
    KR = self.lalloc((2, T), BF16)
    VC = self.lalloc((16, 256), BF16)
    Em = self.lalloc((128,), BF16)
    Fm = self.lalloc((64,), BF16)
    Gm = self.lalloc((64,), BF16)
    ROT = self.lalloc((64,), F32)
    stc = self.lalloc((512,), BF16)
    self.dma("pool", Em.ap[0:64, :], self.din["c_E"], "cE", [], Em.regs)
    self.dma("pool", Fm.ap, self.din["c_F"], "cF", [], Fm.regs)
    self.dma("pool", Gm.ap[0:64, :], self.din["c_G"], "cG", [], Gm.regs)
    self.dma("sp", ROT.ap[0:64, :], self.din["c_rot"], "cROT", [], ROT.regs)
    self.load_strip(stc, self.din["gext"], 8, 512, "strip0")
    mark = self.loc_cur
    cqg = self.lalloc((3, 512), BF16)
    rl = self.lalloc((512,), F32)
    u_n = self.lalloc((512,), F32)
    u_r = self.lalloc((512,), F32)
    u_k = self.lalloc((512,), F32)
    sq_n = self.lalloc((512,), BF16)
    sq_r = self.lalloc((512,), BF16)
    rs_n = self.lalloc((512,), F32)
    rs_r = self.lalloc((512,), F32)
    cos = self.lalloc((512,), F32)
    sin = self.lalloc((512,), F32)
    t1 = self.lalloc((512,), F32)
    t2 = self.lalloc((512,), F32)
    rtm = self.lalloc((8,), F32)
    ones = self.cT["ones"]
    bd64 = self.cT["bd64"]
    gv = VEC_QK + l * 16
    H = slice(0, 64)
    st = {}

    def headnorm_rope(src_r, gain_n, gain_r, eps_col, ss_scale, dst_n, dst_n_regs, dst_r, dst_r_regs, c):
        cs = slice(c * 512, (c + 1) * 512)
        self.act(sq_n.ap, u_n.ap, AF.Square, u_n.regs, sq_n.regs)
        self.act(sq_r.ap[H, :], src_r.ap[H, :], AF.Square, src_r.regs, sq_r.regs)
        bsn = self.bankA()
        self.mm(self.PS[:, bsn, :], bd64, sq_n.ap, True, False, sq_n.regs + [self.creg], [self.PSR[bsn]])
        self.mm(self.PS[:, bsn, :], Em.ap[H, :], sq_r.ap[H, :], False, True, sq_r.regs + Em.regs, [self.PSR[bsn]])
        bsr = self.bankA()
        self.mm(self.PS[H, bsr, :], Fm.ap[:, 0:64], sq_n.ap, True, False, sq_n.regs + Fm.regs, [self.PSR[bsr]])
        self.mm(self.PS[H, bsr, :], Gm.ap[H, 0:64], sq_r.ap[H, :], False, True, sq_r.regs + Gm.regs, [self.PSR[bsr]])
        self.act(rs_n.ap, self.PS[:, bsn, :], AF.Ln, [self.PSR[bsn]], rs_n.regs, scale=ss_scale, bias=self.vcol(eps_col))
        self.act(rs_n.ap, rs_n.ap, AF.Exp, rs_n.regs, rs_n.regs, scale=-0.5)
        self.act(rs_r.ap[H, :], self.PS[H, bsr, :], AF.Ln, [self.PSR[bsr]], rs_r.regs, scale=ss_scale,
                 bias=self.vecs[H, eps_col:eps_col + 1])
        self.act(rs_r.ap[H, :], rs_r.ap[H, :], AF.Exp, rs_r.regs, rs_r.regs, scale=-0.5)
        self.stt(dst_n, u_n.ap, self.vcol(gain_n), rs_n.ap, ALU.mult, ALU.mult, u_n.regs + rs_n.regs + [self.creg], dst_n_regs)
        self.stt(t1.ap[H, :], src_r.ap[H, :], self.vecs[H, gain_r:gain_r + 1], rs_r.ap[H, :], ALU.mult, ALU.mult,
                 src_r.regs + rs_r.regs + [self.creg], t1.regs)
        import os
        if int(os.environ.get("CQ", "9")) <= 3:
            return
        bp = self.bank()
        self.mm(self.PS[H, bp, :], ROT.ap[H, 0:64], t1.ap[H, :], True, True, ROT.regs + t1.regs, [self.PSR[bp]])
        self.tt("dve", t2.ap[H, :], self.PS[H, bp, :], sin.ap[H, :], ALU.mult, [self.PSR[bp]] + sin.regs, t2.regs)
        self.tt("dve", t1.ap[H, :], t1.ap[H, :], cos.ap[H, :], ALU.mult, t1.regs + cos.regs, t1.regs)
        self.tt("dve", dst_r, t1.ap[H, :], t2.ap[H, :], ALU.add, t1.regs + t2.regs, dst_r_regs)

    def load_cs(c):
        self.dma("sp", cos.ap[H, :], self.din["c_cos"][:, c * 512:(c + 1) * 512], "ccos", [], cos.regs)
        self.dma("sp", sin.ap[H, :], self.din["c_sin"][:, c * 512:(c + 1) * 512], "csin", [], sin.regs)

    wuq = self.w_uq[l]
    pcs = []
    for h in range(4):
        pcs.append((wuq[:, h * 96:h * 96 + 64], h * 64, 64))
        pcs.append((wuq[:, h * 96 + 64:h * 96 + 96], 256 + h * 32, 32))
    import os
    CD = int(os.environ.get("CD", "9"))
    if CD <= 0:
        return
    wq, wq_r = self.wload(pcs, 3)
    if CD <= 1:
        load_cs(0)
        return

    def cb_q(ti, c, bk, m):
        cs = slice(c * 512, (c + 1) * 512)
        if ti == 0:
            st["ss"] = self.bankA()
            load_cs(c)
        bss = st["ss"]
        self.act(sq_n.ap, self.PS[:, bk, :], AF.Square, [self.PSR[bk]], sq_n.regs)
        self.mm(self.PS[:, bss, :], ones, sq_n.ap, ti == 0, ti == 2, sq_n.regs + [self.creg], [self.PSR[bss]])
        self.ts("dve", cqg.ap[:, ti, :], self.PS[:, bk, :], self.vcol(gv + 10 + ti), ALU.mult,
                [self.PSR[bk], self.creg], cqg.regs)
        if ti < 2:
            return
        import os
        CQ = int(os.environ.get("CQ", "9"))
        if CQ <= 1:
            return
        self.act(rl.ap, self.PS[:, bss, :], AF.Ln, [self.PSR[bss]], rl.regs, scale=1.0 / 384, bias=self.vcol(VEC_EPS))
        self.act(rl.ap, rl.ap, AF.Exp, rl.regs, rl.regs, scale=-0.5)
        for j in range(2):
            bn = self.bank()
            for k in range(3):
                self.mm(self.PS[:, bn, :], wq[:, k, j * 128:(j + 1) * 128], cqg.ap[:, k, :], k == 0, k == 2,
                        wq_r + cqg.regs, [self.PSR[bn]])
            self.tt("dve", u_n.ap, self.PS[:, bn, :], rl.ap, ALU.mult, [self.PSR[bn]] + rl.regs, u_n.regs)
            br = self.bank()
            for k in range(3):
                self.mm(self.PS[H, br, :], wq[:, k, 256 + j * 64:256 + (j + 1) * 64], cqg.ap[:, k, :], k == 0, k == 2,
                        wq_r + cqg.regs, [self.PSR[br]])
            self.tt("dve", u_r.ap[H, :], self.PS[H, br, :], rl.ap[H, :], ALU.mult, [self.PSR[br]] + rl.regs, u_r.regs)
            if CQ <= 2:
                continue
            headnorm_rope(u_r, gv + 6, gv + 8, VEC_EPS + 2, 1.0, QO[:, j, cs], [QOr[j][c]],
                          QR.ap[H, j, cs], QR.regs, c)

    self.proj_fm(w, [(O_CQ, 384)], cb_q)
    import os
    CC = int(os.environ.get("CCUT", "9"))
    if CC <= 1:
        return

    wukv = self.w_ukv[l]
    pcs = []
    for h in range(4):
        pcs.append((wukv[:, h * 128:h * 128 + 64], h * 64, 64))
        pcs.append((wukv[:, h * 128 + 64:h * 128 + 128], 256 + h * 64, 64))
    wk, wk_r = self.wload(pcs, 2)
    ckvg = cqg

    def cb_k(ti, c, bk, m):
        cs = slice(c * 512, (c + 1) * 512)
        if ti == 0:
            st["ss"] = self.bankA()
            st["tm"] = self.bankA()
            load_cs(c)
        bss, btm = st["ss"], st["tm"]
        if ti < 2:
            self.act(sq_n.ap, self.PS[:, bk, :], AF.Square, [self.PSR[bk]], sq_n.regs)
            self.mm(self.PS[:, bss, :], ones, sq_n.ap, ti == 0, ti == 1, sq_n.regs + [self.creg], [self.PSR[bss]])
            for a in range(4):
                self.mm(self.PS[:, btm, a:a + 1], sq_n.ap[:, a * 128:(a + 1) * 128], ones[:, 0:1],
                        ti == 0 and a == 0, ti == 1 and a == 3, sq_n.regs + [self.creg], [self.PSR[btm]])
            self.ts("dve", ckvg.ap[:, ti, :], self.PS[:, bk, :], self.vcol(gv + 13 + ti), ALU.mult,
                    [self.PSR[bk], self.creg], ckvg.regs)
            return
        self.copy("act", u_k.ap[H, :], self.PS[H, bk, :], [self.PSR[bk]], u_k.regs)
        self.act(rl.ap, self.PS[:, bss, :], AF.Ln, [self.PSR[bss]], rl.regs, scale=1.0 / 256, bias=self.vcol(VEC_EPS))
        self.act(rl.ap, rl.ap, AF.Exp, rl.regs, rl.regs, scale=-0.5)
        self.act(rtm.ap[:, 0:4], self.PS[:, btm, 0:4], AF.Ln, [self.PSR[btm]], rtm.regs, scale=1.0 / 256,
                 bias=self.vcol(VEC_EPS))
        self.act(rtm.ap[:, 0:4], rtm.ap[:, 0:4], AF.Exp, rtm.regs, rtm.regs, scale=-0.5)
        for j in range(2):
            bn = self.bank()
            for k in range(2):
                self.mm(self.PS[:, bn, :], wk[:, k, j * 128:(j + 1) * 128], ckvg.ap[:, k, :], k == 0, k == 1,
                        wk_r + ckvg.regs, [self.PSR[bn]])
            self.tt("dve", u_n.ap, self.PS[:, bn, :], rl.ap, ALU.mult, [self.PSR[bn]] + rl.regs, u_n.regs)
            headnorm_rope(u_k, gv + 7, gv + 9, VEC_EPS, 1.0 / 96, KC.ap[:, j, cs], KC.regs,
                          KR.ap[H, j, cs], KR.regs, c)
        for a in range(4):
            bv = self.bank()
            for k in range(2):
                self.mm(self.PS[:, bv, 0:256], ckvg.ap[:, k, a * 128:(a + 1) * 128], wk[:, k, 256:512], k == 0, k == 1,
                        wk_r + ckvg.regs, [self.PSR[bv]])
            self.ts("dve", VC.ap[:, c * 4 + a, :], self.PS[:, bv, 0:256], rtm.ap[:, a:a + 1], ALU.mult,
                    [self.PSR[bv]] + rtm.regs, VC.regs)

    self.proj_fm(w, [(O_CKV, 256), (O_CKR, 32), (O_CKR, 32)], cb_k, tiles=[(0, 128), (128, 128), (256, 64)])

    if CC <= 2:
        return
    self.loc_cur = mark
    self.attn_bufs()
    anti = self.cT["anti"]
    for h in range(4):
        j, e = h // 2, h % 2
        rs_ = slice(64 * e, 64 * e + 64)
        rr = slice(32 * e, 32 * e + 32)
        for c in range(NCH):
            def qk_fn(sb, qs, n):
                return [(0, n, KC.ap[rs_, j, sb * 128:(sb + 1) * 128], QO[rs_, j, qs:qs + n], KC.regs + [QOr[j][c]]),
                        (0, n, KR.ap[rr, j, sb * 128:(sb + 1) * 128], QR.ap[rr, j, qs:qs + n], KR.regs + QR.regs)]

            def bias_fn(sb, qs, n):
                if qs != sb * 128:
                    return []
                return [(0, n, anti, stc.ap[:, 0:n], stc.regs + [self.creg])]

            def v_fn(sb):
                return VC.ap[:, sb, j * 128:(j + 1) * 128], VC.regs
            self.attn_seg(c * 512, 512, 4 * (c + 1), qk_fn, bias_fn, v_fn, e,
                          QO[rs_, j, c * 512:(c + 1) * 512], [QOr[j][c]])


Builder.mix_C = _mix_C


def _mix_D(self, l):
    w = self.w_in[l]
    QO, QOr = self.QO[3], self.QOr[3]
    self.lreset()
    nacc = self.lalloc((T,), F32)
    dacc = self.lalloc((T,), F32)
    sd = [self.lalloc((256,), BF16) for _ in range(6)]
    QD = self.lalloc((T,), BF16)
    KD = [self.lalloc((T,), BF16) for _ in range(2)]
    for kz in KD:
        self.S.op("dve", lambda hh, kz=kz: hh.memset(kz.ap, 0.0), [], kz.regs)
    VD = self.lalloc((16, 128), BF16)
    self.hn_alloc()
    self.attn_bufs()
    anti = self.cT["anti"]
    ones = self.cT["ones"]
    gv = VEC_QK + l * 16
    allh = lambda k: [self.hTr[k][c] for c in range(NCH)]
    for j in range(2):
        for g, dil in enumerate((1, 4, 16)):
            nbk = 16 // dil
            for e in range(2):
                self.load_strip(sd[g * 2 + e], self.din["gd"], g * 4 + 2 * j + e, 256, "sd%d" % (g * 2 + e))

            def cb(ti, c, bk, m):
                cs = slice(c * 512, (c + 1) * 512)
                if ti == 0:
                    self.head_norm(bk, 128, gv + 4, VEC_EPS + 1, 1.0, QD.ap[:, cs], QD.regs)
                else:
                    self.head_norm(bk, 128, gv + 5, VEC_EPS, 1.0 / 64, None, KD[0].regs + KD[1].regs,
                                   split=(KD[0].ap[0:64, cs], KD[1].ap[64:128, cs]))
            self.proj_fm(w, [(O_DQ + g * 256 + j * 128, 128), (O_DK + g * 256 + j * 128, 128)], cb)

            def tok_ap(k, blk):
                r, n = blk // nbk, blk % nbk
                base = r + dil * n * 128
                return self.hT[:, k, base:base + 127 * dil + 1:dil], allh(k)
            self.proj_tm(w, [(O_DV + g * 256 + j * 128, 128)],
                         lambda blk, bk, o: self.copy("act", VD.ap[:, blk, :], self.PS[:, bk, 0:128], [self.PSR[bk]], VD.regs),
                         tok_ap=tok_ap)
            for e in range(2):
                rows = slice(64 * e, 64 * e + 64)
                st = sd[g * 2 + e]
                qlist = [(r, n) for r in range(dil) for n in range(nbk)]
                for i0 in range(0, len(qlist), 2):
                    tiles = []
                    qinfo = []
                    for (r, n) in qlist[i0:i0 + 2]:
                        bo = self.bankA()
                        bd = self.bankA()
                        qb = r + dil * n * 128
                        qsl = slice(qb, qb + 127 * dil + 1, dil)
                        kbs = ([n - 1] if n > 0 else []) + [n]
                        qinfo.append((bo, bd, qsl))
                        for i, kn in enumerate(kbs):
                            tiles.append(dict(bo=bo, bd=bd, qsl=qsl, kb=r + dil * kn * 128, off=128 if kn != n else 0,
                                              blk=r * nbk + kn, first=(i == 0), last=(i == len(kbs) - 1),
                                              bs=self.bank()))
                    for t in tiles:
                        kb = t["kb"]
                        self.mm(self.PS[:, t["bs"], 0:128], KD[e].ap[:, kb:kb + 127 * dil + 1:dil], QD.ap[:, t["qsl"]],
                                True, False, KD[e].regs + QD.regs, [self.PSR[t["bs"]]])
                    for t in tiles:
                        self.mm(self.PS[:, t["bs"], 0:128], anti, st.ap[:, t["off"]:t["off"] + 128], False, True,
                                st.regs + [self.creg], [self.PSR[t["bs"]]])
                    for ti, t in enumerate(tiles):
                        p = self.Pb[ti % 2]
                        t["p"] = p
                        t["pap"] = p.ap[:, (ti // 2) * 128:(ti // 2) * 128 + 128]
                        self.act(t["pap"], self.PS[:, t["bs"], 0:128], AF.Exp, [self.PSR[t["bs"]]], p.regs)
                    for t in tiles:
                        self.mm(self.PS[:, t["bo"], 0:128], VD.ap[:, t["blk"], :], t["pap"], t["first"], t["last"],
                                VD.regs + t["p"].regs, [self.PSR[t["bo"]]])
                        self.mm(self.PS[:, t["bd"], 0:128], ones, t["pap"], t["first"], t["last"],
                                t["p"].regs + [self.creg], [self.PSR[t["bd"]]])
                    for (bo, bd, qsl) in qinfo:
                        if g == 0:
                            self.copy("act", nacc.ap[rows, qsl], self.PS[rows, bo, 0:128], [self.PSR[bo]], nacc.regs)
                            self.copy("dve", dacc.ap[rows, qsl], self.PS[rows, bd, 0:128], [self.PSR[bd]], dacc.regs)
                        else:
                            self.tt("dve", nacc.ap[rows, qsl], self.PS[rows, bo, 0:128], nacc.ap[rows, qsl], ALU.add,
                                    [self.PSR[bo]] + nacc.regs, nacc.regs)
                            self.tt("dve", dacc.ap[rows, qsl], self.PS[rows, bd, 0:128], dacc.ap[rows, qsl], ALU.add,
                                    [self.PSR[bd]] + dacc.regs, dacc.regs)
        rc = self.rcb
        for c in range(NCH):
            cs = slice(c * 512, (c + 1) * 512)
            self.act(rc.ap, dacc.ap[:, cs], AF.Ln, dacc.regs, rc.regs)
            self.act(rc.ap, rc.ap, AF.Exp, rc.regs, rc.regs, scale=-1.0)
            self.tt("dve", QO[:, j, cs], nacc.ap[:, cs], rc.ap, ALU.mult, nacc.regs + rc.regs, [QOr[j][c]])


NIT = 16
S0 = 32.0


def _mix_A(self, l):
    w = self.w_in[l]
    QO, QOr = self.QO[0], self.QOr[0]
    self.lreset()
    KA = [self.lalloc((T,), BF16) for _ in range(2)]
    for kz in KA:
        self.S.op("dve", lambda hh, kz=kz: hh.memset(kz.ap, 0.0), [], kz.regs)
    VA = self.lalloc((16, 128), BF16)
    IQ = self.lalloc((T,), BF16)
    IQ3 = self.lalloc((T,), BF16)
    IK3 = self.lalloc((T,), BF16)
    wab = self.lalloc((16, 4), F32)
    wsg = self.lalloc((16, 4), F32)
    cm = self.lalloc((128,), F32)
    self.dma("sp", cm.ap, self.din["c_cm"], "ccm", [], cm.regs)
    mark = self.loc_cur
    self.hn_alloc()
    gv = VEC_QK + l * 16
    self.proj_fm(w, [(O_AQ, 256)], lambda ti, c, bk, m: self.head_norm(
        bk, 128, gv + 0, VEC_EPS + 1, 1.0, QO[:, ti, c * 512:(c + 1) * 512], [QOr[ti][c]]))
    self.proj_fm(w, [(O_AK, 64), (O_AK, 64)], lambda ti, c, bk, m: self.head_norm(
        bk, 128, gv + 1, VEC_EPS, 1.0 / 64, None, KA[0].regs + KA[1].regs,
        split=(KA[0].ap[0:64, c * 512:(c + 1) * 512], KA[1].ap[64:128, c * 512:(c + 1) * 512])))

    def cb_i(ti, c, bk, m):
        cs = slice(c * 512, (c + 1) * 512)
        dst = (IQ, IQ3, IK3)[ti]
        self.copy("act" if ti % 2 else "dve", dst.ap[0:m, cs], self.PS[0:m, bk, :], [self.PSR[bk]], dst.regs)
    self.proj_fm(w, [(O_IQ, 128), (O_IK, 32), (O_IK, 32), (O_IK, 32)], cb_i, tiles=[(0, 96), (96, 32), (128, 96)])

    def cb_v(tt, bk, o):
        pr = [self.PSR[bk]]
        self.copy("act", VA.ap[:, tt, 0:64], self.PS[:, bk, 0:64], pr, VA.regs)
        self.copy("dve", VA.ap[:, tt, 64:128], self.PS[:, bk, 0:64], pr, VA.regs)
        self.act(wab.ap[:, tt, :], self.PS[:, bk, 64:68], AF.Abs, pr, wab.regs)
        self.act(wsg.ap[:, tt, :], self.PS[:, bk, 64:68], AF.Sign, pr, wsg.regs)
    self.proj_tm(w, [(O_AV, 64), (O_IW, 4)], cb_v)
    self.loc_cur = mark
    self.attn_bufs()
    score = self.lalloc((T,), F32)
    rt = [self.lalloc((512,), F32) for _ in range(2)]
    mbt = [self.lalloc((T,), BF16) for _ in range(2)]
    strips = [self.lalloc((T,), BF16) for _ in range(2)]
    small = self.lalloc((16,), F32)
    thr = small.ap[:, 0:1]
    cnt = small.ap[:, 1:2]
    g2 = small.ap[:, 2:3]
    anti = self.cT["anti"]
    ident = self.cT["ident"]
    ri = 0
    si = 0
    for qp in range(8):
        for i in range(2):
            qt = 2 * qp + i
            if qt < 2:
                continue
            nk = (qt + 1) * 128
            qsl = slice(qt * 128, (qt + 1) * 128)
            for s0 in range(0, nk, 512):
                sn = min(512, nk - s0)
                for h in range(4):
                    bz = self.bank()
                    if h < 3:
                        lt, rh = IQ.ap[32 * h:32 * h + 32, qsl], IK3.ap[32 * h:32 * h + 32, s0:s0 + sn]
                        rd = IQ.regs + IK3.regs
                    else:
                        lt, rh = IQ3.ap[0:32, qsl], IK3.ap[0:32, s0:s0 + sn]
                        rd = IQ3.regs + IK3.regs
                    self.mm(self.PS[:, bz, 0:sn], lt, rh, True, True, rd, [self.PSR[bz]])
                    r = rt[ri % 2]
                    ri += 1
                    self.act(r.ap[:, 0:sn], self.PS[:, bz, 0:sn], AF.Relu, [self.PSR[bz]] + wab.regs, r.regs,
                             scale=wab.ap[:, qt, h:h + 1])
                    if h == 0:
                        self.ts("dve", score.ap[:, s0:s0 + sn], r.ap[:, 0:sn], wsg.ap[:, qt, 0:1], ALU.mult,
                                r.regs + wsg.regs, score.regs)
                    else:
                        self.stt(score.ap[:, s0:s0 + sn], r.ap[:, 0:sn], wsg.ap[:, qt, h:h + 1], score.ap[:, s0:s0 + sn],
                                 ALU.mult, ALU.add, r.regs + wsg.regs + score.regs, score.regs)
            self.tt("dve", score.ap[:, qsl], score.ap[:, qsl], cm.ap, ALU.add, score.regs + cm.regs, score.regs)
            self.S.op("dve", lambda hh: hh.memset(thr, 0.0), [], small.regs)
            mb = mbt[i]
            for it in range(NIT):
                step = S0 / (2 ** it)
                self.ts("dve", mb.ap[:, 0:nk], score.ap[:, 0:nk], thr, ALU.is_gt, score.regs + small.regs,
                        mb.regs + small.regs, op1=ALU.add, accum=cnt)
                self.ts("dve", g2, cnt, 255.5, ALU.is_ge, small.regs, small.regs, s2=2.0 * step, op1=ALU.mult)
                self.stt(thr, g2, -step, thr, ALU.add, ALU.add, small.regs, small.regs)
            self.ts("dve", thr, thr, -S0 / (2 ** (NIT - 1)), ALU.add, small.regs, small.regs)
            self.ts("dve", mb.ap[:, 0:nk], score.ap[:, 0:nk], thr, ALU.is_le, score.regs + small.regs, mb.regs,
                    s2=NEG, op1=ALU.mult)
        q0 = qp * 256
        for h in range(4):
            j, e = h // 2, h % 2
            rs_ = slice(64 * e, 64 * e + 64)
            st = strips[si % 2]
            self.load_strip(st, self.din["gext"], h, q0 + 256, "strip%d" % (si % 2))
            si += 1

            def qk_fn(sb, qs, n):
                return [(0, n, KA[e].ap[:, sb * 128:(sb + 1) * 128], QO[:, j, qs:qs + n], KA[e].regs + [QOr[j][qp // 2]])]

            def bias_fn(sb, qs, n):
                off = qs - sb * 128
                out = [(0, n, anti, st.ap[:, off:off + n], st.regs + [self.creg])]
                if qp > 0:
                    for i in range(2):
                        t0 = q0 + i * 128
                        if t0 >= qs:
                            out.append((t0 - qs, 128, mbt[i].ap[:, sb * 128:(sb + 1) * 128], ident,
                                        mbt[i].regs + [self.creg]))
                return out

            def v_fn(sb):
                return VA.ap[:, sb, :], VA.regs
            self.attn_seg(q0, 256, 2 * (qp + 1), qk_fn, bias_fn, v_fn, e, QO[rs_, j, q0:q0 + 256], [QOr[j][qp // 2]])


Builder.mix_D = _mix_D
Builder.mix_A = _mix_A


_CACHE = {}


def get_nc(nseq, stages, dbg=None):
    key = (nseq, stages, dbg)
    if key not in _CACHE:
        b = Builder(nseq, stages, dbg)
        _CACHE[key] = b
        b.nc_built = b.build_wrapped()
    return _CACHE[key]


def _build_wrapped(self):
    return self.build()


Builder.build_wrapped = _build_wrapped


def make_inmap(inp):
    shared = {
        "w_ffn_in": np.ascontiguousarray(np.asarray(inp["w_ffn_in"], np.float32)),
        "w_ffn_out": np.ascontiguousarray(np.asarray(inp["w_ffn_out"], np.float32)),
        "vecs": make_vecs(inp),
    }
    for k, v in host_consts().items():
        shared["c_" + k] = v
    for k, v in host_consts2().items():
        shared["c_" + k] = v
    for k in ("w_in", "w_branch", "w_out", "w_mla_uq", "w_mla_ukv"):
        shared[k] = np.ascontiguousarray(np.asarray(inp[k], np.float32))
    shared["gext"], shared["gd"] = make_gext(inp)
    return shared


def kernel(**inp):
    ncores = 8
    nseq = 2
    b = get_nc(nseq, 99)
    x = np.ascontiguousarray(np.asarray(inp["x"], np.float32))
    shared = make_inmap(inp)
    in_maps = []
    for i in range(ncores):
        m = dict(shared)
        m["x"] = x[i * nseq:(i + 1) * nseq]
        in_maps.append(m)
    res = run_bass_kernel_spmd(b.nc_built, in_maps, core_ids=list(range(ncores)))
    return np.concatenate([r["y"] for r in res.results], axis=0)
```
